# Optimizing a Trainium2 kernel written in Bass

```python
import jax, jax.numpy as jnp
from jax import lax
import numpy as np


D_MODEL = 2048
BATCH = 4
SEQ = 4096
DEPTH = 1

N_Q_HEADS = 8
N_KV_HEADS = 2
Q_PER_KV = N_Q_HEADS // N_KV_HEADS
HEAD_DIM = 128
WINDOW = 128
WBLK = 128
GLA_HEADS = 4
GLA_DK = 128
GLA_DV = 256
GLA_LOWRANK = 16
GLA_TAU = 16.0
GLA_CHUNK = 64
ATT_Q = N_Q_HEADS * HEAD_DIM
ATT_KV = N_KV_HEADS * HEAD_DIM
GLA_QK = GLA_HEADS * GLA_DK
GLA_V = GLA_HEADS * GLA_DV
MIX_WIDTH = ATT_Q + GLA_V
IN_SIZES = (ATT_Q, ATT_KV, ATT_KV, GLA_QK, GLA_QK, GLA_V, GLA_V, GLA_LOWRANK, GLA_LOWRANK)
IN_COLS = ATT_Q + 2 * ATT_KV + 2 * GLA_QK + 2 * GLA_V + 2 * GLA_LOWRANK
MEM_LEN = 256
X_HEADS = 4
X_HEAD_DIM = D_MODEL // X_HEADS
N_GROUPS = 4
EXPERTS_PER_GROUP = 8
N_EXPERTS = N_GROUPS * EXPERTS_PER_GROUP
TOP_K = 2
D_FF_EXPERT = D_MODEL // 2
MOE_BLOCK = 256
RMS_EPS = 1e-6
NEG_INF = -1e30

kernel_name = 'hymba_swa_alibi_bigla_hmoe_encoder'


def rmsnorm(x, gain):
    xf = x.astype(jnp.float32)
    y = xf * lax.rsqrt(jnp.mean(xf * xf, axis=-1, keepdims=True) + RMS_EPS)
    return (y * gain.astype(jnp.float32)).astype(x.dtype)


def alibi_slopes():
    return 2.0 ** (-8.0 * (jnp.arange(N_Q_HEADS, dtype=jnp.float32) + 1.0) / N_Q_HEADS)


def window_attention(q, k, v, sink_logit):
    B, S = q.shape[:2]
    nb = S // WBLK
    qb = q.reshape(B, nb, WBLK, N_KV_HEADS, Q_PER_KV, HEAD_DIM)
    pad = ((0, 0), (WBLK, WBLK), (0, 0), (0, 0))

    def band(t):
        tp = jnp.pad(t, pad).reshape(B, nb + 2, WBLK, N_KV_HEADS, HEAD_DIM)
        return jnp.concatenate([tp[:, :-2], tp[:, 1:-1], tp[:, 2:]], axis=2)

    kb, vb = band(k), band(v)
    scores = jnp.einsum('bnqhgd,bnkhd->bhgnqk', qb, kb).astype(jnp.float32) * (HEAD_DIM ** -0.5)
    kpos = jnp.arange(3 * WBLK)
    qpos = jnp.arange(WBLK) + WBLK
    dist = jnp.abs(kpos[None, :] - qpos[:, None])
    kabs = jnp.arange(nb)[:, None] * WBLK + kpos[None, :] - WBLK
    valid = (dist <= WINDOW)[None] & ((kabs >= 0) & (kabs < S))[:, None, :]
    slopes = alibi_slopes().reshape(N_KV_HEADS, Q_PER_KV)
    scores = scores - slopes[:, :, None, None, None] * dist.astype(jnp.float32)
    scores = jnp.where(valid, scores, NEG_INF)
    sink = sink_logit.astype(jnp.float32).reshape(N_KV_HEADS, Q_PER_KV)[None, :, :, None, None, None]
    sink = jnp.broadcast_to(sink, scores.shape[:-1] + (1,))
    probs = jax.nn.softmax(jnp.concatenate([scores, sink], axis=-1), axis=-1)[..., :-1]
    out = jnp.einsum('bhgnqk,bnkhd->bnqhgd', probs.astype(v.dtype), vb)
    return out.reshape(B, S, ATT_Q)


def gla_chunked(q, k, v, g):
    B, H, S, DK = q.shape
    DV = v.shape[-1]
    C = GLA_CHUNK
    N = S // C
    q = q.reshape(B, H, N, C, DK)
    k = k.reshape(B, H, N, C, DK)
    g = g.reshape(B, H, N, C, DK)
    v = v.reshape(B, H, N, C, DV)
    b = jnp.cumsum(g, axis=3)
    b_end = b[:, :, :, -1:, :]
    q_dec = q * jnp.exp(b)
    attn = jnp.einsum('bhncd,bhnsd->bhncs', q_dec, k * jnp.exp(-b))
    lower_tri = jnp.tril(jnp.ones((C, C), dtype=bool))
    attn = jnp.where(lower_tri, attn, 0.0)
    o = jnp.einsum('bhncs,bhnsv->bhncv', attn, v)
    chunk_kv = jnp.einsum('bhncd,bhncv->bhndv', k * jnp.exp(b_end - b), v)
    chunk_decay = jnp.exp(b_end[:, :, :, 0, :])

    def step(state, inp):
        dec, kv = inp
        return dec[..., None] * state + kv, state

    init = jnp.zeros((B, H, DK, DV), q.dtype)
    _, states = lax.scan(step, init, (jnp.moveaxis(chunk_decay, 2, 0), jnp.moveaxis(chunk_kv, 2, 0)))
    states = jnp.moveaxis(states, 0, 2)
    o = o + jnp.einsum('bhncd,bhndv->bhncv', q_dec, states)
    return o.reshape(B, H, S, DV)


def parallel_head_mixer(u, w_in, attn_out_norm, sink_logit, w_gla_gf, b_gla_gf, w_gla_gb, b_gla_gb, gla_out_norm, w_out):
    B, S, _ = u.shape
    proj = u @ w_in
    cuts = [sum(IN_SIZES[:i + 1]) for i in range(len(IN_SIZES) - 1)]
    q_a, k_a, v_a, q_g, k_g, v_g, r_g, lr_f, lr_b = jnp.split(proj, cuts, axis=-1)
    o_a = window_attention(q_a.reshape(B, S, N_KV_HEADS, Q_PER_KV, HEAD_DIM),
                           k_a.reshape(B, S, N_KV_HEADS, HEAD_DIM),
                           v_a.reshape(B, S, N_KV_HEADS, HEAD_DIM), sink_logit)
    o_a = rmsnorm(o_a, attn_out_norm)

    def heads(t, d):
        return t.reshape(B, S, GLA_HEADS, d).transpose(0, 2, 1, 3).astype(jnp.float32)

    qg = heads(q_g, GLA_DK) * (GLA_DK ** -0.5)
    kg = heads(k_g, GLA_DK)
    vg = heads(v_g, GLA_DV)
    g_f = heads(jax.nn.log_sigmoid((lr_f @ w_gla_gf + b_gla_gf).astype(jnp.float32)) / GLA_TAU, GLA_DK)
    g_b = heads(jax.nn.log_sigmoid((lr_b @ w_gla_gb + b_gla_gb).astype(jnp.float32)) / GLA_TAU, GLA_DK)
    o_fwd = gla_chunked(qg, kg, vg, g_f)
    flip = lambda t: jnp.flip(t, axis=2)
    o_bwd = flip(gla_chunked(flip(qg), flip(kg), flip(vg), flip(g_b)))
    o_g = rmsnorm(o_fwd + o_bwd, gla_out_norm)
    o_g = o_g.transpose(0, 2, 1, 3).reshape(B, S, GLA_V).astype(u.dtype) * jax.nn.silu(r_g)
    return jnp.concatenate([o_a.astype(u.dtype), o_g], axis=-1) @ w_out


def memory_cross_attention(hn, memn, w_cq, w_ck, w_cv, w_co):
    B, S, _ = hn.shape
    M = memn.shape[1]
    q = (hn @ w_cq).reshape(B, S, X_HEADS, X_HEAD_DIM)
    k = (memn @ w_ck).reshape(B, M, X_HEADS, X_HEAD_DIM)
    v = (memn @ w_cv).reshape(B, M, X_HEADS, X_HEAD_DIM)
    s = jnp.einsum('bshd,bmhd->bhsm', q, k).astype(jnp.float32) * (X_HEAD_DIM ** -0.5)
    p = jax.nn.softmax(s, axis=-1).astype(v.dtype)
    o = jnp.einsum('bhsm,bmhd->bshd', p, v).reshape(B, S, D_MODEL)
    return o @ w_co


def hierarchical_moe(hn, w_rg, b_rg, w_re, b_re, w_gate, w_up, w_down):
    B, S, D = hn.shape
    T = B * S
    xf = hn.reshape(T, D)
    g_logits = (xf @ w_rg).astype(jnp.float32) + b_rg.astype(jnp.float32)
    g_prob = jax.nn.softmax(g_logits, axis=-1)
    _, g_idx = lax.top_k(g_logits, 1)
    p_group = jnp.take_along_axis(g_prob, g_idx, axis=-1)
    e_logits = ((xf @ w_re).astype(jnp.float32) + b_re.astype(jnp.float32)).reshape(T, N_GROUPS, EXPERTS_PER_GROUP)
    sel = jnp.broadcast_to(g_idx[:, :, None], (T, 1, EXPERTS_PER_GROUP))
    in_group = jnp.take_along_axis(e_logits, sel, axis=1)[:, 0]
    e_top, e_local = lax.top_k(in_group, TOP_K)
    gate = jax.nn.softmax(e_top, axis=-1) * p_group
    e_idx = g_idx * EXPERTS_PER_GROUP + e_local

    A = T * TOP_K
    e_flat = e_idx.reshape(A)
    tok_flat = jnp.arange(A, dtype=jnp.int32) // TOP_K
    w_flat = gate.reshape(A)
    order = jnp.argsort(e_flat)
    e_s, tok_s, w_s = e_flat[order], tok_flat[order], w_flat[order]
    counts = jnp.zeros((N_EXPERTS,), jnp.int32).at[e_flat].add(1)
    padded = ((counts + MOE_BLOCK - 1) // MOE_BLOCK) * MOE_BLOCK
    start = jnp.cumsum(counts) - counts
    pend = jnp.cumsum(padded)
    pstart = pend - padded
    dest = pstart[e_s] + (jnp.arange(A, dtype=jnp.int32) - start[e_s])
    R = A + N_EXPERTS * MOE_BLOCK
    row_tok = jnp.zeros((R,), jnp.int32).at[dest].set(tok_s)
    row_w = jnp.zeros((R,), jnp.float32).at[dest].set(w_s)
    n_blk = R // MOE_BLOCK
    blk_e = jnp.minimum(jnp.searchsorted(pend, jnp.arange(n_blk, dtype=jnp.int32) * MOE_BLOCK, side='right'), N_EXPERTS - 1)
    xs = xf[row_tok].reshape(n_blk, MOE_BLOCK, D)

    def expert_block(args):
        xb, e = args
        hid = jax.nn.silu(xb @ w_gate[e]) * (xb @ w_up[e])
        return hid @ w_down[e]

    ys = lax.map(expert_block, (xs, blk_e)).reshape(R, D)
    out = jnp.zeros_like(xf).at[row_tok].add(ys * row_w[:, None].astype(ys.dtype))
    return out.reshape(B, S, D)


def setup_inputs(seed: int = 0) -> dict:
    key = jax.random.key(seed)
    ks = list(jax.random.split(key, 32))
    L = DEPTH
    cnt = [0]

    def nk():
        cnt[0] += 1
        return ks[cnt[0] - 1]

    def nrm(shape, fan_in):
        return jax.random.normal(nk(), shape, jnp.float32) * (fan_in ** -0.5)

    def gain(shape):
        return 1.0 + 0.05 * jax.random.normal(nk(), shape, jnp.float32)

    def small(shape, scale):
        return scale * jax.random.normal(nk(), shape, jnp.float32)

    return {
        'x': jax.random.normal(nk(), (BATCH, SEQ, D_MODEL), jnp.float32),
        'mem': jax.random.normal(nk(), (BATCH, MEM_LEN, D_MODEL), jnp.float32),
        'norm_mix': gain((L, D_MODEL)),
        'w_in': nrm((L, D_MODEL, IN_COLS), D_MODEL),
        'attn_out_norm': gain((L, ATT_Q)),
        'sink_logit': small((L, N_Q_HEADS), 0.5),
        'w_gla_gf': nrm((L, GLA_LOWRANK, GLA_QK), GLA_LOWRANK),
        'b_gla_gf': small((L, GLA_QK), 0.1),
        'w_gla_gb': nrm((L, GLA_LOWRANK, GLA_QK), GLA_LOWRANK),
        'b_gla_gb': small((L, GLA_QK), 0.1),
        'gla_out_norm': gain((L, GLA_DV)),
        'w_out': nrm((L, MIX_WIDTH, D_MODEL), MIX_WIDTH),
        'norm_cross': gain((L, D_MODEL)),
        'norm_mem': gain((L, D_MODEL)),
        'w_cq': nrm((L, D_MODEL, D_MODEL), D_MODEL),
        'w_ck': nrm((L, D_MODEL, D_MODEL), D_MODEL),
        'w_cv': nrm((L, D_MODEL, D_MODEL), D_MODEL),
        'w_co': nrm((L, D_MODEL, D_MODEL), D_MODEL),
        'norm_ffn': gain((L, D_MODEL)),
        'w_router_group': nrm((L, D_MODEL, N_GROUPS), D_MODEL),
        'b_router_group': small((L, N_GROUPS), 0.01),
        'w_router_expert': nrm((L, D_MODEL, N_EXPERTS), D_MODEL),
        'b_router_expert': small((L, N_EXPERTS), 0.01),
        'w_gate': nrm((L, N_EXPERTS, D_MODEL, D_FF_EXPERT), D_MODEL),
        'w_up': nrm((L, N_EXPERTS, D_MODEL, D_FF_EXPERT), D_MODEL),
        'w_down': nrm((L, N_EXPERTS, D_FF_EXPERT, D_MODEL), D_FF_EXPERT),
        'norm_final': gain((D_MODEL,)),
    }


def reference(x, mem, norm_mix, w_in, attn_out_norm, sink_logit, w_gla_gf, b_gla_gf, w_gla_gb, b_gla_gb,
              gla_out_norm, w_out, norm_cross, norm_mem, w_cq, w_ck, w_cv, w_co, norm_ffn,
              w_router_group, b_router_group, w_router_expert, b_router_expert, w_gate, w_up, w_down,
              norm_final):
    h = x
    for l in range(DEPTH):
        h = h + parallel_head_mixer(rmsnorm(h, norm_mix[l]), w_in[l], attn_out_norm[l], sink_logit[l],
                                    w_gla_gf[l], b_gla_gf[l], w_gla_gb[l], b_gla_gb[l], gla_out_norm[l], w_out[l])
        h = h + memory_cross_attention(rmsnorm(h, norm_cross[l]), rmsnorm(mem, norm_mem[l]),
                                       w_cq[l], w_ck[l], w_cv[l], w_co[l])
        h = h + hierarchical_moe(rmsnorm(h, norm_ffn[l]), w_router_group[l], b_router_group[l],
                                 w_router_expert[l], b_router_expert[l], w_gate[l], w_up[l], w_down[l])
    return rmsnorm(h, norm_final)
```

```python
import math
from contextlib import ExitStack

import numpy as np
import concourse.bass as bass
import concourse.mybir as mybir
from concourse.bass_utils import run_bass_kernel_spmd

F32 = mybir.dt.float32
BF16 = mybir.dt.bfloat16
I32 = mybir.dt.int32
AF = mybir.ActivationFunctionType
ALU = mybir.AluOpType
AX = mybir.AxisListType

D = 2048
KC = 16
MEM = 256
IN_COLS = 4640
EPS = 1e-6
NDSEM = 8


class Cfg:
    def __init__(self, T=2048, NE=32, C=256, KPRE=19, K1=19):
        self.KPRE = min(KPRE, NE)
        self.K1 = min(K1, self.KPRE)
        self.T = T
        self.NE = NE
        self.C = C
        self.NG = NE // 8
        self.NT = T // 128
        self.GS = min(512, T)
        self.NTG = T // self.GS


class Sched:
    ENG = ("pe", "act", "dve", "pool", "sp")
    DQ = ("sp", "act", "pool")

    def __init__(self, nc, es):
        self.nc = nc
        self.csem = {e: es.enter_context(nc.semaphore("c_" + e)) for e in self.ENG}
        self.ccnt = {e: 0 for e in self.ENG}
        self.nds = {"sp": 24, "act": 12, "pool": 8}
        self.dsem = {q: [es.enter_context(nc.semaphore("d_%s_%d" % (q, i))) for i in range(self.nds[q])]
                     for q in self.DQ}
        self.dcnt = {q: 0 for q in self.DQ}
        self.bgsem = [es.enter_context(nc.semaphore("bg_%d" % i)) for i in range(NDSEM)]
        self.bgcnt = 0
        self.bgw = {}
        self.prog = {e: [] for e in self.ENG}
        self.seen = {e: {} for e in self.ENG}
        self.lastw = {}
        self.readers = {}
        self.multi = {}
        self.genw = {}

    def _need(self, eng, toks, is_pe):
        waits = {}
        for t in toks:
            if t is None:
                continue
            sem, val, kind, src = t
            if is_pe and kind == "c" and src == "pe":
                continue
            k = id(sem)
            if self.seen[eng].get(k, 0) >= val:
                continue
            if k not in waits or waits[k][1] < val:
                waits[k] = (sem, val)
        for k, (sem, val) in waits.items():
            self.seen[eng][k] = val
        return list(waits.values())

    def op(self, eng, fn, R=(), W=(), dma=False, bg=False, Wm=()):
        deps = []
        for r in R:
            deps.append(self.lastw.get(r))
            deps.append(self.bgw.get(r))
            deps.extend(self.multi.get(r, ()))
        for w in W:
            deps.append(self.lastw.get(w))
            deps.extend(self.multi.get(w, ()))
            deps.extend(self.readers.get(w, ()))
        for w in Wm:
            rd = self.readers.get(w, ())
            if rd or self.lastw.get(w) is not None:
                self.genw[w] = list(rd) + [self.lastw.get(w)] + list(self.multi.get(w, ()))
                self.multi[w] = []
                self.lastw[w] = None
                self.readers[w] = []
            deps.extend(self.genw.get(w, ()))
        if bg:
            n = self.bgcnt
            self.bgcnt += 1
            slot = n % NDSEM
            sem = self.bgsem[slot]
            if n >= NDSEM:
                deps.append((sem, 16 * (n // NDSEM), "b", eng))
            tok = (sem, 16 * (n // NDSEM + 1), "b", eng)
            inc = (sem, 16)
            waits = self._need(eng, deps, False)
            self.prog[eng].append((waits, fn, inc))
            for w in W:
                self.bgw[w] = tok
            return tok
        if dma:
            n = self.dcnt[eng]
            self.dcnt[eng] += 1
            nd = self.nds[eng]
            slot = n % nd
            sem = self.dsem[eng][slot]
            if n >= nd:
                deps.append((sem, 16 * (n // nd), "d", eng))
            tok = (sem, 16 * (n // nd + 1), "d", eng)
            inc = (sem, 16)
        else:
            self.ccnt[eng] += 1
            tok = (self.csem[eng], self.ccnt[eng], "c", eng)
            inc = (self.csem[eng], 1)
        waits = self._need(eng, deps, eng == "pe" and not dma)
        self.prog[eng].append((waits, fn, inc))
        for r in R:
            self.readers.setdefault(r, []).append(tok)
        for w in W:
            self.lastw[w] = tok
            self.readers[w] = []
            self.multi[w] = []
            self.genw[w] = []
        for w in Wm:
            self.multi.setdefault(w, []).append(tok)
        return tok

    def barrier(self, final=False):
        self.marks = getattr(self, "marks", [])
        self.marks.append(dict(self.ccnt))
        toks = []
        if final:
            n = self.bgcnt
            for slot in range(NDSEM):
                k = (n - slot + NDSEM - 1) // NDSEM if n > slot else 0
                if k > 0:
                    toks.append((self.bgsem[slot], 16 * k, "b", "pool"))
        for e in self.ENG:
            if self.ccnt[e] > 0:
                toks.append((self.csem[e], self.ccnt[e], "c", e))
        for q in self.DQ:
            n = self.dcnt[q]
            nd = self.nds[q]
            for slot in range(nd):
                k = (n - slot + nd - 1) // nd if n > slot else 0
                if k > 0:
                    toks.append((self.dsem[q][slot], 16 * k, "d", q))
        for e in self.ENG:
            waits = self._need(e, toks, False)
            if waits:
                self.prog[e].append((waits, None, None))
        self.lastw.clear()
        self.readers.clear()
        self.multi.clear()
        self.genw.clear()

    def emit(self):
        prog = self.prog

        def run(name, eng):
            for waits, fn, inc in prog[name]:
                for sem, val in waits:
                    eng.wait_ge(sem, val)
                if fn is None:
                    continue
                fn(eng).then_inc(inc[0], inc[1])

        with self.nc.Block() as block:
            @block.tensor
            def _(e):
                run("pe", e)

            @block.scalar
            def _(e):
                run("act", e)

            @block.vector
            def _(e):
                run("dve", e)

            @block.gpsimd
            def _(e):
                run("pool", e)

            @block.sync
            def _(e):
                run("sp", e)


class Arena:
    def __init__(self, nc, es, nbytes):
        self.n = nbytes // 2
        self.t = es.enter_context(nc.sbuf_tensor("arena", [128, self.n], BF16))
        self.off = 0
        self.peak = 0

    def mark(self):
        return self.off

    def release(self, m):
        self.off = m

    def alloc(self, shape, dtype, parts=128):
        esz = 2 if dtype == BF16 else 4
        n = 1
        for s in shape:
            n *= s
        nb = (n * esz + 63) // 64 * 64
        a = self.off
        self.off += nb // 2
        assert self.off <= self.n, "SBUF arena overflow: %d > %d" % (self.off * 2, self.n * 2)
        self.peak = max(self.peak, self.off)
        v = self.t[0:parts, a:a + n * esz // 2]
        if dtype != BF16:
            v = v.bitcast(dtype)
        if len(shape) == 2:
            v = v.rearrange("p (a b) -> p a b", b=shape[1])
        elif len(shape) == 3:
            v = v.rearrange("p (a b c) -> p a b c", b=shape[1], c=shape[2])
        return v


class Ring:
    def __init__(self, A, name, n, shape, dtype, parts=128):
        self.bufs = [A.alloc(shape, dtype, parts) for _ in range(n)]
        self.name = name
        self.i = 0

    def next(self):
        k = self.i % len(self.bufs)
        self.i += 1
        return self.bufs[k], (self.name, k)


def build(cfg, debug=False):
    T, NE, C, NG, NT, GS, NTG = cfg.T, cfg.NE, cfg.C, cfg.NG, cfg.NT, cfg.GS, cfg.NTG
    NR = NG + NE
    NSLOT = NE * C
    nc = bass.Bass("TRN2", target_bir_lowering=False)

    def din(name, shape, dt=F32):
        return nc.dram_tensor(name, list(shape), dt, kind="ExternalInput").ap()

    skind = "ExternalOutput" if debug else "Internal"

    def dscr(name, shape, dt):
        return nc.dram_tensor(name, list(shape), dt, kind=skind).ap()

    x_own = din("x_own", [T, D])
    x_oth = din("x_oth", [T, D])
    mem = din("mem", [MEM, D])
    g_mix = din("g_mix", [1, D])
    g_cross = din("g_cross", [1, D])
    g_mem = din("g_mem", [1, D])
    g_ffn = din("g_ffn", [1, D])
    g_final = din("g_final", [1, D])
    w_in = din("w_in", [D, IN_COLS])
    ga_col = din("ga_col", [128, 8])
    sink = din("sink", [1, 8])
    wgP = din("wgP", [17, 512])
    wgM = din("wgM", [17, 512])
    gg_col = din("gg_col", [128, 2])
    w_out = din("w_out", [D, D])
    w_cq = din("w_cq", [D, D])
    w_ck = din("w_ck", [D, D])
    w_cv = din("w_cv", [D, D])
    w_co = din("w_co", [D, D])
    w_r = din("w_r", [D, NR])
    b_r = din("b_r", [1, NR])
    w_gate = din("w_gate", [NE, D, 1024])
    w_up = din("w_up", [NE, D, 1024])
    w_down = din("w_down", [NE, 1024, D])
    c_ident = din("c_ident", [128, 128])
    c_tri = din("c_tri", [128, 4, 128])
    c_amask = din("c_amask", [128, 3 * 8 * 128])
    c_ec = din("c_ec", [1, NE])

    out = nc.dram_tensor("out", [T, D], F32, kind="ExternalOutput").ap()

    kaT_d = dscr("kaT_d", [2, 128, T + 256], BF16)
    va_d = dscr("va_d", [T + 256, 256], BF16)
    qaT_d = dscr("qaT_d", [8, 128, T], BF16)
    qgT_d = dscr("qgT_d", [4, 128, T], BF16)
    kgT_d = dscr("kgT_d", [4, 128, T], BF16)
    kg_d = dscr("kg_d", [T, 512], BF16)
    vg_d = dscr("vg_d", [T, 1024], BF16)
    rgT_d = dscr("rgT_d", [8, 128, T], BF16)
    lP_d = dscr("lP_d", [T, 512], F32)
    lM_d = dscr("lM_d", [T, 512], F32)
    A_d = dscr("A_d", [NT, 128, 16, 128], BF16)
    h_d = dscr("h_d", [T, D], F32)
    Xs_d = dscr("Xs_d", [NSLOT, D], BF16)
    Hn_d = nc.dram_tensor("Hn_d", [T, D], BF16, kind="Internal").ap()
    Y_d = dscr("Y_d", [NSLOT, D], BF16)
    KPRE = cfg.KPRE
    w_in_b = nc.dram_tensor("w_in_b", [128, KC * IN_COLS], BF16, kind="Internal").ap()
    wbig_b = {}
    for nm in ("w_out", "w_cq", "w_ck", "w_cv", "w_co"):
        wbig_b[nm] = nc.dram_tensor(nm + "_b", [128, KC * D], BF16, kind="Internal").ap()
    wg_b = nc.dram_tensor("wg_b", [max(KPRE, 1), 128, KC * 1024], BF16, kind="Internal").ap()
    wu_b = nc.dram_tensor("wu_b", [max(KPRE, 1), 128, KC * 1024], BF16, kind="Internal").ap()
    wd_b = nc.dram_tensor("wd_b", [max(KPRE, 1), 128, 8 * D], BF16, kind="Internal").ap()
    if debug:
        dbg_S = dscr("dbg_S", [128, 1024], F32)
        dbg_idx = dscr("dbg_idx", [128, NT * 2], F32)
        dbg_w = dscr("dbg_w", [128, NT * 2], F32)

    with ExitStack() as es:
        S = Sched(nc, es)
        A = Arena(nc, es, 206 * 1024)
        psb = [es.enter_context(nc.psum_tensor("ps%d" % i, [128, 512], F32)) for i in range(8)]
        psi = [0]

        def PS():
            k = psi[0] % 8
            psi[0] += 1
            return psb[k], ("ps", k)

        def DMA(q, out_, in_, R=(), W=()):
            return S.op(q, lambda e: e.dma_start(out=out_, in_=in_), R, W, dma=True)

        def MM(out_, lhsT, rhs, start, stop, R, W):
            return S.op("pe", lambda e: e.matmul(out_, lhsT=lhsT, rhs=rhs, start=start, stop=stop), R, W)

        def TR(out_, in_, R, W):
            return S.op("pe", lambda e: e.transpose(out=out_, in_=in_, identity=ident), R, W)

        def ACT(out_, in_, func, R, W, bias=None, scale=None, accum=None, Wm=()):
            kw = {}
            if bias is not None:
                kw["bias"] = bias
            if scale is not None:
                kw["scale"] = scale
            if accum is not None:
                kw["accum_out"] = accum
            return S.op("act", lambda e: e.activation(out=out_, in_=in_, func=func, **kw), R, W, Wm=Wm)

        def TT(eng, out_, in0, in1, op, R, W):
            return S.op(eng, lambda e: e.tensor_tensor(out=out_, in0=in0, in1=in1, op=op), R, W)

        def TS(eng, out_, in0, s1, op0, R, W, s2=None, op1=None, Wm=()):
            if op1 is None:
                return S.op(eng, lambda e: e.tensor_scalar(out=out_, in0=in0, scalar1=s1, scalar2=None, op0=op0), R, W, Wm=Wm)
            return S.op(eng, lambda e: e.tensor_scalar(out=out_, in0=in0, scalar1=s1, scalar2=s2, op0=op0, op1=op1), R, W, Wm=Wm)

        def STT(out_, in0, scalar, in1, op0, op1, R, W):
            return S.op("dve", lambda e: e.scalar_tensor_tensor(out=out_, in0=in0, scalar=scalar, in1=in1, op0=op0, op1=op1), R, W)

        def CP(eng, out_, in_, R, W, Wm=()):
            if eng == "act":
                return ACT(out_, in_, AF.Copy, R, W, Wm=Wm)
            return S.op(eng, lambda e: e.tensor_copy(out=out_, in_=in_), R, W, Wm=Wm)

        def RECIP(out_, in_, R, W):
            return S.op("dve", lambda e: e.reciprocal(out=out_, in_=in_), R, W)

        def RMAX(out_, in_, axis, R, W):
            return S.op("dve", lambda e: e.tensor_reduce(out=out_, in_=in_, axis=axis, op=ALU.max), R, W)

        def RSUM(out_, in_, axis, R, W):
            return S.op("dve", lambda e: e.tensor_reduce(out=out_, in_=in_, axis=axis, op=ALU.add), R, W)

        bcreg = []

        def BC(e):
            if not bcreg:
                bcreg.append(e.to_reg(NSLOT - 1))
            return bcreg[0]

        def MEMSET(eng, ap, val, W):
            return S.op(eng, lambda e: e.memset(ap, val), (), W)

        identf = A.alloc([128], F32)
        ident = A.alloc([128], BF16)
        tri = A.alloc([4, 128], F32)
        ones_bf = A.alloc([128], BF16)
        ones_f = A.alloc([1], F32)
        onesF = A.alloc([128], F32)
        Sst = A.alloc([4, 256], F32)
        Sbf = A.alloc([4, 256], BF16)
        gainb = A.alloc([D], F32)
        idx_all = A.alloc([NT * 2], I32)
        w_all = A.alloc([NT * 2], F32)
        LE, GE, GT, LT = tri[:, 0, :], tri[:, 1, :], tri[:, 2, :], tri[:, 3, :]

        DMA("sp", identf, c_ident, W=["identf"])
        DMA("sp", tri, c_tri, W=["tri"])
        CP("dve", ident, identf, ["identf"], ["ident"])
        MEMSET("dve", ones_bf, 1.0, ["ones_bf"])
        MEMSET("dve", ones_f, 1.0, ["ones_f"])
        MEMSET("dve", onesF, 1.0, ["onesF"])
        MEMSET("dve", Sst, 0.0, ["Sst"])
        MEMSET("dve", Sbf, 0.0, ["Sbf"])

        small = Ring(A, "small", 6, [1], F32)
        junk = A.alloc([D], BF16)

        def load_gain(g_ap):
            DMA("sp", gainb, g_ap.partition_broadcast(128), W=["gainb"])

        def norm_pre(xt, kx, xs_ring):
            ss, kss = small.next()
            rs, krs = small.next()
            rstd, krstd = small.next()
            ACT(junk, xt, AF.Square, [kx], [kss], accum=ss)
            ACT(rs, ss, AF.Sqrt, [kss], [krs], bias=EPS, scale=1.0 / D)
            RECIP(rstd, rs, [krs], [krstd])
            xs, kxs = xs_ring.next()
            STT(xs, xt, rstd, gainb, ALU.mult, ALU.mult, [kx, krstd, "gainb"], [kxs])
            return xs, kxs

        def norm_post(xs, kxs, dstT, col0, kdst):
            for b in range(2):
                pt, kp = PS()
                pb = pt[:, :].bitcast(BF16)
                for j in range(8):
                    kc = b * 8 + j
                    TR(pb[:, j * 128:(j + 1) * 128], xs[:, kc * 128:(kc + 1) * 128], [kxs, "ident"], [kp])
                CP("act" if b == 0 else "dve", dstT[:, b * 8:(b + 1) * 8, col0:col0 + 128],
                   pb.rearrange("p (a b) -> p a b", b=128), [kp], [], Wm=[kdst])

        def norm_T(xt, kx, dstT, col0, kdst, xs_ring, hn_out=None):
            xs, kxs = norm_pre(xt, kx, xs_ring)
            norm_post(xs, kxs, dstT, col0, kdst)
            return xs, kxs

        def load_w(q, dst, src2d, c0, ncol, kdst):
            DMA(q, dst, src2d[:, c0:c0 + ncol].rearrange("(k p) n -> p k n", p=128), W=[kdst])

        def BGDMA(out_, in_, W):
            return S.op("pool", lambda e: e.dma_start(out=out_, in_=in_), (), W, bg=True)

        wq = [0]

        def load_wb(dst, wb, ncols_total, c0, ncol, kdst, kpre):
            q = "sp"
            DMA(q, dst, wb.rearrange("p (k n) -> p k n", n=ncols_total)[:, :, c0:c0 + ncol], R=[kpre], W=[kdst])

        def emit_precast_experts(e0, e1):
            for e_ in range(e0, min(e1, KPRE)):
                BGDMA(wg_b[e_].rearrange("p (k n) -> p k n", n=1024), w_gate[e_].rearrange("(k p) n -> p k n", p=128),
                      [("pre_g", e_)])
                BGDMA(wu_b[e_].rearrange("p (k n) -> p k n", n=1024), w_up[e_].rearrange("(k p) n -> p k n", p=128),
                      [("pre_u", e_)])
                BGDMA(wd_b[e_].rearrange("p (k n) -> p k n", n=D), w_down[e_].rearrange("(k p) n -> p k n", p=128),
                      [("pre_d", e_)])

        def emit_precast():
            for blk in range(0, IN_COLS, 1024):
                n = min(1024, IN_COLS - blk)
                BGDMA(w_in_b.rearrange("p (k n) -> p k n", n=IN_COLS)[:, :, blk:blk + n],
                      w_in[:, blk:blk + n].rearrange("(k p) n -> p k n", p=128), [("pre_w_in", blk // 1024)])
            emit_precast_experts(0, cfg.K1)

        m_base = A.mark()

        zt = A.alloc([D], BF16)
        MEMSET("pool", zt, 0.0, ["zt"])
        for r0 in range(0, NSLOT, 128):
            DMA("act", Xs_d[r0:r0 + 128, :], zt, R=["zt"], W=[])

        Wkg = A.alloc([KC, 512], BF16)
        Wvg = A.alloc([KC, 1024], BF16)
        Wlr = A.alloc([KC, 32], BF16)
        Wkva = A.alloc([KC, 512], BF16)
        wgP_sb = A.alloc([512], F32, parts=17)
        wgM_sb = A.alloc([512], F32, parts=17)
        load_w("pool", Wkg, w_in, 2048, 512, "Wkg")
        load_w("pool", Wvg[:, :, 0:512], w_in, 2560, 512, "Wvg0")
        load_w("pool", Wvg[:, :, 512:1024], w_in, 3072, 512, "Wvg1")
        load_w("pool", Wlr, w_in, 4608, 32, "Wlr")
        load_w("pool", Wkva, w_in, 1024, 512, "Wkva")
        DMA("sp", wgP_sb, wgP, W=["wgP"])
        DMA("sp", wgM_sb, wgM, W=["wgM"])
        load_gain(g_mix)
        emit_precast()

        xt_ring = Ring(A, "xt", 3, [D], F32)
        xs_ring = Ring(A, "xs", 2, [D], BF16)
        uTt_ring = Ring(A, "uTt", 3, [KC, 128], BF16)
        lrT_ring = Ring(A, "lrT", 3, [128], F32, parts=17)
        for b_, k_ in zip(lrT_ring.bufs, range(3)):
            MEMSET("dve", b_, 1.0, [("lrT", k_)])
        v_ring = Ring(A, "vsb", 3, [1024], BF16)
        ks_ring = Ring(A, "ksb", 3, [512], BF16)
        e1_ring = Ring(A, "e1", 2, [512], F32)
        l_ring = Ring(A, "l", 2, [512], F32)
        eK_ring = Ring(A, "eK", 2, [512], F32)
        kd_ring = Ring(A, "kd", 2, [512], BF16)
        dec_ring = Ring(A, "dec", 2, [4], F32)
        st_ring = Ring(A, "stg", 2, [512], BF16)

        def gate_l(lrT, klrT, wg_sb, kwg, l, kl):
            pz, kpz = PS()
            MM(pz[:, :], lrT, wg_sb, True, True, [klrT, kwg], [kpz])
            e1, ke1 = e1_ring.next()
            ACT(e1, pz[:, :], AF.Exp, [kpz], [ke1], scale=-1.0)
            ACT(l, e1, AF.Ln, [ke1], [kl], bias=1.0)

        p1 = {}

        def p1_N(i):
            xt, kx = xt_ring.next()
            DMA("sp", xt, x_oth[i * 128:(i + 1) * 128, :], W=[kx])
            uTt, ku = uTt_ring.next()
            norm_T(xt, kx, uTt, 0, ku, xs_ring)
            p1[i] = dict(uTt=uTt, ku=ku)

        def p1_Ak(i):
            c = p1[i]
            uTt, ku = c["uTt"], c["ku"]
            pk, kpk = PS()
            for kc in range(KC):
                MM(pk[:, :], uTt[:, kc, :], Wkg[:, kc, :], kc == 0, kc == KC - 1, [ku, "Wkg"], [kpk])
            ksb, kks = ks_ring.next()
            CP("dve", ksb, pk[:, :], [kpk], [kks])
            c.update(ksb=ksb, kks=kks)
            c["vsb"], c["kv"] = v_ring.next()

        def p1_Av(i, hf):
            c = p1[i]
            uTt, ku, vsb, kv = c["uTt"], c["ku"], c["vsb"], c["kv"]
            pv, kpv = PS()
            for kc in range(KC):
                MM(pv[:, :], uTt[:, kc, :], Wvg[:, kc, hf * 512:(hf + 1) * 512], kc == 0, kc == KC - 1,
                   [ku, "Wvg%d" % hf], [kpv])
            CP("act", vsb[:, hf * 512:(hf + 1) * 512], pv[:, :], [kpv], [(kv, hf)])

        def p1_Alr(i):
            c = p1[i]
            uTt, ku = c["uTt"], c["ku"]
            pl, kpl = PS()
            for kc in range(KC):
                MM(pl[0:16, 0:128], Wlr[:, kc, 0:16], uTt[:, kc, :], kc == 0, kc == KC - 1, [ku, "Wlr"], [kpl])
            lrT, klrT = lrT_ring.next()
            CP("dve", lrT[0:16, :], pl[0:16, 0:128], [kpl], [klrT])
            c.update(lrT=lrT, klrT=klrT)
            if i == NT - 1:
                for g in range(2):
                    pa, kpa = PS()
                    for kc in range(KC):
                        MM(pa[:, 0:128], Wkva[:, kc, g * 128:(g + 1) * 128], uTt[:, kc, :], kc == 0, kc == KC - 1,
                           [ku, "Wkva"], [kpa])
                    st, kst = st_ring.next()
                    CP("act", st[:, 0:128], pa[:, 0:128], [kpa], [kst])
                    DMA("act", kaT_d[g, :, 0:128], st[:, 0:128], R=[kst], W=[])
                pa, kpa = PS()
                for kc in range(KC):
                    MM(pa[:, 0:256], uTt[:, kc, :], Wkva[:, kc, 256:512], kc == 0, kc == KC - 1, [ku, "Wkva"], [kpa])
                st, kst = st_ring.next()
                CP("act", st[:, 0:256], pa[:, 0:256], [kpa], [kst])
                DMA("act", va_d[0:128, :], st[:, 0:256], R=[kst], W=[])

        def p1_A(i):
            p1_Ak(i)
            p1_Av(i, 0)
            p1_Av(i, 1)
            p1_Alr(i)

        def p1_B1(i):
            c = p1[i]
            c["l"], c["kl"] = l_ring.next()
            gate_l(c["lrT"], c["klrT"], wgP_sb, "wgP", c["l"], c["kl"])

        def p1_B2(i):
            c = p1[i]
            l, kl = c["l"], c["kl"]
            psu, kpsu = PS()
            MM(psu[:, :], GT, l, True, True, ["tri", kl], [kpsu])
            eK, keK = eK_ring.next()
            ACT(eK, psu[:, :], AF.Exp, [kpsu], [keK], scale=-1.0 / 16)
            kd, kkd = kd_ring.next()
            TT("dve", kd, c["ksb"], eK, ALU.mult, [c["kks"], keK], [kkd])
            ptot, kpt = PS()
            for h in range(4):
                MM(ptot[:, h:h + 1], l[:, h * 128:(h + 1) * 128], ones_f, True, True, [kl, "ones_f"], [kpt])
            dec, kdec = dec_ring.next()
            ACT(dec, ptot[:, 0:4], AF.Exp, [kpt], [kdec], scale=-1.0 / 16)
            c.update(kd=kd, kkd=kkd, dec=dec, kdec=kdec)

        def p1_B3(i, hs):
            c = p1[i]
            kd, kkd, dec, kdec, vsb, kv = (c[k] for k in ("kd", "kkd", "dec", "kdec", "vsb", "kv"))
            for h in hs:
                pkv, kpkv = PS()
                MM(pkv[:, 0:256], kd[:, h * 128:(h + 1) * 128], vsb[:, h * 256:(h + 1) * 256], True, True,
                   [kkd, (kv, 0), (kv, 1)], [kpkv])
                STT(Sst[:, h, :], Sst[:, h, :], dec[:, h:h + 1], pkv[:, 0:256], ALU.mult, ALU.add,
                    [("Sst", h), kdec, kpkv], [("Sst", h)])

        p1_N(0)
        if NT > 1:
            p1_N(1)
        p1_A(0)
        for i in range(NT):
            nx = i + 1 < NT
            p1_B1(i)
            if nx:
                p1_Ak(i + 1)
            p1_B2(i)
            if nx:
                p1_Av(i + 1, 0)
            p1_B3(i, (0, 1))
            if nx:
                p1_Av(i + 1, 1)
            p1_B3(i, (2, 3))
            if nx:
                p1_Alr(i + 1)
            if i + 2 < NT:
                p1_N(i + 2)
            p1.pop(i)
        for h in range(4):
            CP("act", Sbf[:, h, :], Sst[:, h, :], [("Sst", h)], [("Sbf", h)])
        if debug:
            DMA("sp", dbg_S, Sst.rearrange("p a b -> p (a b)"), R=[("Sst", h) for h in range(4)])
        S.barrier()
        A.release(m_base)

        uT = A.alloc([KC, T], BF16)
        wgP_sb = A.alloc([512], F32, parts=17)
        wgM_sb = A.alloc([512], F32, parts=17)
        DMA("sp", wgP_sb, wgP, W=["wgP"])
        DMA("sp", wgM_sb, wgM, W=["wgM"])
        W_ring = Ring(A, "W", 3, [KC, 512], BF16)
        m2 = A.mark()
        xt_ring = Ring(A, "xt", 2, [D], F32)
        xs_ring = Ring(A, "xs", 2, [D], BF16)
        for i in range(NT):
            xt, kx = xt_ring.next()
            DMA("sp", xt, x_own[i * 128:(i + 1) * 128, :], W=[kx])
            norm_T(xt, kx, uT, i * 128, ("uT", i), xs_ring)
        S.barrier()
        A.release(m2)
        fst_ring = Ring(A, "fst", 3, [T], BF16)
        tst_ring = Ring(A, "tst", 2, [4, 512], BF16)
        lb_ring = Ring(A, "lb", 2, [4, 512], F32)
        e1b_ring = Ring(A, "e1b", 1, [4, 512], F32)
        lrTP = A.alloc([T], F32, parts=17)
        lrTM = A.alloc([T], F32, parts=17)
        MEMSET("dve", lrTP, 1.0, ["lrTP"])
        MEMSET("dve", lrTM, 1.0, ["lrTM"])
        uT_all = [("uT", i) for i in range(NT)]
        evq = [0]

        def ev_eng():
            evq[0] += 1
            return "act" if evq[0] % 2 else "dve"

        def proj_fm(Wt, kW, jl, dst_row, func=None, scale=None, c0=0):
            st, kst = fst_ring.next()
            for tg in range(NTG):
                p, kp = PS()
                for kc in range(KC):
                    MM(p[:, 0:GS], Wt[:, kc, jl * 128:(jl + 1) * 128], uT[:, kc, tg * GS:(tg + 1) * GS],
                       kc == 0, kc == KC - 1, [kW] + uT_all[tg * GS // 128:(tg + 1) * GS // 128], [kp])
                if func is not None:
                    ACT(st[:, tg * GS:(tg + 1) * GS], p[:, 0:GS], func, [kp], [], Wm=[kst])
                elif scale is not None:
                    e = ev_eng()
                    if e == "act":
                        ACT(st[:, tg * GS:(tg + 1) * GS], p[:, 0:GS], AF.Copy, [kp], [], scale=scale, Wm=[kst])
                    else:
                        TS("dve", st[:, tg * GS:(tg + 1) * GS], p[:, 0:GS], scale, ALU.mult, [kp], [], Wm=[kst])
                else:
                    CP(ev_eng(), st[:, tg * GS:(tg + 1) * GS], p[:, 0:GS], [kp], [], Wm=[kst])
            DMA("act", dst_row[:, c0:c0 + T], st, R=[kst], W=[])

        def proj_tm(Wt, kW, cl0, ncol, dst, dc0, r0=0):
            for i4 in range(0, NT, 4):
                n4 = min(4, NT - i4)
                st, kst = tst_ring.next()
                for ii in range(n4):
                    i = i4 + ii
                    p, kp = PS()
                    for kc in range(KC):
                        MM(p[:, 0:ncol], uT[:, kc, i * 128:(i + 1) * 128], Wt[:, kc, cl0:cl0 + ncol],
                           kc == 0, kc == KC - 1, [kW, ("uT", i)], [kp])
                    CP(ev_eng(), st[:, ii, 0:ncol], p[:, 0:ncol], [kp], [], Wm=[kst])
                DMA("act", dst[r0 + i4 * 128:r0 + (i4 + n4) * 128, dc0:dc0 + ncol].rearrange("(a p) c -> p a c", p=128),
                    st[:, 0:n4, 0:ncol], R=[kst], W=[])

        sc_a = 1.0 / math.sqrt(128.0)
        for blk in range(2):
            Wt, kW = W_ring.next()
            load_wb(Wt, w_in_b, IN_COLS, blk * 512, 512, kW, ("pre_w_in", (blk * 512) // 1024))
            for jl in range(4):
                proj_fm(Wt, kW, jl, qaT_d[blk * 4 + jl], scale=sc_a)
        Wt, kW = W_ring.next()
        load_wb(Wt, w_in_b, IN_COLS, 1024, 512, kW, ("pre_w_in", (1024) // 1024))
        for g in range(2):
            proj_fm(Wt, kW, g, kaT_d[g], c0=128)
        proj_tm(Wt, kW, 256, 256, va_d, 0, r0=128)
        Wt, kW = W_ring.next()
        load_wb(Wt, w_in_b, IN_COLS, 1536, 512, kW, ("pre_w_in", (1536) // 1024))
        for jl in range(4):
            proj_fm(Wt, kW, jl, qgT_d[jl], scale=sc_a)
        Wt, kW = W_ring.next()
        load_wb(Wt, w_in_b, IN_COLS, 2048, 512, kW, ("pre_w_in", (2048) // 1024))
        for jl in range(4):
            proj_fm(Wt, kW, jl, kgT_d[jl])
        proj_tm(Wt, kW, 0, 512, kg_d, 0)
        for blk in range(2):
            Wt, kW = W_ring.next()
            load_wb(Wt, w_in_b, IN_COLS, 2560 + blk * 512, 512, kW, ("pre_w_in", (2560 + blk * 512) // 1024))
            proj_tm(Wt, kW, 0, 512, vg_d, blk * 512)
        for blk in range(2):
            Wt, kW = W_ring.next()
            load_wb(Wt, w_in_b, IN_COLS, 3584 + blk * 512, 512, kW, ("pre_w_in", (3584 + blk * 512) // 1024))
            for jl in range(4):
                proj_fm(Wt, kW, jl, rgT_d[blk * 4 + jl], func=AF.Silu)
        Wt, kW = W_ring.next()
        load_wb(Wt[:, :, 0:32], w_in_b, IN_COLS, 4608, 32, kW, ("pre_w_in", (4608) // 1024))
        for dr, lrT in ((0, lrTP), (1, lrTM)):
            for tg in range(NTG):
                p, kp = PS()
                for kc in range(KC):
                    MM(p[0:16, 0:GS], Wt[:, kc, dr * 16:(dr + 1) * 16], uT[:, kc, tg * GS:(tg + 1) * GS],
                       kc == 0, kc == KC - 1, [kW] + uT_all[tg * GS // 128:(tg + 1) * GS // 128], [kp])
                CP("dve", lrT[0:16, tg * GS:(tg + 1) * GS], p[0:16, 0:GS], [kp], ["lrTP" if dr == 0 else "lrTM"])
        for dr, lrT, wg_sb, kwg, l_d in ((0, lrTP, wgP_sb, "wgP", lP_d), (1, lrTM, wgM_sb, "wgM", lM_d)):
            klr = "lrTP" if dr == 0 else "lrTM"
            for i4 in range(0, NT, 4):
                n4 = min(4, NT - i4)
                e1b, ke1b = e1b_ring.next()
                lb, klb = lb_ring.next()
                for ii in range(n4):
                    i = i4 + ii
                    pz, kpz = PS()
                    MM(pz[:, :], lrT[:, i * 128:(i + 1) * 128], wg_sb, True, True, [klr, kwg], [kpz])
                    ACT(e1b[:, ii, :], pz[:, :], AF.Exp, [kpz], [(ke1b, ii)], scale=-1.0)
                ACT(lb[:, 0:n4, :], e1b[:, 0:n4, :], AF.Ln, [(ke1b, ii) for ii in range(n4)], [klb], bias=1.0)
                DMA("sp", l_d[i4 * 128:(i4 + n4) * 128, :].rearrange("(a p) c -> p a c", p=128), lb[:, 0:n4, :],
                    R=[klb], W=[])
        S.barrier()
        A.release(m_base)

        amaskf = A.alloc([3, 8, 128], F32)
        amask = A.alloc([3, 8, 128], BF16)
        esink = A.alloc([8], F32)
        gacol = A.alloc([8], F32)
        DMA("sp", amaskf.rearrange("p a b c -> p (a b c)"), c_amask, W=["amaskf"])
        CP("dve", amask, amaskf, ["amaskf"], ["amask"])
        DMA("sp", esink, sink.partition_broadcast(128), W=["esink0"])
        DMA("sp", gacol, ga_col, W=["gacol"])
        ACT(esink, esink, AF.Exp, ["esink0"], ["esink"])
        NQG = GS // 128
        qa_ring = Ring(A, "qa", 2, [8, GS], BF16)
        ka_ring = Ring(A, "ka", 4, [2, 384], BF16)
        va_ring = Ring(A, "va", 5, [3, 256], BF16)
        E_ring = Ring(A, "E", 4, [512], F32)
        PT_ring = Ring(A, "PT", 20, [512], BF16)
        den_ring = Ring(A, "den", 2, [512], F32)
        rden_ring = Ring(A, "rden", 2, [512], F32)
        osb_ring = Ring(A, "osb", 3, [8, 128], F32)
        sq_ring = Ring(A, "sq", 2, [8, 128], BF16)
        rs_ring = Ring(A, "rs", 2, [128], F32)
        rstd_ring = Ring(A, "rstd", 2, [128], F32)
        oast_ring = Ring(A, "oast", 3, [8, 128], BF16)
        p3 = {}
        qa_cur = [None]

        def p3_S1(n):
            npos = 3 if n < NT - 1 else 2
            if n % NQG == 0:
                qa_cur[0] = qa_ring.next()
                t0 = n * 128
                DMA("sp", qa_cur[0][0], qaT_d[:, :, t0:t0 + GS].rearrange("h d t -> d h t"), R=[], W=[qa_cur[0][1]])
            qa, kqa = qa_cur[0]
            q0 = (n % NQG) * 128
            ka, kka = ka_ring.next()
            va, kva = va_ring.next()
            DMA("sp", ka[:, :, 0:npos * 128], kaT_d[:, :, n * 128:n * 128 + npos * 128].rearrange("g d t -> d g t"),
                R=[], W=[kka])
            DMA("sp", va[:, 0:npos, :], va_d[n * 128:n * 128 + npos * 128, :].rearrange("(b p) c -> p b c", p=128),
                R=[], W=[kva])
            PTs = {}
            for g in range(2):
                for pos in range(npos):
                    p, kp = PS()
                    MM(p[:, :].rearrange("p (h t) -> p h t", t=128), ka[:, g, pos * 128:(pos + 1) * 128],
                       qa[:, g * 4:(g + 1) * 4, q0:q0 + 128], True, False, [kka, kqa], [kp])
                    MM(p[:, :], ident, amask[:, pos, g * 4:(g + 1) * 4, :].rearrange("p h t -> p (h t)"), False, True,
                       ["ident", "amask"], [kp])
                    PT, kPT = PT_ring.next()
                    ACT(PT, p[:, :], AF.Exp, [kp], [kPT])
                    PTs[(g, pos)] = (PT, kPT)
            p3[n] = dict(npos=npos, va=va, kva=kva, PTs=PTs)

        def p3_S2(n):
            c = p3[n]
            npos, va, kva, PTs = c["npos"], c["va"], c["kva"], c["PTs"]
            osb, kosb = osb_ring.next()
            for g in range(2):
                po, kpo = PS()
                for pos in range(npos):
                    MM(po[:, :], va[:, pos, g * 128:(g + 1) * 128], PTs[(g, pos)][0], pos == 0, pos == npos - 1,
                       [kva, PTs[(g, pos)][1]], [kpo])
                pd, kpd = PS()
                for pos in range(npos):
                    MM(pd[:, :], ones_bf, PTs[(g, pos)][0], pos == 0, pos == npos - 1,
                       ["ones_bf", PTs[(g, pos)][1]], [kpd])
                den, kden = den_ring.next()
                TT("dve", den.rearrange("p (h t) -> p h t", t=128), pd[:, :].rearrange("p (h t) -> p h t", t=128),
                   esink[:, g * 4:(g + 1) * 4].unsqueeze(2).to_broadcast([128, 4, 128]), ALU.add,
                   [kpd, "esink"], [kden])
                rden, krden = rden_ring.next()
                RECIP(rden, den, [kden], [krden])
                TT("dve", osb[:, g * 4:(g + 1) * 4, :].rearrange("p h t -> p (h t)"), po[:, :], rden, ALU.mult,
                   [kpo, krden], [(kosb, g)])
            c.update(osb=osb, kosb=kosb)

        def p3_S3(n):
            c = p3.pop(n)
            osb, kosb = c["osb"], c["kosb"]
            sq, ksq = sq_ring.next()
            ACT(sq, osb, AF.Square, [(kosb, 0), (kosb, 1)], [ksq])
            pss, kpss = PS()
            for h in range(8):
                MM(pss[:, 0:128], ones_bf, sq[:, h, :], h == 0, h == 7, ["ones_bf", ksq], [kpss])
            rs, krs = rs_ring.next()
            ACT(rs, pss[:, 0:128], AF.Sqrt, [kpss], [krs], bias=EPS, scale=1.0 / 1024)
            rstd, krstd = rstd_ring.next()
            RECIP(rstd, rs, [krs], [krstd])
            oast, koast = oast_ring.next()
            for h in range(8):
                STT(oast[:, h, :], osb[:, h, :], gacol[:, h:h + 1], rstd, ALU.mult, ALU.mult,
                    [(kosb, 0), (kosb, 1), "gacol", krstd], [(koast, h)])
            DMA("act", A_d[n, :, 0:8, :], oast, R=[(koast, h) for h in range(8)], W=[])

        for step in range(NT + 2):
            if step < NT:
                p3_S1(step)
            if 0 <= step - 1 < NT:
                p3_S2(step - 1)
            if 0 <= step - 2 < NT:
                p3_S3(step - 2)
        S.barrier()
        A.release(m_base)

        oP = A.alloc([NT, 8, 128], F32)
        ggcol = A.alloc([2], F32)
        DMA("sp", ggcol, gg_col, W=["ggcol"])
        qg_ring = Ring(A, "qg", 3, [4, 128], BF16)
        kgT_ring = Ring(A, "kgT", 3, [4, 128], BF16)
        kg_ring = Ring(A, "kg", 3, [512], BF16)
        vg_ring = Ring(A, "vg", 3, [1024], BF16)
        lg_ring = Ring(A, "lg", 3, [512], F32)
        rg_ring = Ring(A, "rg", 2, [8, 128], BF16)
        eK_ring = Ring(A, "eK", 2, [512], F32)
        kd_ring = Ring(A, "kd", 3, [512], BF16)
        eP_ring = Ring(A, "eP", 3, [512], F32)
        eN_ring = Ring(A, "eN", 2, [512], F32)
        qd_ring = Ring(A, "qd", 3, [4, 128], BF16)
        ki_ring = Ring(A, "ki", 2, [4, 128], BF16)
        at_ring = Ring(A, "at", 3, [4, 128], BF16)
        og_ring = Ring(A, "og", 2, [8, 128], F32)
        sqg_ring = Ring(A, "sqg", 2, [8, 128], BF16)
        rsg_ring = Ring(A, "rsg", 2, [512], F32)
        rstdg_ring = Ring(A, "rstdg", 2, [512], F32)
        tmpg_ring = Ring(A, "tmpg", 2, [8, 128], F32)
        ogst_ring = Ring(A, "ogst", 3, [8, 128], BF16)
        Sk = [("Sst", h) for h in range(4)]
        Sbk = [("Sbf", h) for h in range(4)]
        Sbf2 = [Sbf, A.alloc([4, 256], BF16)]
        sb_par = [0]

        def gla_front(c, dr):
            l_d = lP_d if dr == 0 else lM_d
            TRI = LE if dr == 0 else GE
            STR = GT if dr == 0 else LT
            tsl = slice(c * 128, (c + 1) * 128)
            qg, kqg = qg_ring.next()
            kgT, kkgT = kgT_ring.next()
            kg, kkg = kg_ring.next()
            vg, kvg = vg_ring.next()
            lg, klg = lg_ring.next()
            DMA("sp", qg, qgT_d[:, :, tsl].rearrange("h d t -> d h t"), R=[], W=[kqg])
            DMA("sp", kgT, kgT_d[:, :, tsl].rearrange("h d t -> d h t"), R=[], W=[kkgT])
            DMA("sp", kg, kg_d[tsl, :], R=[], W=[kkg])
            DMA("sp", vg, vg_d[tsl, :], R=[], W=[kvg])
            DMA("sp", lg, l_d[tsl, :], R=[], W=[klg])
            psu, kpsu = PS()
            MM(psu[:, :], STR, lg, True, True, ["tri", klg], [kpsu])
            pb, kpb = PS()
            for h in range(4):
                MM(pb[:, h * 128:(h + 1) * 128], lg[:, h * 128:(h + 1) * 128], TRI, True, True, ["tri", klg], [kpb])
            eK, keK = eK_ring.next()
            ACT(eK, psu[:, :], AF.Exp, [kpsu], [keK], scale=-1.0 / 16)
            eP, keP = eP_ring.next()
            ACT(eP, pb[:, :], AF.Exp, [kpb], [keP], scale=-1.0 / 16)
            eN, keN = eN_ring.next()
            ACT(eN, pb[:, :], AF.Exp, [kpb], [keN], scale=1.0 / 16)
            kd, kkd = kd_ring.next()
            TT("dve", kd, kg, eK, ALU.mult, [kkg, keK], [kkd])
            qd, kqd = qd_ring.next()
            TT("dve", qd.rearrange("p h t -> p (h t)"), qg.rearrange("p h t -> p (h t)"), eP, ALU.mult,
               [kqg, keP], [kqd])
            ki, kki = ki_ring.next()
            TT("dve", ki.rearrange("p h t -> p (h t)"), kgT.rearrange("p h t -> p (h t)"), eN, ALU.mult,
               [kkgT, keN], [kki])
            pa, kpa = PS()
            for h in range(4):
                MM(pa[:, h * 128:(h + 1) * 128], ki[:, h, :], qd[:, h, :], True, True, [kki, kqd], [kpa])
            at, kat = at_ring.next()
            TT("dve", at, pa[:, :].rearrange("p (h t) -> p h t", t=128),
               TRI.unsqueeze(1).to_broadcast([128, 4, 128]), ALU.mult, [kpa, "tri"], [kat])
            return dict(c=c, dr=dr, vg=vg, kvg=kvg, kd=kd, kkd=kkd, eP=eP, keP=keP, qd=qd, kqd=kqd, at=at, kat=kat)

        def gla_back(f):
            c, dr = f["c"], f["dr"]
            vg, kvg, kd, kkd, eP, keP, qd, kqd, at, kat = (f[k] for k in
                                                         ("vg", "kvg", "kd", "kkd", "eP", "keP", "qd", "kqd", "at", "kat"))
            pos_ = []
            for half in range(2):
                po, kpo = PS()
                pos_.append((po, kpo))
            for h in range(4):
                for dvc in range(2):
                    r = 2 * h + dvc
                    po, kpo = pos_[r // 4]
                    reg = po[:, (r % 4) * 128:(r % 4 + 1) * 128]
                    MM(reg, vg[:, h * 256 + dvc * 128:h * 256 + (dvc + 1) * 128], at[:, h, :], True, False,
                       [kvg, kat], [kpo])
                    MM(reg, Sbf[:, h, dvc * 128:(dvc + 1) * 128], qd[:, h, :], False, True,
                       [("Sbf", h), kqd], [kpo])
            pkvs = []
            for hp in range(2):
                pkv, kpkv = PS()
                pkvs.append((pkv, kpkv))
                for hh in range(2):
                    h = 2 * hp + hh
                    MM(pkv[:, hh * 256:(hh + 1) * 256], kd[:, h * 128:(h + 1) * 128], vg[:, h * 256:(h + 1) * 256],
                       True, True, [kkd, kvg], [kpkv])
            dcol = 127 if dr == 0 else 0
            for h in range(4):
                pkv, kpkv = pkvs[h // 2]
                STT(Sst[:, h, :], Sst[:, h, :], eP[:, h * 128 + dcol:h * 128 + dcol + 1],
                    pkv[:, (h % 2) * 256:(h % 2 + 1) * 256], ALU.mult, ALU.add,
                    [("Sst", h), keP, kpkv], [("Sst", h)])
            CP("act", Sbf.rearrange("p a b -> p (a b)"), Sst.rearrange("p a b -> p (a b)"), Sk, Sbk)
            if dr == 0:
                for half in range(2):
                    po, kpo = pos_[half]
                    CP("act" if half == 0 else "dve", oP[:, c, half * 4:(half + 1) * 4, :].rearrange("p r t -> p (r t)"),
                       po[:, :], [kpo], [], Wm=[("oP", c)])
                return
            og, kog = og_ring.next()
            for half in range(2):
                po, kpo = pos_[half]
                TT("dve", og[:, half * 4:(half + 1) * 4, :].rearrange("p r t -> p (r t)"), po[:, :],
                   oP[:, c, half * 4:(half + 1) * 4, :].rearrange("p r t -> p (r t)"), ALU.add,
                   [kpo, ("oP", c)], [(kog, half)])
            sq, ksq = sqg_ring.next()
            ACT(sq, og, AF.Square, [(kog, 0), (kog, 1)], [ksq])
            pss, kpss = PS()
            for h in range(4):
                for dvc in range(2):
                    MM(pss[:, h * 128:(h + 1) * 128], ones_bf, sq[:, 2 * h + dvc, :], dvc == 0, dvc == 1,
                       ["ones_bf", ksq], [kpss])
            rs, krs = rsg_ring.next()
            ACT(rs, pss[:, :], AF.Sqrt, [kpss], [krs], bias=EPS, scale=1.0 / 256)
            rstd, krstd = rstdg_ring.next()
            RECIP(rstd, rs, [krs], [krstd])
            rg, krg = rg_ring.next()
            DMA("sp", rg, rgT_d[:, :, c * 128:(c + 1) * 128].rearrange("j d t -> d j t"), R=[], W=[krg])
            tmp, ktmp = tmpg_ring.next()
            for h in range(4):
                for dvc in range(2):
                    r = 2 * h + dvc
                    STT(tmp[:, r, :], og[:, r, :], ggcol[:, dvc:dvc + 1], rstd[:, h * 128:(h + 1) * 128],
                        ALU.mult, ALU.mult, [(kog, 0), (kog, 1), "ggcol", krstd], [(ktmp, r)])
            ogst, kogst = ogst_ring.next()
            TT("dve", ogst, tmp, rg, ALU.mult, [(ktmp, r) for r in range(8)] + [krg], [kogst])
            DMA("act", A_d[c, :, 8:16, :], ogst, R=[kogst], W=[])

        for dr in range(2):
            order = list(range(NT)) if dr == 0 else list(range(NT - 1, -1, -1))
            if dr == 1:
                MEMSET("dve", Sst, 0.0, Sk)
                MEMSET("dve", Sbf, 0.0, Sbk)
            ogst_box = [None, None]
            fr = gla_front(order[0], dr)
            for ix, c in enumerate(order):
                nxt = gla_front(order[ix + 1], dr) if ix + 1 < NT else None
                fr["ogst"] = ogst_box
                fr["first_in_group"] = (ix % NQG == 0)
                fr["last_in_group"] = (ix % NQG == NQG - 1)
                gla_back(fr)
                fr = nxt
        S.barrier()
        A.release(m_base)

        emit_precast_experts(cfg.K1, KPRE)
        Wbig = A.alloc([KC, D], BF16)
        U = A.alloc([KC, T], BF16)
        m5 = A.mark()
        for blk in range(4):
            load_w("pool", Wbig[:, :, blk * 512:(blk + 1) * 512], w_out, blk * 512, 512, ("Wbig", blk))
        load_gain(g_cross)
        xt_ring = Ring(A, "xt", 3, [D], F32)
        xs_ring = Ring(A, "xs", 2, [D], BF16)
        aT_ring = Ring(A, "aT", 2, [KC, 128], BF16)
        p5 = {}

        def p5_A(i):
            tsl = slice(i * 128, (i + 1) * 128)
            aT, kaT = aT_ring.next()
            DMA("sp", aT, A_d[i], R=[], W=[(kaT, 0), (kaT, 1)])
            xt, kx = xt_ring.next()
            DMA("sp", xt, x_own[tsl, :], W=[kx])
            for blk in range(4):
                p, kp = PS()
                for fc in range(KC):
                    MM(p[:, :], aT[:, fc, :], Wbig[:, fc, blk * 512:(blk + 1) * 512], fc == 0, fc == KC - 1,
                       [(kaT, 0), (kaT, 1), ("Wbig", blk)], [kp])
                TT("dve", xt[:, blk * 512:(blk + 1) * 512], p[:, :], xt[:, blk * 512:(blk + 1) * 512], ALU.add,
                   [kp, kx], [kx])
            DMA("act", h_d[tsl, :], xt, R=[kx], W=[("h_d", i)])
            p5[i] = (xt, kx)

        p5_A(0)
        for i in range(NT):
            xt, kx = p5.pop(i)
            xs, kxs = norm_pre(xt, kx, xs_ring)
            if i + 1 < NT:
                p5_A(i + 1)
            norm_post(xs, kxs, U, i * 128, ("U", i))
        S.barrier()
        A.release(m5)

        Q = Wbig.rearrange("p k n -> p (k n)")[:, 0:KC * T].rearrange("p (k t) -> p k t", t=T)
        memT = A.alloc([KC, MEM], BF16)
        kcT = A.alloc([KC, MEM], BF16)
        vc = A.alloc([2, D], BF16)
        m6 = A.mark()
        load_gain(g_mem)
        xt_ring = Ring(A, "xt", 2, [D], F32)
        xs_ring = Ring(A, "xs", 2, [D], BF16)
        for mt in range(2):
            xt, kx = xt_ring.next()
            DMA("sp", xt, mem[mt * 128:(mt + 1) * 128, :], W=[kx])
            norm_T(xt, kx, memT, mt * 128, ("memT", mt), xs_ring)
        S.barrier()
        A.release(m6)
        W6 = Ring(A, "W6", 3, [KC, 256], BF16)
        mk = [("memT", 0), ("memT", 1)]
        for blk in range(8):
            Wt, kW = W6.next()
            load_w("pool", Wt, w_ck, blk * 256, 256, kW)
            for jl in range(2):
                p, kp = PS()
                for kc in range(KC):
                    MM(p[:, 0:MEM], Wt[:, kc, jl * 128:(jl + 1) * 128], memT[:, kc, :], kc == 0, kc == KC - 1,
                       [kW] + mk, [kp])
                CP(ev_eng(), kcT[:, blk * 2 + jl, :], p[:, 0:MEM], [kp], [("kcT", blk * 2 + jl)])
        for blk in range(8):
            Wt, kW = W6.next()
            load_w("pool", Wt, w_cv, blk * 256, 256, kW)
            for mt in range(2):
                p, kp = PS()
                for kc in range(KC):
                    MM(p[:, 0:256], memT[:, kc, mt * 128:(mt + 1) * 128], Wt[:, kc, :], kc == 0, kc == KC - 1,
                       [kW] + mk, [kp])
                CP(ev_eng(), vc[:, mt, blk * 256:(blk + 1) * 256], p[:, 0:256], [kp], [], Wm=[("vc", blk // 2)])
        sc_x = 1.0 / math.sqrt(512.0)
        U_all = [("U", i) for i in range(NT)]
        for blk in range(8):
            Wt, kW = W6.next()
            load_w("pool", Wt, w_cq, blk * 256, 256, kW)
            for jl in range(2):
                j = blk * 2 + jl
                for tg in range(NTG):
                    p, kp = PS()
                    for kc in range(KC):
                        MM(p[:, 0:GS], Wt[:, kc, jl * 128:(jl + 1) * 128], U[:, kc, tg * GS:(tg + 1) * GS],
                           kc == 0, kc == KC - 1, [kW] + U_all[tg * GS // 128:(tg + 1) * GS // 128], [kp])
                    if ev_eng() == "act":
                        ACT(Q[:, j, tg * GS:(tg + 1) * GS], p[:, 0:GS], AF.Copy, [kp], [("Q", j, tg)], scale=sc_x)
                    else:
                        TS("dve", Q[:, j, tg * GS:(tg + 1) * GS], p[:, 0:GS], sc_x, ALU.mult, [kp], [("Q", j, tg)])
        S.barrier()
        A.release(m6)
        PT_ring = Ring(A, "PTx", 4, [GS], BF16)
        rden_ring = Ring(A, "rdenx", 2, [GS], F32)
        for hd in range(4):
            for tg in range(NTG):
                PTs = []
                for mt in range(2):
                    p, kp = PS()
                    for cch in range(4):
                        j = hd * 4 + cch
                        MM(p[:, 0:GS], kcT[:, j, mt * 128:(mt + 1) * 128], Q[:, j, tg * GS:(tg + 1) * GS],
                           cch == 0, cch == 3, [("kcT", j), ("Q", j, tg)], [kp])
                    PT, kPT = PT_ring.next()
                    ACT(PT, p[:, 0:GS], AF.Exp, [kp], [kPT])
                    PTs.append((PT, kPT))
                pd, kpd = PS()
                for mt in range(2):
                    MM(pd[:, 0:GS], ones_bf, PTs[mt][0], mt == 0, mt == 1, ["ones_bf", PTs[mt][1]], [kpd])
                rden, krden = rden_ring.next()
                RECIP(rden, pd[:, 0:GS], [kpd], [krden])
                for cch in range(4):
                    j = hd * 4 + cch
                    po, kpo = PS()
                    for mt in range(2):
                        MM(po[:, 0:GS], vc[:, mt, j * 128:(j + 1) * 128], PTs[mt][0], mt == 0, mt == 1,
                           [("vc", j // 4), PTs[mt][1]], [kpo])
                    TT("dve", U[:, j, tg * GS:(tg + 1) * GS], po[:, 0:GS], rden, ALU.mult, [kpo, krden],
                       [("oc", j, tg)])
        S.barrier()
        A.release(m5)
        for blk in range(4):
            load_w("pool", Wbig[:, :, blk * 512:(blk + 1) * 512], w_co, blk * 512, 512, ("Wbig", blk))
        load_gain(g_ffn)
        Wr = A.alloc([KC, NR], BF16)
        Wrf = A.alloc([KC, NR], F32)
        DMA("sp", Wrf, w_r.rearrange("(k p) n -> p k n", p=128), W=["Wrf"])
        CP("dve", Wr, Wrf, ["Wrf"], ["Wr"])
        brb = A.alloc([NR], F32)
        DMA("sp", brb, b_r.partition_broadcast(128), W=["brb"])
        ecb = A.alloc([NE], F32)
        DMA("sp", ecb, c_ec.partition_broadcast(128), W=["ecb"])
        L_all = A.alloc([NT, NR], F32)
        idxf_all = A.alloc([NT * 2], F32)
        m6c = A.mark()
        xt_ring = Ring(A, "xt", 3, [D], F32)
        xs_ring = Ring(A, "xs", 2, [D], BF16)
        hT_ring = Ring(A, "hT", 2, [KC, 128], BF16)
        c6 = {}

        def c6_A(i):
            tsl = slice(i * 128, (i + 1) * 128)
            xt, kx = xt_ring.next()
            DMA("sp", xt, h_d[tsl, :], R=[("h_d", i)], W=[kx])
            for blk in range(4):
                p, kp = PS()
                for fc in range(KC):
                    MM(p[:, :], U[:, fc, tsl], Wbig[:, fc, blk * 512:(blk + 1) * 512], fc == 0, fc == KC - 1,
                       [("oc", fc, i * 128 // GS), ("Wbig", blk)], [kp])
                TT("dve", xt[:, blk * 512:(blk + 1) * 512], p[:, :], xt[:, blk * 512:(blk + 1) * 512], ALU.add,
                   [kp, kx], [kx])
            DMA("act", h_d[tsl, :], xt, R=[kx], W=[("h_d", i)])
            c6[i] = (xt, kx)

        def c6_Bpre(i):
            xt, kx = c6.pop(i)
            hn, khn = norm_pre(xt, kx, xs_ring)
            DMA("act", Hn_d[i * 128:(i + 1) * 128, :], hn, R=[khn], W=[])
            c6[("hn", i)] = (hn, khn)

        def c6_B(i):
            hn, khn = c6.pop(("hn", i))
            hT, khT = hT_ring.next()
            norm_post(hn, khn, hT, 0, khT)
            pr, kpr = PS()
            for kc in range(KC):
                MM(pr[:, 0:NR], hT[:, kc, :], Wr[:, kc, :], kc == 0, kc == KC - 1, [khT, "Wr"], [kpr])
            TT("dve", L_all[:, i, :], pr[:, 0:NR], brb, ALU.add, [kpr, "brb"], [("L_all", i)])

        c6_A(0)
        for i in range(NT):
            c6_Bpre(i)
            if i + 1 < NT:
                c6_A(i + 1)
            c6_B(i)
        S.barrier()
        A.release(m6c)

        NTE = NT * NE
        assert NTE <= 512
        tcnt = [0]

        def tmp(shape):
            tcnt[0] += 1
            return A.alloc(shape, F32), ("rt", tcnt[0])

        Lg = L_all[:, :, 0:NG]
        Le = L_all[:, :, NG:NR]
        gmax, kgmax = tmp([NT])
        RMAX(gmax, Lg, AX.X, [], [kgmax])
        gmb = gmax.unsqueeze(2).to_broadcast([128, NT, NG])
        eg, keg = tmp([NT, NG])
        TT("dve", eg, Lg, gmb, ALU.subtract, [kgmax], [keg])
        ACT(eg, eg, AF.Exp, [keg], [(keg, 1)])
        gsum, kgsum = tmp([NT])
        RSUM(gsum, eg, AX.X, [(keg, 1)], [kgsum])
        pgrp, kpgrp = tmp([NT])
        RECIP(pgrp, gsum, [kgsum], [kpgrp])
        pen, kpen = tmp([NT, NG])
        TT("dve", pen, Lg, gmb, ALU.is_equal, [kgmax], [kpen])
        TS("dve", pen, pen, 1.0, ALU.subtract, [kpen], [(kpen, 1)], s2=1e30, op1=ALU.mult)
        EL, kEL = tmp([NT, NE])
        TT("dve", EL.rearrange("p t (g e) -> p t g e", e=8), Le.rearrange("p t (g e) -> p t g e", e=8),
           pen.unsqueeze(3).to_broadcast([128, NT, NG, 8]), ALU.add, [(kpen, 1)], [kEL])
        m1, km1 = tmp([NT])
        RMAX(m1, EL, AX.X, [kEL], [km1])
        oh1, koh1 = tmp([NT, NE])
        TT("dve", oh1, EL, m1.unsqueeze(2).to_broadcast([128, NT, NE]), ALU.is_equal, [kEL, km1], [koh1])
        EL2, kEL2 = tmp([NT, NE])
        STT(EL2.rearrange("p t e -> p (t e)"), oh1.rearrange("p t e -> p (t e)"), -1e30,
            EL.rearrange("p t e -> p (t e)"), ALU.mult, ALU.add, [koh1, kEL], [kEL2])
        m2_, km2 = tmp([NT])
        RMAX(m2_, EL2, AX.X, [kEL2], [km2])
        oh2, koh2 = tmp([NT, NE])
        TT("dve", oh2, EL2, m2_.unsqueeze(2).to_broadcast([128, NT, NE]), ALU.is_equal, [kEL2, km2], [koh2])
        e2, ke2 = tmp([NT])
        TT("dve", e2, m2_, m1, ALU.subtract, [km1, km2], [ke2])
        ACT(e2, e2, AF.Exp, [ke2], [(ke2, 1)])
        rr, krr = tmp([NT])
        TS("dve", rr, e2, 1.0, ALU.add, [(ke2, 1)], [krr])
        RECIP(rr, rr, [krr], [(krr, 1)])
        w3 = w_all.rearrange("p (t k) -> p t k", k=2)
        TT("dve", w3[:, :, 0], pgrp, rr, ALU.mult, [kpgrp, (krr, 1)], [("w_all", 0)])
        TT("dve", w3[:, :, 1], pgrp, w3[:, :, 0], ALU.subtract, [kpgrp, ("w_all", 0)], [("w_all", 1)])
        Ab, kAb = tmp([NT, NE])
        TT("dve", Ab, oh1, oh2, ALU.add, [koh1, koh2], [kAb])
        Ab2 = Ab.rearrange("p t e -> p (t e)")
        pk_, kpk_ = PS()
        MM(pk_[:, 0:NTE], LT, Ab2, True, True, ["tri", kAb], [kpk_])
        pc_, kpc_ = PS()
        MM(pc_[:, 0:NTE], onesF, Ab2, True, True, ["onesF", kAb], [kpc_])
        cnt, kcnt = tmp([NT, NE])
        CP("act", cnt.rearrange("p t e -> p (t e)"), pc_[:, 0:NTE], [kpc_], [kcnt])
        base, kbase = tmp([NT, NE])
        MEMSET("dve", base[:, 0, :], 0.0, [(kbase, 0)])
        for t in range(1, NT):
            TT("dve", base[:, t, :], base[:, t - 1, :], cnt[:, t - 1, :], ALU.add, [(kbase, t - 1), kcnt], [(kbase, t)])
        kbase_all = [(kbase, t) for t in range(NT)]
        RE, kRE = tmp([NT, NE])
        TT("dve", RE.rearrange("p t e -> p (t e)"), pk_[:, 0:NTE], base.rearrange("p t e -> p (t e)"), ALU.add,
           [kpk_] + kbase_all, [kRE])
        REc, kREc = tmp([NT, NE])
        TT("dve", REc, RE, ecb.unsqueeze(1).to_broadcast([128, NT, NE]), ALU.add, [kRE, "ecb"], [kREc])
        ix3 = idxf_all.rearrange("p (t k) -> p t k", k=2)
        ii3 = idx_all.rearrange("p (t k) -> p t k", k=2)
        for k, (oh, koh) in enumerate(((oh1, koh1), (oh2, koh2))):
            t1, kt1 = tmp([NT, NE])
            TT("dve", t1, oh, REc, ALU.mult, [koh, kREc], [kt1])
            ix, kix = tmp([NT])
            RSUM(ix, t1, AX.X, [kt1], [kix])
            TT("dve", t1, oh, RE, ALU.mult, [koh, kRE, kix], [(kt1, 1)])
            rsel, krsel = tmp([NT])
            RSUM(rsel, t1, AX.X, [(kt1, 1)], [krsel])
            TS("dve", rsel, rsel, float(C) - 0.5, ALU.is_gt, [krsel], [(krsel, 1)], s2=1e7, op1=ALU.mult)
            TT("dve", ix3[:, :, k], ix, rsel, ALU.add, [kix, (krsel, 1)], [("idxf", k)])
            CP("dve", ii3[:, :, k], ix3[:, :, k], [("idxf", k)], [("idx_all", k)])
        hn_ring = Ring(A, "hnr", 3, [D], BF16)
        for i in range(NT):
            hn, khn = hn_ring.next()
            DMA("sp", hn, Hn_d[i * 128:(i + 1) * 128, :], W=[khn])
            for k in range(2):
                S.op("pool", (lambda hn_, ixap: (lambda e: e.indirect_dma_start(
                    out=Xs_d, out_offset=bass.IndirectOffsetOnAxis(ap=ixap, axis=0), in_=hn_, in_offset=None,
                    bounds_check=BC(e), oob_is_err=False)))(hn, idx_all[:, 2 * i + k:2 * i + k + 1]),
                    R=[khn, ("idx_all", k)], W=[], dma=True)
        if debug:
            DMA("sp", dbg_idx, idxf_all, R=[("idxf", k) for k in range(2)])
            DMA("sp", dbg_w, w_all, R=[("w_all", k) for k in range(2)])
        S.barrier()
        A.release(m_base)

        CT = C // 128
        Wx = Ring(A, "Wx", 8, [KC, 512], BF16)
        xtm_ring = Ring(A, "xtm", 2, [CT, D], BF16)
        xsT_ring = Ring(A, "xsT", 2, [KC, C], BF16)
        hid_ring = Ring(A, "hid", 2, [8, C], BF16)
        sg_ring = Ring(A, "sg", 2, [C], F32)
        y_ring = Ring(A, "ysb", 2, [D], BF16)
        for e_ in range(NE):
            Wg2, Wu2, Wd2 = [], [], []
            for hf in range(2):
                wg_, kg_ = Wx.next()
                wu_, ku_ = Wx.next()
                if e_ < KPRE:
                    DMA("sp", wg_, wg_b[e_].rearrange("p (k n) -> p k n", n=1024)[:, :, hf * 512:(hf + 1) * 512],
                        R=[("pre_g", e_)], W=[kg_])
                    DMA("sp", wu_, wu_b[e_].rearrange("p (k n) -> p k n", n=1024)[:, :, hf * 512:(hf + 1) * 512],
                        R=[("pre_u", e_)], W=[ku_])
                else:
                    DMA("pool", wg_, w_gate[e_][:, hf * 512:(hf + 1) * 512].rearrange("(k p) n -> p k n", p=128), W=[kg_])
                    DMA("pool", wu_, w_up[e_][:, hf * 512:(hf + 1) * 512].rearrange("(k p) n -> p k n", p=128), W=[ku_])
                Wg2.append((wg_, kg_))
                Wu2.append((wu_, ku_))
            for hf in range(2):
                wd_, kd_ = Wx.next()
                wdv = wd_.rearrange("p k n -> p (k n)").rearrange("p (k n) -> p k n", n=D)
                if e_ < KPRE:
                    DMA("sp", wdv, wd_b[e_].rearrange("p (k n) -> p k n", n=D)[:, hf * 4:(hf + 1) * 4, :],
                        R=[("pre_d", e_)], W=[kd_])
                else:
                    DMA("pool", wdv, w_down[e_][hf * 512:(hf + 1) * 512, :].rearrange("(k p) n -> p k n", p=128), W=[kd_])
                Wd2.append((wdv, kd_))
            if e_ == 0:
                xtm_n = xtm_ring.next()
                DMA("act", xtm_n[0], Xs_d[0:C, :].rearrange("(a p) d -> p a d", p=128), W=[xtm_n[1]])
            xtm, kxtm = xtm_n
            if e_ + 1 < NE:
                xtm_n = xtm_ring.next()
                DMA("act", xtm_n[0], Xs_d[(e_ + 1) * C:(e_ + 2) * C, :].rearrange("(a p) d -> p a d", p=128),
                    W=[xtm_n[1]])
            xsT, kxsT = xsT_ring.next()
            for r in range(CT):
                for b in range(2):
                    pt, kp = PS()
                    pb = pt[:, :].bitcast(BF16)
                    for j in range(8):
                        kc = b * 8 + j
                        TR(pb[:, j * 128:(j + 1) * 128], xtm[:, r, kc * 128:(kc + 1) * 128], [kxtm, "ident"], [kp])
                    CP(ev_eng(), xsT[:, b * 8:(b + 1) * 8, r * 128:(r + 1) * 128],
                       pb.rearrange("p (a b) -> p a b", b=128), [kp], [(kxsT, r, b)])
            kxs_all = [(kxsT, r, b) for r in range(CT) for b in range(2)]
            hid, khid = hid_ring.next()
            for j in range(8):
                pg, kpg = PS()
                Wg, kWg = Wg2[j // 4]
                Wu, kWu = Wu2[j // 4]
                jl = j % 4
                for kc in range(KC):
                    MM(pg[:, 0:C], Wg[:, kc, jl * 128:(jl + 1) * 128], xsT[:, kc, :], kc == 0, kc == KC - 1,
                       [kWg] + kxs_all, [kpg])
                pu, kpu = PS()
                for kc in range(KC):
                    MM(pu[:, 0:C], Wu[:, kc, jl * 128:(jl + 1) * 128], xsT[:, kc, :], kc == 0, kc == KC - 1,
                       [kWu] + kxs_all, [kpu])
                sg, ksg = sg_ring.next()
                ACT(sg, pg[:, 0:C], AF.Silu, [kpg], [ksg])
                TT("dve", hid[:, j, :], pu[:, 0:C], sg, ALU.mult, [kpu, ksg], [(khid, j)])
            khid_all = [(khid, j) for j in range(8)]
            for r in range(CT):
                ysb, ky = y_ring.next()
                for blk in range(4):
                    p, kp = PS()
                    for j in range(8):
                        MM(p[:, :], hid[:, j, r * 128:(r + 1) * 128], Wd2[j // 4][0][:, j % 4, blk * 512:(blk + 1) * 512],
                           j == 0, j == 7, khid_all + [Wd2[j // 4][1]], [kp])
                    CP(ev_eng(), ysb[:, blk * 512:(blk + 1) * 512], p[:, :], [kp], [], Wm=[ky])
                DMA("act", Y_d[e_ * C + r * 128:e_ * C + (r + 1) * 128, :], ysb, R=[ky], W=[])
        S.barrier()
        A.release(m_base)

        load_gain(g_final)
        xt_ring = Ring(A, "xt", 2, [D], F32)
        y1_ring = Ring(A, "y1", 3, [D], BF16)
        y2_ring = Ring(A, "y2", 3, [D], BF16)
        o_ring = Ring(A, "o", 2, [D], F32)
        for i in range(NT):
            tsl = slice(i * 128, (i + 1) * 128)
            xt, kx = xt_ring.next()
            DMA("sp", xt, h_d[tsl, :], W=[kx])
            ys = []
            for k, ring in enumerate((y1_ring, y2_ring)):
                yb, kyb = ring.next()
                S.op("pool", (lambda yb_, ixap: (lambda e: e.indirect_dma_start(
                    out=yb_, out_offset=None, in_=Y_d, in_offset=bass.IndirectOffsetOnAxis(ap=ixap, axis=0),
                    bounds_check=BC(e), oob_is_err=False)))(yb, idx_all[:, 2 * i + k:2 * i + k + 1]),
                    R=[], W=[kyb], dma=True)
                ys.append((yb, kyb))
            STT(xt, ys[0][0], w_all[:, 2 * i:2 * i + 1], xt, ALU.mult, ALU.add, [ys[0][1], kx], [kx])
            STT(xt, ys[1][0], w_all[:, 2 * i + 1:2 * i + 2], xt, ALU.mult, ALU.add, [ys[1][1], kx], [kx])
            ss, kss = small.next()
            rs, krs = small.next()
            rstd, krstd = small.next()
            ACT(junk, xt, AF.Square, [kx], [kss], accum=ss)
            ACT(rs, ss, AF.Sqrt, [kss], [krs], bias=EPS, scale=1.0 / D)
            RECIP(rstd, rs, [krs], [krstd])
            ot, ko = o_ring.next()
            STT(ot, xt, rstd, gainb, ALU.mult, ALU.mult, [kx, krstd, "gainb"], [ko])
            DMA("act", out[tsl, :], ot, R=[ko], W=[("out", i)])
        S.barrier(final=True)
        S.emit()
        nc._marks = S.marks
    return nc


def _consts(cfg):
    s = np.arange(128)[:, None]
    t = np.arange(128)[None, :]
    tri = np.stack([(s <= t), (s >= t), (s > t), (s < t)], axis=1).astype(np.float32)
    slopes = 2.0 ** (-8.0 * (np.arange(8, dtype=np.float64) + 1.0) / 8.0)
    k = np.arange(128, dtype=np.float64)[:, None]
    q = np.arange(128, dtype=np.float64)[None, :]
    am = np.zeros((128, 3, 8, 128), np.float64)
    for pos in range(3):
        if pos == 0:
            dist = q + 128 - k
        elif pos == 1:
            dist = np.abs(q - k)
        else:
            dist = k + 128 - q
        valid = (dist <= 128)
        for h in range(8):
            am[:, pos, h, :] = np.where(valid, -slopes[h] * dist, -30000.0)
    ec = (np.arange(cfg.NE, dtype=np.float32) * cfg.C)[None, :]
    return dict(c_ident=np.eye(128, dtype=np.float32), c_tri=np.ascontiguousarray(tri),
                c_amask=np.ascontiguousarray(am.reshape(128, -1).astype(np.float32)), c_ec=ec)


def make_in_maps(inputs, cfg, n_cores):
    f32 = lambda a: np.ascontiguousarray(np.asarray(a, dtype=np.float32))
    T = cfg.T
    x = f32(inputs["x"])
    mem = f32(inputs["mem"])
    w_in = f32(inputs["w_in"][0])
    w_in_sw = np.concatenate([w_in[:, :4608], w_in[:, 4624:4640], w_in[:, 4608:4624]], axis=1)
    w_in_sw = np.ascontiguousarray(w_in_sw)
    wgf = f32(np.concatenate([inputs["w_gla_gf"][0], inputs["b_gla_gf"][0][None, :]], axis=0))
    wgb = f32(np.concatenate([inputs["w_gla_gb"][0], inputs["b_gla_gb"][0][None, :]], axis=0))
    shared = dict(
        g_mix=f32(inputs["norm_mix"][0][None, :]), g_cross=f32(inputs["norm_cross"][0][None, :]),
        g_mem=f32(inputs["norm_mem"][0][None, :]), g_ffn=f32(inputs["norm_ffn"][0][None, :]),
        g_final=f32(inputs["norm_final"][None, :]),
        ga_col=f32(np.asarray(inputs["attn_out_norm"][0]).reshape(8, 128).T),
        sink=f32(inputs["sink_logit"][0][None, :]),
        gg_col=f32(np.asarray(inputs["gla_out_norm"][0]).reshape(2, 128).T),
        w_out=f32(inputs["w_out"][0]), w_cq=f32(inputs["w_cq"][0]), w_ck=f32(inputs["w_ck"][0]),
        w_cv=f32(inputs["w_cv"][0]), w_co=f32(inputs["w_co"][0]),
        w_r=f32(np.concatenate([inputs["w_router_group"][0], inputs["w_router_expert"][0]], axis=1)),
        b_r=f32(np.concatenate([inputs["b_router_group"][0], inputs["b_router_expert"][0]])[None, :]),
        w_gate=f32(inputs["w_gate"][0]), w_up=f32(inputs["w_up"][0]), w_down=f32(inputs["w_down"][0]),
    )
    shared.update(_consts(cfg))
    maps = []
    for c in range(n_cores):
        b, half = c // 2, c % 2
        m = dict(shared)
        if half == 1:
            m["x_own"] = np.ascontiguousarray(x[b, T:2 * T])
            m["x_oth"] = np.ascontiguousarray(x[b, 0:T])
            m["w_in"] = w_in
            m["wgP"], m["wgM"] = wgf, wgb
        else:
            m["x_own"] = np.ascontiguousarray(x[b, 0:T][::-1])
            m["x_oth"] = np.ascontiguousarray(x[b, T:2 * T][::-1])
            m["w_in"] = w_in_sw
            m["wgP"], m["wgM"] = wgb, wgf
        m["mem"] = mem[b]
        maps.append(m)
    return maps


def assemble(results, cfg, n_cores, key="out"):
    T = cfg.T
    B = n_cores // 2
    outp = np.empty((B, 2 * T, D), np.float32)
    for c in range(n_cores):
        b, half = c // 2, c % 2
        o = np.asarray(results[c][key])
        if half == 1:
            outp[b, T:2 * T] = o
        else:
            outp[b, 0:T] = o[::-1]
    return outp


_NC_CACHE = {}


def kernel(**inputs):
    cfg = Cfg()
    if "nc" not in _NC_CACHE:
        _NC_CACHE["nc"] = build(cfg)
    nc = _NC_CACHE["nc"]
    in_maps = make_in_maps(inputs, cfg, 8)
    res = run_bass_kernel_spmd(nc, in_maps, core_ids=list(range(8)))
    return assemble(res.results, cfg, 8)
```

```python
import math
from contextlib import ExitStack

import numpy as np
import concourse.bass as bass
import concourse.mybir as mybir
from concourse.bass_utils import run_bass_kernel_spmd

F32 = mybir.dt.float32
BF16 = mybir.dt.bfloat16
I32 = mybir.dt.int32
AF = mybir.ActivationFunctionType
ALU = mybir.AluOpType
AX = mybir.AxisListType

D = 2048
KC = 16
MEM = 256
IN_COLS = 4640
EPS = 1e-6
NDSEM = 8


class Cfg:
    def __init__(self, T=2048, NE=32, C=256, KPRE=12, K1=12):
        self.KPRE = min(KPRE, NE)
        self.K1 = min(K1, self.KPRE)
        self.T = T
        self.NE = NE
        self.C = C
        self.NG = NE // 8
        self.NT = T // 128
        self.GS = min(512, T)
        self.NTG = T // self.GS


class Sched:
    ENG = ("pe", "act", "dve", "pool", "sp")
    DQ = ("sp", "act", "pool")

    def __init__(self, nc, es):
        self.nc = nc
        self.csem = {e: es.enter_context(nc.semaphore("c_" + e)) for e in self.ENG}
        self.ccnt = {e: 0 for e in self.ENG}
        self.nds = {"sp": 24, "act": 12, "pool": 8}
        self.dsem = {q: [es.enter_context(nc.semaphore("d_%s_%d" % (q, i))) for i in range(self.nds[q])]
                     for q in self.DQ}
        self.dcnt = {q: 0 for q in self.DQ}
        self.bgsem = [es.enter_context(nc.semaphore("bg_%d" % i)) for i in range(NDSEM)]
        self.bgcnt = 0
        self.bgw = {}
        self.prog = {e: [] for e in self.ENG}
        self.seen = {e: {} for e in self.ENG}
        self.lastw = {}
        self.readers = {}
        self.multi = {}
        self.genw = {}

    def _need(self, eng, toks, is_pe):
        waits = {}
        for t in toks:
            if t is None:
                continue
            sem, val, kind, src = t
            if is_pe and kind == "c" and src == "pe":
                continue
            k = id(sem)
            if self.seen[eng].get(k, 0) >= val:
                continue
            if k not in waits or waits[k][1] < val:
                waits[k] = (sem, val)
        for k, (sem, val) in waits.items():
            self.seen[eng][k] = val
        return list(waits.values())

    def op(self, eng, fn, R=(), W=(), dma=False, bg=False, Wm=()):
        deps = []
        for r in R:
            deps.append(self.lastw.get(r))
            deps.append(self.bgw.get(r))
            deps.extend(self.multi.get(r, ()))
        for w in W:
            deps.append(self.lastw.get(w))
            deps.extend(self.multi.get(w, ()))
            deps.extend(self.readers.get(w, ()))
        for w in Wm:
            rd = self.readers.get(w, ())
            if rd or self.lastw.get(w) is not None:
                self.genw[w] = list(rd) + [self.lastw.get(w)] + list(self.multi.get(w, ()))
                self.multi[w] = []
                self.lastw[w] = None
                self.readers[w] = []
            deps.extend(self.genw.get(w, ()))
        if bg:
            n = self.bgcnt
            self.bgcnt += 1
            slot = n % NDSEM
            sem = self.bgsem[slot]
            if n >= NDSEM:
                deps.append((sem, 16 * (n // NDSEM), "b", eng))
            tok = (sem, 16 * (n // NDSEM + 1), "b", eng)
            inc = (sem, 16)
            waits = self._need(eng, deps, False)
            self.prog[eng].append((waits, fn, inc))
            for w in W:
                self.bgw[w] = tok
            return tok
        if dma:
            n = self.dcnt[eng]
            self.dcnt[eng] += 1
            nd = self.nds[eng]
            slot = n % nd
            sem = self.dsem[eng][slot]
            if n >= nd:
                deps.append((sem, 16 * (n // nd), "d", eng))
            tok = (sem, 16 * (n // nd + 1), "d", eng)
            inc = (sem, 16)
        else:
            self.ccnt[eng] += 1
            tok = (self.csem[eng], self.ccnt[eng], "c", eng)
            inc = (self.csem[eng], 1)
        waits = self._need(eng, deps, eng == "pe" and not dma)
        self.prog[eng].append((waits, fn, inc))
        for r in R:
            self.readers.setdefault(r, []).append(tok)
        for w in W:
            self.lastw[w] = tok
            self.readers[w] = []
            self.multi[w] = []
            self.genw[w] = []
        for w in Wm:
            self.multi.setdefault(w, []).append(tok)
        return tok

    def barrier(self, final=False):
        self.marks = getattr(self, "marks", [])
        self.marks.append(dict(self.ccnt))
        toks = []
        if final:
            n = self.bgcnt
            for slot in range(NDSEM):
                k = (n - slot + NDSEM - 1) // NDSEM if n > slot else 0
                if k > 0:
                    toks.append((self.bgsem[slot], 16 * k, "b", "pool"))
        for e in self.ENG:
            if self.ccnt[e] > 0:
                toks.append((self.csem[e], self.ccnt[e], "c", e))
        for q in self.DQ:
            n = self.dcnt[q]
            nd = self.nds[q]
            for slot in range(nd):
                k = (n - slot + nd - 1) // nd if n > slot else 0
                if k > 0:
                    toks.append((self.dsem[q][slot], 16 * k, "d", q))
        for e in self.ENG:
            waits = self._need(e, toks, False)
            if waits:
                self.prog[e].append((waits, None, None))
        self.lastw.clear()
        self.readers.clear()
        self.multi.clear()
        self.genw.clear()

    def emit(self):
        prog = self.prog

        def run(name, eng):
            for waits, fn, inc in prog[name]:
                for sem, val in waits:
                    eng.wait_ge(sem, val)
                if fn is None:
                    continue
                fn(eng).then_inc(inc[0], inc[1])

        with self.nc.Block() as block:
            @block.tensor
            def _(e):
                run("pe", e)

            @block.scalar
            def _(e):
                run("act", e)

            @block.vector
            def _(e):
                run("dve", e)

            @block.gpsimd
            def _(e):
                run("pool", e)

            @block.sync
            def _(e):
                run("sp", e)


class Arena:
    def __init__(self, nc, es, nbytes):
        self.n = nbytes // 2
        self.t = es.enter_context(nc.sbuf_tensor("arena", [128, self.n], BF16))
        self.off = 0
        self.peak = 0

    def mark(self):
        return self.off

    def release(self, m):
        self.off = m

    def alloc(self, shape, dtype, parts=128):
        esz = 2 if dtype == BF16 else 4
        n = 1
        for s in shape:
            n *= s
        nb = (n * esz + 63) // 64 * 64
        a = self.off
        self.off += nb // 2
        assert self.off <= self.n, "SBUF arena overflow: %d > %d" % (self.off * 2, self.n * 2)
        self.peak = max(self.peak, self.off)
        v = self.t[0:parts, a:a + n * esz // 2]
        if dtype != BF16:
            v = v.bitcast(dtype)
        if len(shape) == 2:
            v = v.rearrange("p (a b) -> p a b", b=shape[1])
        elif len(shape) == 3:
            v = v.rearrange("p (a b c) -> p a b c", b=shape[1], c=shape[2])
        return v


class Ring:
    def __init__(self, A, name, n, shape, dtype, parts=128):
        self.bufs = [A.alloc(shape, dtype, parts) for _ in range(n)]
        self.name = name
        self.i = 0

    def next(self):
        k = self.i % len(self.bufs)
        self.i += 1
        return self.bufs[k], (self.name, k)


def build(cfg, debug=False):
    T, NE, C, NG, NT, GS, NTG = cfg.T, cfg.NE, cfg.C, cfg.NG, cfg.NT, cfg.GS, cfg.NTG
    NR = NG + NE
    NSLOT = NE * C
    nc = bass.Bass("TRN2", target_bir_lowering=False)

    def din(name, shape, dt=F32):
        return nc.dram_tensor(name, list(shape), dt, kind="ExternalInput").ap()

    skind = "ExternalOutput" if debug else "Internal"

    def dscr(name, shape, dt):
        return nc.dram_tensor(name, list(shape), dt, kind=skind).ap()

    x_own = din("x_own", [T, D])
    x_oth = din("x_oth", [T, D])
    mem = din("mem", [MEM, D])
    g_mix = din("g_mix", [1, D])
    g_cross = din("g_cross", [1, D])
    g_mem = din("g_mem", [1, D])
    g_ffn = din("g_ffn", [1, D])
    g_final = din("g_final", [1, D])
    w_in = din("w_in", [D, IN_COLS])
    ga_col = din("ga_col", [128, 8])
    sink = din("sink", [1, 8])
    wgP = din("wgP", [17, 512])
    wgM = din("wgM", [17, 512])
    gg_col = din("gg_col", [128, 2])
    w_out = din("w_out", [D, D])
    w_cq = din("w_cq", [D, D])
    w_ck = din("w_ck", [D, D])
    w_cv = din("w_cv", [D, D])
    w_co = din("w_co", [D, D])
    w_r = din("w_r", [D, NR])
    b_r = din("b_r", [1, NR])
    w_gate = din("w_gate", [NE, D, 1024])
    w_up = din("w_up", [NE, D, 1024])
    w_down = din("w_down", [NE, 1024, D])
    c_ident = din("c_ident", [128, 128])
    c_tri = din("c_tri", [128, 4, 128])
    c_amask = din("c_amask", [128, 3 * 8 * 128])
    c_ec = din("c_ec", [1, NE])

    out = nc.dram_tensor("out", [T, D], F32, kind="ExternalOutput").ap()

    kaT_d = dscr("kaT_d", [2, 128, T + 256], BF16)
    va_d = dscr("va_d", [T + 256, 256], BF16)
    qaT_d = dscr("qaT_d", [8, 128, T], BF16)
    qgT_d = dscr("qgT_d", [4, 128, T], BF16)
    kgT_d = dscr("kgT_d", [4, 128, T], BF16)
    kg_d = dscr("kg_d", [T, 512], BF16)
    vg_d = dscr("vg_d", [T, 1024], BF16)
    rgT_d = dscr("rgT_d", [8, 128, T], BF16)
    lP_d = dscr("lP_d", [T, 512], F32)
    lM_d = dscr("lM_d", [T, 512], F32)
    A_d = dscr("A_d", [NT, 128, 16, 128], BF16)
    h_d = dscr("h_d", [T, D], F32)
    Xs_d = dscr("Xs_d", [NSLOT, D], BF16)
    Hn_d = nc.dram_tensor("Hn_d", [T, D], BF16, kind="Internal").ap()
    Y_d = dscr("Y_d", [NSLOT, D], BF16)
    KPRE = cfg.KPRE
    w_in_b = nc.dram_tensor("w_in_b", [128, KC * IN_COLS], BF16, kind="Internal").ap()
    wbig_b = {}
    for nm in ("w_out", "w_cq", "w_ck", "w_cv", "w_co"):
        wbig_b[nm] = nc.dram_tensor(nm + "_b", [128, KC * D], BF16, kind="Internal").ap()
    wg_b = nc.dram_tensor("wg_b", [max(KPRE, 1), 128, KC * 1024], BF16, kind="Internal").ap()
    wu_b = nc.dram_tensor("wu_b", [max(KPRE, 1), 128, KC * 1024], BF16, kind="Internal").ap()
    wd_b = nc.dram_tensor("wd_b", [max(KPRE, 1), 128, 8 * D], BF16, kind="Internal").ap()
    if debug:
        dbg_S = dscr("dbg_S", [128, 1024], F32)
        dbg_idx = dscr("dbg_idx", [128, NT * 2], F32)
        dbg_w = dscr("dbg_w", [128, NT * 2], F32)

    with ExitStack() as es:
        S = Sched(nc, es)
        A = Arena(nc, es, 206 * 1024)
        psb = [es.enter_context(nc.psum_tensor("ps%d" % i, [128, 512], F32)) for i in range(8)]
        psi = [0]

        def PS():
            k = psi[0] % 8
            psi[0] += 1
            return psb[k], ("ps", k)

        def DMA(q, out_, in_, R=(), W=()):
            return S.op(q, lambda e: e.dma_start(out=out_, in_=in_), R, W, dma=True)

        def MM(out_, lhsT, rhs, start, stop, R, W):
            return S.op("pe", lambda e: e.matmul(out_, lhsT=lhsT, rhs=rhs, start=start, stop=stop), R, W)

        def TR(out_, in_, R, W):
            return S.op("pe", lambda e: e.transpose(out=out_, in_=in_, identity=ident), R, W)

        def ACT(out_, in_, func, R, W, bias=None, scale=None, accum=None, Wm=()):
            kw = {}
            if bias is not None:
                kw["bias"] = bias
            if scale is not None:
                kw["scale"] = scale
            if accum is not None:
                kw["accum_out"] = accum
            return S.op("act", lambda e: e.activation(out=out_, in_=in_, func=func, **kw), R, W, Wm=Wm)

        def TT(eng, out_, in0, in1, op, R, W):
            return S.op(eng, lambda e: e.tensor_tensor(out=out_, in0=in0, in1=in1, op=op), R, W)

        def TS(eng, out_, in0, s1, op0, R, W, s2=None, op1=None, Wm=()):
            if op1 is None:
                return S.op(eng, lambda e: e.tensor_scalar(out=out_, in0=in0, scalar1=s1, scalar2=None, op0=op0), R, W, Wm=Wm)
            return S.op(eng, lambda e: e.tensor_scalar(out=out_, in0=in0, scalar1=s1, scalar2=s2, op0=op0, op1=op1), R, W, Wm=Wm)

        def STT(out_, in0, scalar, in1, op0, op1, R, W):
            return S.op("dve", lambda e: e.scalar_tensor_tensor(out=out_, in0=in0, scalar=scalar, in1=in1, op0=op0, op1=op1), R, W)

        def CP(eng, out_, in_, R, W, Wm=()):
            if eng == "act":
                return ACT(out_, in_, AF.Copy, R, W, Wm=Wm)
            return S.op(eng, lambda e: e.tensor_copy(out=out_, in_=in_), R, W, Wm=Wm)

        def RECIP(out_, in_, R, W):
            return S.op("dve", lambda e: e.reciprocal(out=out_, in_=in_), R, W)

        def RMAX(out_, in_, axis, R, W):
            return S.op("dve", lambda e: e.tensor_reduce(out=out_, in_=in_, axis=axis, op=ALU.max), R, W)

        def RSUM(out_, in_, axis, R, W):
            return S.op("dve", lambda e: e.tensor_reduce(out=out_, in_=in_, axis=axis, op=ALU.add), R, W)

        bcreg = []

        def BC(e):
            if not bcreg:
                bcreg.append(e.to_reg(NSLOT - 1))
            return bcreg[0]

        def MEMSET(eng, ap, val, W):
            return S.op(eng, lambda e: e.memset(ap, val), (), W)

        identf = A.alloc([128], F32)
        ident = A.alloc([128], BF16)
        tri = A.alloc([4, 128], F32)
        ones_bf = A.alloc([128], BF16)
        ones_f = A.alloc([1], F32)
        onesF = A.alloc([128], F32)
        Sst = A.alloc([4, 256], F32)
        Sbf = A.alloc([4, 256], BF16)
        gainb = A.alloc([D], F32)
        idx_all = A.alloc([NT * 2], I32)
        w_all = A.alloc([NT * 2], F32)
        LE, GE, GT, LT = tri[:, 0, :], tri[:, 1, :], tri[:, 2, :], tri[:, 3, :]

        DMA("sp", identf, c_ident, W=["identf"])
        DMA("sp", tri, c_tri, W=["tri"])
        CP("dve", ident, identf, ["identf"], ["ident"])
        MEMSET("dve", ones_bf, 1.0, ["ones_bf"])
        MEMSET("dve", ones_f, 1.0, ["ones_f"])
        MEMSET("dve", onesF, 1.0, ["onesF"])
        MEMSET("dve", Sst, 0.0, ["Sst"])
        MEMSET("dve", Sbf, 0.0, ["Sbf"])

        small = Ring(A, "small", 6, [1], F32)
        junk = A.alloc([D], BF16)

        def load_gain(g_ap):
            DMA("sp", gainb, g_ap.partition_broadcast(128), W=["gainb"])

        def norm_pre(xt, kx, xs_ring):
            ss, kss = small.next()
            rs, krs = small.next()
            rstd, krstd = small.next()
            ACT(junk, xt, AF.Square, [kx], [kss], accum=ss)
            ACT(rs, ss, AF.Sqrt, [kss], [krs], bias=EPS, scale=1.0 / D)
            RECIP(rstd, rs, [krs], [krstd])
            xs, kxs = xs_ring.next()
            STT(xs, xt, rstd, gainb, ALU.mult, ALU.mult, [kx, krstd, "gainb"], [kxs])
            return xs, kxs

        def norm_post(xs, kxs, dstT, col0, kdst):
            for b in range(2):
                pt, kp = PS()
                pb = pt[:, :].bitcast(BF16)
                for j in range(8):
                    kc = b * 8 + j
                    TR(pb[:, j * 128:(j + 1) * 128], xs[:, kc * 128:(kc + 1) * 128], [kxs, "ident"], [kp])
                CP("act" if b == 0 else "dve", dstT[:, b * 8:(b + 1) * 8, col0:col0 + 128],
                   pb.rearrange("p (a b) -> p a b", b=128), [kp], [], Wm=[kdst])

        def norm_T(xt, kx, dstT, col0, kdst, xs_ring, hn_out=None):
            xs, kxs = norm_pre(xt, kx, xs_ring)
            norm_post(xs, kxs, dstT, col0, kdst)
            return xs, kxs

        def load_w(q, dst, src2d, c0, ncol, kdst):
            DMA(q, dst, src2d[:, c0:c0 + ncol].rearrange("(k p) n -> p k n", p=128), W=[kdst])

        def BGDMA(out_, in_, W):
            return S.op("pool", lambda e: e.dma_start(out=out_, in_=in_), (), W, bg=True)

        wq = [0]

        def load_wb(dst, wb, ncols_total, c0, ncol, kdst, kpre):
            q = "sp"
            DMA(q, dst, wb.rearrange("p (k n) -> p k n", n=ncols_total)[:, :, c0:c0 + ncol], R=[kpre], W=[kdst])

        def emit_precast_experts(e0, e1):
            for e_ in range(e0, min(e1, KPRE)):
                BGDMA(wg_b[e_].rearrange("p (k n) -> p k n", n=1024), w_gate[e_].rearrange("(k p) n -> p k n", p=128),
                      [("pre_g", e_)])
                BGDMA(wu_b[e_].rearrange("p (k n) -> p k n", n=1024), w_up[e_].rearrange("(k p) n -> p k n", p=128),
                      [("pre_u", e_)])
                BGDMA(wd_b[e_].rearrange("p (k n) -> p k n", n=D), w_down[e_].rearrange("(k p) n -> p k n", p=128),
                      [("pre_d", e_)])

        def emit_precast():
            for blk in range(0, IN_COLS, 1024):
                n = min(1024, IN_COLS - blk)
                BGDMA(w_in_b.rearrange("p (k n) -> p k n", n=IN_COLS)[:, :, blk:blk + n],
                      w_in[:, blk:blk + n].rearrange("(k p) n -> p k n", p=128), [("pre_w_in", blk // 1024)])
            emit_precast_experts(0, cfg.K1)

        m_base = A.mark()

        zt = A.alloc([D], BF16)
        MEMSET("pool", zt, 0.0, ["zt"])
        for r0 in range(0, NSLOT, 128):
            DMA("act", Xs_d[r0:r0 + 128, :], zt, R=["zt"], W=[])

        Wkg = A.alloc([KC, 512], BF16)
        Wvg = A.alloc([KC, 1024], BF16)
        Wlr = A.alloc([KC, 32], BF16)
        Wkva = A.alloc([KC, 512], BF16)
        wgP_sb = A.alloc([512], F32, parts=17)
        wgM_sb = A.alloc([512], F32, parts=17)
        load_w("pool", Wkg, w_in, 2048, 512, "Wkg")
        load_w("pool", Wvg[:, :, 0:512], w_in, 2560, 512, "Wvg0")
        load_w("pool", Wvg[:, :, 512:1024], w_in, 3072, 512, "Wvg1")
        load_w("pool", Wlr, w_in, 4608, 32, "Wlr")
        load_w("pool", Wkva, w_in, 1024, 512, "Wkva")
        DMA("sp", wgP_sb, wgP, W=["wgP"])
        DMA("sp", wgM_sb, wgM, W=["wgM"])
        load_gain(g_mix)
        emit_precast()

        xt_ring = Ring(A, "xt", 3, [D], F32)
        xs_ring = Ring(A, "xs", 2, [D], BF16)
        uTt_ring = Ring(A, "uTt", 3, [KC, 128], BF16)
        lrT_ring = Ring(A, "lrT", 3, [128], F32, parts=17)
        for b_, k_ in zip(lrT_ring.bufs, range(3)):
            MEMSET("dve", b_, 1.0, [("lrT", k_)])
        v_ring = Ring(A, "vsb", 3, [1024], BF16)
        ks_ring = Ring(A, "ksb", 3, [512], BF16)
        e1_ring = Ring(A, "e1", 2, [512], F32)
        l_ring = Ring(A, "l", 2, [512], F32)
        eK_ring = Ring(A, "eK", 2, [512], F32)
        kd_ring = Ring(A, "kd", 2, [512], BF16)
        dec_ring = Ring(A, "dec", 2, [4], F32)
        st_ring = Ring(A, "stg", 2, [512], BF16)

        def gate_l(lrT, klrT, wg_sb, kwg, l, kl):
            pz, kpz = PS()
            MM(pz[:, :], lrT, wg_sb, True, True, [klrT, kwg], [kpz])
            e1, ke1 = e1_ring.next()
            ACT(e1, pz[:, :], AF.Exp, [kpz], [ke1], scale=-1.0)
            ACT(l, e1, AF.Ln, [ke1], [kl], bias=1.0)

        p1 = {}

        def p1_N(i):
            xt, kx = xt_ring.next()
            DMA("sp", xt, x_oth[i * 128:(i + 1) * 128, :], W=[kx])
            uTt, ku = uTt_ring.next()
            norm_T(xt, kx, uTt, 0, ku, xs_ring)
            p1[i] = dict(uTt=uTt, ku=ku)

        def p1_Ak(i):
            c = p1[i]
            uTt, ku = c["uTt"], c["ku"]
            pk, kpk = PS()
            for kc in range(KC):
                MM(pk[:, :], uTt[:, kc, :], Wkg[:, kc, :], kc == 0, kc == KC - 1, [ku, "Wkg"], [kpk])
            ksb, kks = ks_ring.next()
            CP("dve", ksb, pk[:, :], [kpk], [kks])
            c.update(ksb=ksb, kks=kks)
            c["vsb"], c["kv"] = v_ring.next()

        def p1_Av(i, hf):
            c = p1[i]
            uTt, ku, vsb, kv = c["uTt"], c["ku"], c["vsb"], c["kv"]
            pv, kpv = PS()
            for kc in range(KC):
                MM(pv[:, :], uTt[:, kc, :], Wvg[:, kc, hf * 512:(hf + 1) * 512], kc == 0, kc == KC - 1,
                   [ku, "Wvg%d" % hf], [kpv])
            CP("act", vsb[:, hf * 512:(hf + 1) * 512], pv[:, :], [kpv], [(kv, hf)])

        def p1_Alr(i):
            c = p1[i]
            uTt, ku = c["uTt"], c["ku"]
            pl, kpl = PS()
            for kc in range(KC):
                MM(pl[0:16, 0:128], Wlr[:, kc, 0:16], uTt[:, kc, :], kc == 0, kc == KC - 1, [ku, "Wlr"], [kpl])
            lrT, klrT = lrT_ring.next()
            CP("dve", lrT[0:16, :], pl[0:16, 0:128], [kpl], [klrT])
            c.update(lrT=lrT, klrT=klrT)
            if i == NT - 1:
                for g in range(2):
                    pa, kpa = PS()
                    for kc in range(KC):
                        MM(pa[:, 0:128], Wkva[:, kc, g * 128:(g + 1) * 128], uTt[:, kc, :], kc == 0, kc == KC - 1,
                           [ku, "Wkva"], [kpa])
                    st, kst = st_ring.next()
                    CP("act", st[:, 0:128], pa[:, 0:128], [kpa], [kst])
                    DMA("act", kaT_d[g, :, 0:128], st[:, 0:128], R=[kst], W=[])
                pa, kpa = PS()
                for kc in range(KC):
                    MM(pa[:, 0:256], uTt[:, kc, :], Wkva[:, kc, 256:512], kc == 0, kc == KC - 1, [ku, "Wkva"], [kpa])
                st, kst = st_ring.next()
                CP("act", st[:, 0:256], pa[:, 0:256], [kpa], [kst])
                DMA("act", va_d[0:128, :], st[:, 0:256], R=[kst], W=[])

        def p1_A(i):
            p1_Ak(i)
            p1_Av(i, 0)
            p1_Av(i, 1)
            p1_Alr(i)

        def p1_B1(i):
            c = p1[i]
            c["l"], c["kl"] = l_ring.next()
            gate_l(c["lrT"], c["klrT"], wgP_sb, "wgP", c["l"], c["kl"])

        def p1_B2(i):
            c = p1[i]
            l, kl = c["l"], c["kl"]
            psu, kpsu = PS()
            MM(psu[:, :], GT, l, True, True, ["tri", kl], [kpsu])
            eK, keK = eK_ring.next()
            ACT(eK, psu[:, :], AF.Exp, [kpsu], [keK], scale=-1.0 / 16)
            kd, kkd = kd_ring.next()
            TT("dve", kd, c["ksb"], eK, ALU.mult, [c["kks"], keK], [kkd])
            ptot, kpt = PS()
            for h in range(4):
                MM(ptot[:, h:h + 1], l[:, h * 128:(h + 1) * 128], ones_f, True, True, [kl, "ones_f"], [kpt])
            dec, kdec = dec_ring.next()
            ACT(dec, ptot[:, 0:4], AF.Exp, [kpt], [kdec], scale=-1.0 / 16)
            c.update(kd=kd, kkd=kkd, dec=dec, kdec=kdec)

        def p1_B3(i, hs):
            c = p1[i]
            kd, kkd, dec, kdec, vsb, kv = (c[k] for k in ("kd", "kkd", "dec", "kdec", "vsb", "kv"))
            for h in hs:
                pkv, kpkv = PS()
                MM(pkv[:, 0:256], kd[:, h * 128:(h + 1) * 128], vsb[:, h * 256:(h + 1) * 256], True, True,
                   [kkd, (kv, 0), (kv, 1)], [kpkv])
                STT(Sst[:, h, :], Sst[:, h, :], dec[:, h:h + 1], pkv[:, 0:256], ALU.mult, ALU.add,
                    [("Sst", h), kdec, kpkv], [("Sst", h)])

        p1_N(0)
        if NT > 1:
            p1_N(1)
        p1_A(0)
        for i in range(NT):
            nx = i + 1 < NT
            p1_B1(i)
            if nx:
                p1_Ak(i + 1)
            p1_B2(i)
            if nx:
                p1_Av(i + 1, 0)
            p1_B3(i, (0, 1))
            if nx:
                p1_Av(i + 1, 1)
            p1_B3(i, (2, 3))
            if nx:
                p1_Alr(i + 1)
            if i + 2 < NT:
                p1_N(i + 2)
            p1.pop(i)
        for h in range(4):
            CP("act", Sbf[:, h, :], Sst[:, h, :], [("Sst", h)], [("Sbf", h)])
        if debug:
            DMA("sp", dbg_S, Sst.rearrange("p a b -> p (a b)"), R=[("Sst", h) for h in range(4)])
        S.barrier()
        A.release(m_base)

        uT = A.alloc([KC, T], BF16)
        wgP_sb = A.alloc([512], F32, parts=17)
        wgM_sb = A.alloc([512], F32, parts=17)
        DMA("sp", wgP_sb, wgP, W=["wgP"])
        DMA("sp", wgM_sb, wgM, W=["wgM"])
        W_ring = Ring(A, "W", 3, [KC, 512], BF16)
        m2 = A.mark()
        xt_ring = Ring(A, "xt", 2, [D], F32)
        xs_ring = Ring(A, "xs", 2, [D], BF16)
        for i in range(NT):
            xt, kx = xt_ring.next()
            DMA("sp", xt, x_own[i * 128:(i + 1) * 128, :], W=[kx])
            norm_T(xt, kx, uT, i * 128, ("uT", i), xs_ring)
        S.barrier()
        A.release(m2)
        fst_ring = Ring(A, "fst", 3, [T], BF16)
        tst_ring = Ring(A, "tst", 2, [4, 512], BF16)
        lb_ring = Ring(A, "lb", 2, [4, 512], F32)
        e1b_ring = Ring(A, "e1b", 1, [4, 512], F32)
        lrTP = A.alloc([T], F32, parts=17)
        lrTM = A.alloc([T], F32, parts=17)
        MEMSET("dve", lrTP, 1.0, ["lrTP"])
        MEMSET("dve", lrTM, 1.0, ["lrTM"])
        uT_all = [("uT", i) for i in range(NT)]
        evq = [0]

        def ev_eng():
            evq[0] += 1
            return "act" if evq[0] % 2 else "dve"

        def proj_fm(Wt, kW, jl, dst_row, func=None, scale=None, c0=0):
            st, kst = fst_ring.next()
            for tg in range(NTG):
                p, kp = PS()
                for kc in range(KC):
                    MM(p[:, 0:GS], Wt[:, kc, jl * 128:(jl + 1) * 128], uT[:, kc, tg * GS:(tg + 1) * GS],
                       kc == 0, kc == KC - 1, [kW] + uT_all[tg * GS // 128:(tg + 1) * GS // 128], [kp])
                if func is not None:
                    ACT(st[:, tg * GS:(tg + 1) * GS], p[:, 0:GS], func, [kp], [], Wm=[kst])
                elif scale is not None:
                    e = ev_eng()
                    if e == "act":
                        ACT(st[:, tg * GS:(tg + 1) * GS], p[:, 0:GS], AF.Copy, [kp], [], scale=scale, Wm=[kst])
                    else:
                        TS("dve", st[:, tg * GS:(tg + 1) * GS], p[:, 0:GS], scale, ALU.mult, [kp], [], Wm=[kst])
                else:
                    CP(ev_eng(), st[:, tg * GS:(tg + 1) * GS], p[:, 0:GS], [kp], [], Wm=[kst])
            DMA("act", dst_row[:, c0:c0 + T], st, R=[kst], W=[])

        def proj_tm(Wt, kW, cl0, ncol, dst, dc0, r0=0):
            for i4 in range(0, NT, 4):
                n4 = min(4, NT - i4)
                st, kst = tst_ring.next()
                for ii in range(n4):
                    i = i4 + ii
                    p, kp = PS()
                    for kc in range(KC):
                        MM(p[:, 0:ncol], uT[:, kc, i * 128:(i + 1) * 128], Wt[:, kc, cl0:cl0 + ncol],
                           kc == 0, kc == KC - 1, [kW, ("uT", i)], [kp])
                    CP(ev_eng(), st[:, ii, 0:ncol], p[:, 0:ncol], [kp], [], Wm=[kst])
                DMA("act", dst[r0 + i4 * 128:r0 + (i4 + n4) * 128, dc0:dc0 + ncol].rearrange("(a p) c -> p a c", p=128),
                    st[:, 0:n4, 0:ncol], R=[kst], W=[])

        sc_a = 1.0 / math.sqrt(128.0)
        for blk in range(2):
            Wt, kW = W_ring.next()
            load_wb(Wt, w_in_b, IN_COLS, blk * 512, 512, kW, ("pre_w_in", (blk * 512) // 1024))
            for jl in range(4):
                proj_fm(Wt, kW, jl, qaT_d[blk * 4 + jl], scale=sc_a)
        Wt, kW = W_ring.next()
        load_wb(Wt, w_in_b, IN_COLS, 1024, 512, kW, ("pre_w_in", (1024) // 1024))
        for g in range(2):
            proj_fm(Wt, kW, g, kaT_d[g], c0=128)
        proj_tm(Wt, kW, 256, 256, va_d, 0, r0=128)
        Wt, kW = W_ring.next()
        load_wb(Wt, w_in_b, IN_COLS, 1536, 512, kW, ("pre_w_in", (1536) // 1024))
        for jl in range(4):
            proj_fm(Wt, kW, jl, qgT_d[jl], scale=sc_a)
        Wt, kW = W_ring.next()
        load_wb(Wt, w_in_b, IN_COLS, 2048, 512, kW, ("pre_w_in", (2048) // 1024))
        for jl in range(4):
            proj_fm(Wt, kW, jl, kgT_d[jl])
        proj_tm(Wt, kW, 0, 512, kg_d, 0)
        for blk in range(2):
            Wt, kW = W_ring.next()
            load_wb(Wt, w_in_b, IN_COLS, 2560 + blk * 512, 512, kW, ("pre_w_in", (2560 + blk * 512) // 1024))
            proj_tm(Wt, kW, 0, 512, vg_d, blk * 512)
        for blk in range(2):
            Wt, kW = W_ring.next()
            load_wb(Wt, w_in_b, IN_COLS, 3584 + blk * 512, 512, kW, ("pre_w_in", (3584 + blk * 512) // 1024))
            for jl in range(4):
                proj_fm(Wt, kW, jl, rgT_d[blk * 4 + jl], func=AF.Silu)
        Wt, kW = W_ring.next()
        load_wb(Wt[:, :, 0:32], w_in_b, IN_COLS, 4608, 32, kW, ("pre_w_in", (4608) // 1024))
        for dr, lrT in ((0, lrTP), (1, lrTM)):
            for tg in range(NTG):
                p, kp = PS()
                for kc in range(KC):
                    MM(p[0:16, 0:GS], Wt[:, kc, dr * 16:(dr + 1) * 16], uT[:, kc, tg * GS:(tg + 1) * GS],
                       kc == 0, kc == KC - 1, [kW] + uT_all[tg * GS // 128:(tg + 1) * GS // 128], [kp])
                CP("dve", lrT[0:16, tg * GS:(tg + 1) * GS], p[0:16, 0:GS], [kp], ["lrTP" if dr == 0 else "lrTM"])
        for dr, lrT, wg_sb, kwg, l_d in ((0, lrTP, wgP_sb, "wgP", lP_d), (1, lrTM, wgM_sb, "wgM", lM_d)):
            klr = "lrTP" if dr == 0 else "lrTM"
            for i4 in range(0, NT, 4):
                n4 = min(4, NT - i4)
                e1b, ke1b = e1b_ring.next()
                lb, klb = lb_ring.next()
                for ii in range(n4):
                    i = i4 + ii
                    pz, kpz = PS()
                    MM(pz[:, :], lrT[:, i * 128:(i + 1) * 128], wg_sb, True, True, [klr, kwg], [kpz])
                    ACT(e1b[:, ii, :], pz[:, :], AF.Exp, [kpz], [(ke1b, ii)], scale=-1.0)
                ACT(lb[:, 0:n4, :], e1b[:, 0:n4, :], AF.Ln, [(ke1b, ii) for ii in range(n4)], [klb], bias=1.0)
                DMA("sp", l_d[i4 * 128:(i4 + n4) * 128, :].rearrange("(a p) c -> p a c", p=128), lb[:, 0:n4, :],
                    R=[klb], W=[])
        S.barrier()
        A.release(m_base)

        amaskf = A.alloc([3, 8, 128], F32)
        amask = A.alloc([3, 8, 128], BF16)
        esink = A.alloc([8], F32)
        gacol = A.alloc([8], F32)
        DMA("sp", amaskf.rearrange("p a b c -> p (a b c)"), c_amask, W=["amaskf"])
        CP("dve", amask, amaskf, ["amaskf"], ["amask"])
        DMA("sp", esink, sink.partition_broadcast(128), W=["esink0"])
        DMA("sp", gacol, ga_col, W=["gacol"])
        ACT(esink, esink, AF.Exp, ["esink0"], ["esink"])
        NQG = GS // 128
        qa_ring = Ring(A, "qa", 2, [8, GS], BF16)
        ka_ring = Ring(A, "ka", 4, [2, 384], BF16)
        va_ring = Ring(A, "va", 5, [3, 256], BF16)
        E_ring = Ring(A, "E", 4, [512], F32)
        PT_ring = Ring(A, "PT", 20, [512], BF16)
        den_ring = Ring(A, "den", 2, [512], F32)
        rden_ring = Ring(A, "rden", 2, [512], F32)
        osb_ring = Ring(A, "osb", 3, [8, 128], F32)
        sq_ring = Ring(A, "sq", 2, [8, 128], BF16)
        rs_ring = Ring(A, "rs", 2, [128], F32)
        rstd_ring = Ring(A, "rstd", 2, [128], F32)
        oast_ring = Ring(A, "oast", 3, [8, 128], BF16)
        p3 = {}
        qa_cur = [None]

        def p3_S1(n):
            npos = 3 if n < NT - 1 else 2
            if n % NQG == 0:
                qa_cur[0] = qa_ring.next()
                t0 = n * 128
                DMA("sp", qa_cur[0][0], qaT_d[:, :, t0:t0 + GS].rearrange("h d t -> d h t"), R=[], W=[qa_cur[0][1]])
            qa, kqa = qa_cur[0]
            q0 = (n % NQG) * 128
            ka, kka = ka_ring.next()
            va, kva = va_ring.next()
            DMA("sp", ka[:, :, 0:npos * 128], kaT_d[:, :, n * 128:n * 128 + npos * 128].rearrange("g d t -> d g t"),
                R=[], W=[kka])
            DMA("sp", va[:, 0:npos, :], va_d[n * 128:n * 128 + npos * 128, :].rearrange("(b p) c -> p b c", p=128),
                R=[], W=[kva])
            PTs = {}
            for g in range(2):
                for pos in range(npos):
                    p, kp = PS()
                    MM(p[:, :].rearrange("p (h t) -> p h t", t=128), ka[:, g, pos * 128:(pos + 1) * 128],
                       qa[:, g * 4:(g + 1) * 4, q0:q0 + 128], True, False, [kka, kqa], [kp])
                    MM(p[:, :], ident, amask[:, pos, g * 4:(g + 1) * 4, :].rearrange("p h t -> p (h t)"), False, True,
                       ["ident", "amask"], [kp])
                    PT, kPT = PT_ring.next()
                    ACT(PT, p[:, :], AF.Exp, [kp], [kPT])
                    PTs[(g, pos)] = (PT, kPT)
            p3[n] = dict(npos=npos, va=va, kva=kva, PTs=PTs)

        def p3_S2(n):
            c = p3[n]
            npos, va, kva, PTs = c["npos"], c["va"], c["kva"], c["PTs"]
            osb, kosb = osb_ring.next()
            for g in range(2):
                po, kpo = PS()
                for pos in range(npos):
                    MM(po[:, :], va[:, pos, g * 128:(g + 1) * 128], PTs[(g, pos)][0], pos == 0, pos == npos - 1,
                       [kva, PTs[(g, pos)][1]], [kpo])
                pd, kpd = PS()
                for pos in range(npos):
                    MM(pd[:, :], ones_bf, PTs[(g, pos)][0], pos == 0, pos == npos - 1,
                       ["ones_bf", PTs[(g, pos)][1]], [kpd])
                den, kden = den_ring.next()
                TT("dve", den.rearrange("p (h t) -> p h t", t=128), pd[:, :].rearrange("p (h t) -> p h t", t=128),
                   esink[:, g * 4:(g + 1) * 4].unsqueeze(2).to_broadcast([128, 4, 128]), ALU.add,
                   [kpd, "esink"], [kden])
                rden, krden = rden_ring.next()
                RECIP(rden, den, [kden], [krden])
                TT("dve", osb[:, g * 4:(g + 1) * 4, :].rearrange("p h t -> p (h t)"), po[:, :], rden, ALU.mult,
                   [kpo, krden], [(kosb, g)])
            c.update(osb=osb, kosb=kosb)

        def p3_S3(n):
            c = p3.pop(n)
            osb, kosb = c["osb"], c["kosb"]
            sq, ksq = sq_ring.next()
            ACT(sq, osb, AF.Square, [(kosb, 0), (kosb, 1)], [ksq])
            pss, kpss = PS()
            for h in range(8):
                MM(pss[:, 0:128], ones_bf, sq[:, h, :], h == 0, h == 7, ["ones_bf", ksq], [kpss])
            rs, krs = rs_ring.next()
            ACT(rs, pss[:, 0:128], AF.Sqrt, [kpss], [krs], bias=EPS, scale=1.0 / 1024)
            rstd, krstd = rstd_ring.next()
            RECIP(rstd, rs, [krs], [krstd])
            oast, koast = oast_ring.next()
            for h in range(8):
                STT(oast[:, h, :], osb[:, h, :], gacol[:, h:h + 1], rstd, ALU.mult, ALU.mult,
                    [(kosb, 0), (kosb, 1), "gacol", krstd], [(koast, h)])
            DMA("act", A_d[n, :, 0:8, :], oast, R=[(koast, h) for h in range(8)], W=[])

        for step in range(NT + 2):
            if step < NT:
                p3_S1(step)
            if 0 <= step - 1 < NT:
                p3_S2(step - 1)
            if 0 <= step - 2 < NT:
                p3_S3(step - 2)
        S.barrier()
        A.release(m_base)

        oP = A.alloc([NT, 8, 128], F32)
        ggcol = A.alloc([2], F32)
        DMA("sp", ggcol, gg_col, W=["ggcol"])
        qg_ring = Ring(A, "qg", 3, [4, 128], BF16)
        kgT_ring = Ring(A, "kgT", 3, [4, 128], BF16)
        kg_ring = Ring(A, "kg", 3, [512], BF16)
        vg_ring = Ring(A, "vg", 3, [1024], BF16)
        lg_ring = Ring(A, "lg", 3, [512], F32)
        rg_ring = Ring(A, "rg", 2, [8, 128], BF16)
        eK_ring = Ring(A, "eK", 2, [512], F32)
        kd_ring = Ring(A, "kd", 3, [512], BF16)
        eP_ring = Ring(A, "eP", 3, [512], F32)
        eN_ring = Ring(A, "eN", 2, [512], F32)
        qd_ring = Ring(A, "qd", 3, [4, 128], BF16)
        ki_ring = Ring(A, "ki", 2, [4, 128], BF16)
        at_ring = Ring(A, "at", 3, [4, 128], BF16)
        og_ring = Ring(A, "og", 2, [8, 128], F32)
        sqg_ring = Ring(A, "sqg", 2, [8, 128], BF16)
        rsg_ring = Ring(A, "rsg", 2, [512], F32)
        rstdg_ring = Ring(A, "rstdg", 2, [512], F32)
        tmpg_ring = Ring(A, "tmpg", 2, [8, 128], F32)
        ogst_ring = Ring(A, "ogst", 3, [8, 128], BF16)
        Sk = [("Sst", h) for h in range(4)]
        Sbk = [("Sbf", h) for h in range(4)]
        Sbf2 = [Sbf, A.alloc([4, 256], BF16)]
        sb_par = [0]

        def gla_front(c, dr):
            l_d = lP_d if dr == 0 else lM_d
            TRI = LE if dr == 0 else GE
            STR = GT if dr == 0 else LT
            tsl = slice(c * 128, (c + 1) * 128)
            qg, kqg = qg_ring.next()
            kgT, kkgT = kgT_ring.next()
            kg, kkg = kg_ring.next()
            vg, kvg = vg_ring.next()
            lg, klg = lg_ring.next()
            DMA("sp", qg, qgT_d[:, :, tsl].rearrange("h d t -> d h t"), R=[], W=[kqg])
            DMA("sp", kgT, kgT_d[:, :, tsl].rearrange("h d t -> d h t"), R=[], W=[kkgT])
            DMA("sp", kg, kg_d[tsl, :], R=[], W=[kkg])
            DMA("sp", vg, vg_d[tsl, :], R=[], W=[kvg])
            DMA("sp", lg, l_d[tsl, :], R=[], W=[klg])
            psu, kpsu = PS()
            MM(psu[:, :], STR, lg, True, True, ["tri", klg], [kpsu])
            pb, kpb = PS()
            for h in range(4):
                MM(pb[:, h * 128:(h + 1) * 128], lg[:, h * 128:(h + 1) * 128], TRI, True, True, ["tri", klg], [kpb])
            eK, keK = eK_ring.next()
            ACT(eK, psu[:, :], AF.Exp, [kpsu], [keK], scale=-1.0 / 16)
            eP, keP = eP_ring.next()
            ACT(eP, pb[:, :], AF.Exp, [kpb], [keP], scale=-1.0 / 16)
            eN, keN = eN_ring.next()
            ACT(eN, pb[:, :], AF.Exp, [kpb], [keN], scale=1.0 / 16)
            kd, kkd = kd_ring.next()
            TT("dve", kd, kg, eK, ALU.mult, [kkg, keK], [kkd])
            qd, kqd = qd_ring.next()
            TT("dve", qd.rearrange("p h t -> p (h t)"), qg.rearrange("p h t -> p (h t)"), eP, ALU.mult,
               [kqg, keP], [kqd])
            ki, kki = ki_ring.next()
            TT("dve", ki.rearrange("p h t -> p (h t)"), kgT.rearrange("p h t -> p (h t)"), eN, ALU.mult,
               [kkgT, keN], [kki])
            pa, kpa = PS()
            for h in range(4):
                MM(pa[:, h * 128:(h + 1) * 128], ki[:, h, :], qd[:, h, :], True, True, [kki, kqd], [kpa])
            at, kat = at_ring.next()
            TT("dve", at, pa[:, :].rearrange("p (h t) -> p h t", t=128),
               TRI.unsqueeze(1).to_broadcast([128, 4, 128]), ALU.mult, [kpa, "tri"], [kat])
            return dict(c=c, dr=dr, vg=vg, kvg=kvg, kd=kd, kkd=kkd, eP=eP, keP=keP, qd=qd, kqd=kqd, at=at, kat=kat)

        def gla_back(f):
            c, dr = f["c"], f["dr"]
            vg, kvg, kd, kkd, eP, keP, qd, kqd, at, kat = (f[k] for k in
                                                         ("vg", "kvg", "kd", "kkd", "eP", "keP", "qd", "kqd", "at", "kat"))
            pos_ = []
            for half in range(2):
                po, kpo = PS()
                pos_.append((po, kpo))
            for h in range(4):
                for dvc in range(2):
                    r = 2 * h + dvc
                    po, kpo = pos_[r // 4]
                    reg = po[:, (r % 4) * 128:(r % 4 + 1) * 128]
                    MM(reg, vg[:, h * 256 + dvc * 128:h * 256 + (dvc + 1) * 128], at[:, h, :], True, False,
                       [kvg, kat], [kpo])
                    MM(reg, Sbf[:, h, dvc * 128:(dvc + 1) * 128], qd[:, h, :], False, True,
                       [("Sbf", h), kqd], [kpo])
            pkvs = []
            for hp in range(2):
                pkv, kpkv = PS()
                pkvs.append((pkv, kpkv))
                for hh in range(2):
                    h = 2 * hp + hh
                    MM(pkv[:, hh * 256:(hh + 1) * 256], kd[:, h * 128:(h + 1) * 128], vg[:, h * 256:(h + 1) * 256],
                       True, True, [kkd, kvg], [kpkv])
            dcol = 127 if dr == 0 else 0
            for h in range(4):
                pkv, kpkv = pkvs[h // 2]
                STT(Sst[:, h, :], Sst[:, h, :], eP[:, h * 128 + dcol:h * 128 + dcol + 1],
                    pkv[:, (h % 2) * 256:(h % 2 + 1) * 256], ALU.mult, ALU.add,
                    [("Sst", h), keP, kpkv], [("Sst", h)])
            CP("act", Sbf.rearrange("p a b -> p (a b)"), Sst.rearrange("p a b -> p (a b)"), Sk, Sbk)
            if dr == 0:
                for half in range(2):
                    po, kpo = pos_[half]
                    CP("act" if half == 0 else "dve", oP[:, c, half * 4:(half + 1) * 4, :].rearrange("p r t -> p (r t)"),
                       po[:, :], [kpo], [], Wm=[("oP", c)])
                return
            og, kog = og_ring.next()
            for half in range(2):
                po, kpo = pos_[half]
                TT("dve", og[:, half * 4:(half + 1) * 4, :].rearrange("p r t -> p (r t)"), po[:, :],
                   oP[:, c, half * 4:(half + 1) * 4, :].rearrange("p r t -> p (r t)"), ALU.add,
                   [kpo, ("oP", c)], [(kog, half)])
            sq, ksq = sqg_ring.next()
            ACT(sq, og, AF.Square, [(kog, 0), (kog, 1)], [ksq])
            pss, kpss = PS()
            for h in range(4):
                for dvc in range(2):
                    MM(pss[:, h * 128:(h + 1) * 128], ones_bf, sq[:, 2 * h + dvc, :], dvc == 0, dvc == 1,
                       ["ones_bf", ksq], [kpss])
            rs, krs = rsg_ring.next()
            ACT(rs, pss[:, :], AF.Sqrt, [kpss], [krs], bias=EPS, scale=1.0 / 256)
            rstd, krstd = rstdg_ring.next()
            RECIP(rstd, rs, [krs], [krstd])
            rg, krg = rg_ring.next()
            DMA("sp", rg, rgT_d[:, :, c * 128:(c + 1) * 128].rearrange("j d t -> d j t"), R=[], W=[krg])
            tmp, ktmp = tmpg_ring.next()
            for h in range(4):
                for dvc in range(2):
                    r = 2 * h + dvc
                    STT(tmp[:, r, :], og[:, r, :], ggcol[:, dvc:dvc + 1], rstd[:, h * 128:(h + 1) * 128],
                        ALU.mult, ALU.mult, [(kog, 0), (kog, 1), "ggcol", krstd], [(ktmp, r)])
            ogst, kogst = ogst_ring.next()
            TT("dve", ogst, tmp, rg, ALU.mult, [(ktmp, r) for r in range(8)] + [krg], [kogst])
            DMA("act", A_d[c, :, 8:16, :], ogst, R=[kogst], W=[])

        for dr in range(2):
            order = list(range(NT)) if dr == 0 else list(range(NT - 1, -1, -1))
            if dr == 1:
                MEMSET("dve", Sst, 0.0, Sk)
                MEMSET("dve", Sbf, 0.0, Sbk)
            ogst_box = [None, None]
            fr = gla_front(order[0], dr)
            for ix, c in enumerate(order):
                nxt = gla_front(order[ix + 1], dr) if ix + 1 < NT else None
                fr["ogst"] = ogst_box
                fr["first_in_group"] = (ix % NQG == 0)
                fr["last_in_group"] = (ix % NQG == NQG - 1)
                gla_back(fr)
                fr = nxt
        S.barrier()
        A.release(m_base)

        emit_precast_experts(cfg.K1, KPRE)
        Wbig = A.alloc([KC, D], BF16)
        U = A.alloc([KC, T], BF16)
        m5 = A.mark()
        for blk in range(4):
            load_w("pool", Wbig[:, :, blk * 512:(blk + 1) * 512], w_out, blk * 512, 512, ("Wbig", blk))
        load_gain(g_cross)
        xt_ring = Ring(A, "xt", 3, [D], F32)
        xs_ring = Ring(A, "xs", 2, [D], BF16)
        aT_ring = Ring(A, "aT", 2, [KC, 128], BF16)
        p5 = {}

        def p5_A(i):
            tsl = slice(i * 128, (i + 1) * 128)
            aT, kaT = aT_ring.next()
            DMA("sp", aT, A_d[i], R=[], W=[(kaT, 0), (kaT, 1)])
            xt, kx = xt_ring.next()
            DMA("sp", xt, x_own[tsl, :], W=[kx])
            for blk in range(4):
                p, kp = PS()
                for fc in range(KC):
                    MM(p[:, :], aT[:, fc, :], Wbig[:, fc, blk * 512:(blk + 1) * 512], fc == 0, fc == KC - 1,
                       [(kaT, 0), (kaT, 1), ("Wbig", blk)], [kp])
                TT("dve", xt[:, blk * 512:(blk + 1) * 512], p[:, :], xt[:, blk * 512:(blk + 1) * 512], ALU.add,
                   [kp, kx], [kx])
            DMA("act", h_d[tsl, :], xt, R=[kx], W=[("h_d", i)])
            p5[i] = (xt, kx)

        p5_A(0)
        for i in range(NT):
            xt, kx = p5.pop(i)
            xs, kxs = norm_pre(xt, kx, xs_ring)
            if i + 1 < NT:
                p5_A(i + 1)
            norm_post(xs, kxs, U, i * 128, ("U", i))
        S.barrier()
        A.release(m5)

        Q = Wbig.rearrange("p k n -> p (k n)")[:, 0:KC * T].rearrange("p (k t) -> p k t", t=T)
        memT = A.alloc([KC, MEM], BF16)
        kcT = A.alloc([KC, MEM], BF16)
        vc = A.alloc([2, D], BF16)
        m6 = A.mark()
        load_gain(g_mem)
        xt_ring = Ring(A, "xt", 2, [D], F32)
        xs_ring = Ring(A, "xs", 2, [D], BF16)
        for mt in range(2):
            xt, kx = xt_ring.next()
            DMA("sp", xt, mem[mt * 128:(mt + 1) * 128, :], W=[kx])
            norm_T(xt, kx, memT, mt * 128, ("memT", mt), xs_ring)
        S.barrier()
        A.release(m6)
        W6 = Ring(A, "W6", 3, [KC, 256], BF16)
        mk = [("memT", 0), ("memT", 1)]
        for blk in range(8):
            Wt, kW = W6.next()
            load_w("pool", Wt, w_ck, blk * 256, 256, kW)
            for jl in range(2):
                p, kp = PS()
                for kc in range(KC):
                    MM(p[:, 0:MEM], Wt[:, kc, jl * 128:(jl + 1) * 128], memT[:, kc, :], kc == 0, kc == KC - 1,
                       [kW] + mk, [kp])
                CP(ev_eng(), kcT[:, blk * 2 + jl, :], p[:, 0:MEM], [kp], [("kcT", blk * 2 + jl)])
        for blk in range(8):
            Wt, kW = W6.next()
            load_w("pool", Wt, w_cv, blk * 256, 256, kW)
            for mt in range(2):
                p, kp = PS()
                for kc in range(KC):
                    MM(p[:, 0:256], memT[:, kc, mt * 128:(mt + 1) * 128], Wt[:, kc, :], kc == 0, kc == KC - 1,
                       [kW] + mk, [kp])
                CP(ev_eng(), vc[:, mt, blk * 256:(blk + 1) * 256], p[:, 0:256], [kp], [], Wm=[("vc", blk // 2)])
        sc_x = 1.0 / math.sqrt(512.0)
        U_all = [("U", i) for i in range(NT)]
        for blk in range(8):
            Wt, kW = W6.next()
            load_w("pool", Wt, w_cq, blk * 256, 256, kW)
            for jl in range(2):
                j = blk * 2 + jl
                for tg in range(NTG):
                    p, kp = PS()
                    for kc in range(KC):
                        MM(p[:, 0:GS], Wt[:, kc, jl * 128:(jl + 1) * 128], U[:, kc, tg * GS:(tg + 1) * GS],
                           kc == 0, kc == KC - 1, [kW] + U_all[tg * GS // 128:(tg + 1) * GS // 128], [kp])
                    if ev_eng() == "act":
                        ACT(Q[:, j, tg * GS:(tg + 1) * GS], p[:, 0:GS], AF.Copy, [kp], [("Q", j, tg)], scale=sc_x)
                    else:
                        TS("dve", Q[:, j, tg * GS:(tg + 1) * GS], p[:, 0:GS], sc_x, ALU.mult, [kp], [("Q", j, tg)])
        S.barrier()
        A.release(m6)
        PT_ring = Ring(A, "PTx", 4, [GS], BF16)
        rden_ring = Ring(A, "rdenx", 2, [GS], F32)
        for hd in range(4):
            for tg in range(NTG):
                PTs = []
                for mt in range(2):
                    p, kp = PS()
                    for cch in range(4):
                        j = hd * 4 + cch
                        MM(p[:, 0:GS], kcT[:, j, mt * 128:(mt + 1) * 128], Q[:, j, tg * GS:(tg + 1) * GS],
                           cch == 0, cch == 3, [("kcT", j), ("Q", j, tg)], [kp])
                    PT, kPT = PT_ring.next()
                    ACT(PT, p[:, 0:GS], AF.Exp, [kp], [kPT])
                    PTs.append((PT, kPT))
                pd, kpd = PS()
                for mt in range(2):
                    MM(pd[:, 0:GS], ones_bf, PTs[mt][0], mt == 0, mt == 1, ["ones_bf", PTs[mt][1]], [kpd])
                rden, krden = rden_ring.next()
                RECIP(rden, pd[:, 0:GS], [kpd], [krden])
                for cch in range(4):
                    j = hd * 4 + cch
                    po, kpo = PS()
                    for mt in range(2):
                        MM(po[:, 0:GS], vc[:, mt, j * 128:(j + 1) * 128], PTs[mt][0], mt == 0, mt == 1,
                           [("vc", j // 4), PTs[mt][1]], [kpo])
                    TT("dve", U[:, j, tg * GS:(tg + 1) * GS], po[:, 0:GS], rden, ALU.mult, [kpo, krden],
                       [("oc", j, tg)])
        S.barrier()
        A.release(m5)
        for blk in range(4):
            load_w("pool", Wbig[:, :, blk * 512:(blk + 1) * 512], w_co, blk * 512, 512, ("Wbig", blk))
        load_gain(g_ffn)
        Wr = A.alloc([KC, NR], BF16)
        Wrf = A.alloc([KC, NR], F32)
        DMA("sp", Wrf, w_r.rearrange("(k p) n -> p k n", p=128), W=["Wrf"])
        CP("dve", Wr, Wrf, ["Wrf"], ["Wr"])
        brb = A.alloc([NR], F32)
        DMA("sp", brb, b_r.partition_broadcast(128), W=["brb"])
        ecb = A.alloc([NE], F32)
        DMA("sp", ecb, c_ec.partition_broadcast(128), W=["ecb"])
        L_all = A.alloc([NT, NR], F32)
        idxf_all = A.alloc([NT * 2], F32)
        m6c = A.mark()
        xt_ring = Ring(A, "xt", 3, [D], F32)
        xs_ring = Ring(A, "xs", 2, [D], BF16)
        hT_ring = Ring(A, "hT", 2, [KC, 128], BF16)
        c6 = {}

        def c6_A(i):
            tsl = slice(i * 128, (i + 1) * 128)
            xt, kx = xt_ring.next()
            DMA("sp", xt, h_d[tsl, :], R=[("h_d", i)], W=[kx])
            for blk in range(4):
                p, kp = PS()
                for fc in range(KC):
                    MM(p[:, :], U[:, fc, tsl], Wbig[:, fc, blk * 512:(blk + 1) * 512], fc == 0, fc == KC - 1,
                       [("oc", fc, i * 128 // GS), ("Wbig", blk)], [kp])
                TT("dve", xt[:, blk * 512:(blk + 1) * 512], p[:, :], xt[:, blk * 512:(blk + 1) * 512], ALU.add,
                   [kp, kx], [kx])
            DMA("act", h_d[tsl, :], xt, R=[kx], W=[("h_d", i)])
            c6[i] = (xt, kx)

        def c6_Bpre(i):
            xt, kx = c6.pop(i)
            hn, khn = norm_pre(xt, kx, xs_ring)
            DMA("act", Hn_d[i * 128:(i + 1) * 128, :], hn, R=[khn], W=[])
            c6[("hn", i)] = (hn, khn)

        def c6_B(i):
            hn, khn = c6.pop(("hn", i))
            hT, khT = hT_ring.next()
            norm_post(hn, khn, hT, 0, khT)
            pr, kpr = PS()
            for kc in range(KC):
                MM(pr[:, 0:NR], hT[:, kc, :], Wr[:, kc, :], kc == 0, kc == KC - 1, [khT, "Wr"], [kpr])
            TT("dve", L_all[:, i, :], pr[:, 0:NR], brb, ALU.add, [kpr, "brb"], [("L_all", i)])

        c6_A(0)
        for i in range(NT):
            c6_Bpre(i)
            if i + 1 < NT:
                c6_A(i + 1)
            c6_B(i)
        S.barrier()
        A.release(m6c)

        NTE = NT * NE
        assert NTE <= 512
        tcnt = [0]

        def tmp(shape):
            tcnt[0] += 1
            return A.alloc(shape, F32), ("rt", tcnt[0])

        Lg = L_all[:, :, 0:NG]
        Le = L_all[:, :, NG:NR]
        gmax, kgmax = tmp([NT])
        RMAX(gmax, Lg, AX.X, [], [kgmax])
        gmb = gmax.unsqueeze(2).to_broadcast([128, NT, NG])
        eg, keg = tmp([NT, NG])
        TT("dve", eg, Lg, gmb, ALU.subtract, [kgmax], [keg])
        ACT(eg, eg, AF.Exp, [keg], [(keg, 1)])
        gsum, kgsum = tmp([NT])
        RSUM(gsum, eg, AX.X, [(keg, 1)], [kgsum])
        pgrp, kpgrp = tmp([NT])
        RECIP(pgrp, gsum, [kgsum], [kpgrp])
        pen, kpen = tmp([NT, NG])
        TT("dve", pen, Lg, gmb, ALU.is_equal, [kgmax], [kpen])
        TS("dve", pen, pen, 1.0, ALU.subtract, [kpen], [(kpen, 1)], s2=1e30, op1=ALU.mult)
        EL, kEL = tmp([NT, NE])
        TT("dve", EL.rearrange("p t (g e) -> p t g e", e=8), Le.rearrange("p t (g e) -> p t g e", e=8),
           pen.unsqueeze(3).to_broadcast([128, NT, NG, 8]), ALU.add, [(kpen, 1)], [kEL])
        m1, km1 = tmp([NT])
        RMAX(m1, EL, AX.X, [kEL], [km1])
        oh1, koh1 = tmp([NT, NE])
        TT("dve", oh1, EL, m1.unsqueeze(2).to_broadcast([128, NT, NE]), ALU.is_equal, [kEL, km1], [koh1])
        EL2, kEL2 = tmp([NT, NE])
        STT(EL2.rearrange("p t e -> p (t e)"), oh1.rearrange("p t e -> p (t e)"), -1e30,
            EL.rearrange("p t e -> p (t e)"), ALU.mult, ALU.add, [koh1, kEL], [kEL2])
        m2_, km2 = tmp([NT])
        RMAX(m2_, EL2, AX.X, [kEL2], [km2])
        oh2, koh2 = tmp([NT, NE])
        TT("dve", oh2, EL2, m2_.unsqueeze(2).to_broadcast([128, NT, NE]), ALU.is_equal, [kEL2, km2], [koh2])
        e2, ke2 = tmp([NT])
        TT("dve", e2, m2_, m1, ALU.subtract, [km1, km2], [ke2])
        ACT(e2, e2, AF.Exp, [ke2], [(ke2, 1)])
        rr, krr = tmp([NT])
        TS("dve", rr, e2, 1.0, ALU.add, [(ke2, 1)], [krr])
        RECIP(rr, rr, [krr], [(krr, 1)])
        w3 = w_all.rearrange("p (t k) -> p t k", k=2)
        TT("dve", w3[:, :, 0], pgrp, rr, ALU.mult, [kpgrp, (krr, 1)], [("w_all", 0)])
        TT("dve", w3[:, :, 1], pgrp, w3[:, :, 0], ALU.subtract, [kpgrp, ("w_all", 0)], [("w_all", 1)])
        Ab, kAb = tmp([NT, NE])
        TT("dve", Ab, oh1, oh2, ALU.add, [koh1, koh2], [kAb])
        Ab2 = Ab.rearrange("p t e -> p (t e)")
        pk_, kpk_ = PS()
        MM(pk_[:, 0:NTE], LT, Ab2, True, True, ["tri", kAb], [kpk_])
        pc_, kpc_ = PS()
        MM(pc_[:, 0:NTE], onesF, Ab2, True, True, ["onesF", kAb], [kpc_])
        cnt, kcnt = tmp([NT, NE])
        CP("act", cnt.rearrange("p t e -> p (t e)"), pc_[:, 0:NTE], [kpc_], [kcnt])
        base, kbase = tmp([NT, NE])
        MEMSET("dve", base[:, 0, :], 0.0, [(kbase, 0)])
        for t in range(1, NT):
            TT("dve", base[:, t, :], base[:, t - 1, :], cnt[:, t - 1, :], ALU.add, [(kbase, t - 1), kcnt], [(kbase, t)])
        kbase_all = [(kbase, t) for t in range(NT)]
        RE, kRE = tmp([NT, NE])
        TT("dve", RE.rearrange("p t e -> p (t e)"), pk_[:, 0:NTE], base.rearrange("p t e -> p (t e)"), ALU.add,
           [kpk_] + kbase_all, [kRE])
        REc, kREc = tmp([NT, NE])
        TT("dve", REc, RE, ecb.unsqueeze(1).to_broadcast([128, NT, NE]), ALU.add, [kRE, "ecb"], [kREc])
        ix3 = idxf_all.rearrange("p (t k) -> p t k", k=2)
        ii3 = idx_all.rearrange("p (t k) -> p t k", k=2)
        for k, (oh, koh) in enumerate(((oh1, koh1), (oh2, koh2))):
            t1, kt1 = tmp([NT, NE])
            TT("dve", t1, oh, REc, ALU.mult, [koh, kREc], [kt1])
            ix, kix = tmp([NT])
            RSUM(ix, t1, AX.X, [kt1], [kix])
            TT("dve", t1, oh, RE, ALU.mult, [koh, kRE, kix], [(kt1, 1)])
            rsel, krsel = tmp([NT])
            RSUM(rsel, t1, AX.X, [(kt1, 1)], [krsel])
            TS("dve", rsel, rsel, float(C) - 0.5, ALU.is_gt, [krsel], [(krsel, 1)], s2=1e7, op1=ALU.mult)
            TT("dve", ix3[:, :, k], ix, rsel, ALU.add, [kix, (krsel, 1)], [("idxf", k)])
            CP("dve", ii3[:, :, k], ix3[:, :, k], [("idxf", k)], [("idx_all", k)])
        hn_ring = Ring(A, "hnr", 3, [D], BF16)
        for i in range(NT):
            hn, khn = hn_ring.next()
            DMA("sp", hn, Hn_d[i * 128:(i + 1) * 128, :], W=[khn])
            for k in range(2):
                S.op("pool", (lambda hn_, ixap: (lambda e: e.indirect_dma_start(
                    out=Xs_d, out_offset=bass.IndirectOffsetOnAxis(ap=ixap, axis=0), in_=hn_, in_offset=None,
                    bounds_check=BC(e), oob_is_err=False)))(hn, idx_all[:, 2 * i + k:2 * i + k + 1]),
                    R=[khn, ("idx_all", k)], W=[], dma=True)
        if debug:
            DMA("sp", dbg_idx, idxf_all, R=[("idxf", k) for k in range(2)])
            DMA("sp", dbg_w, w_all, R=[("w_all", k) for k in range(2)])
        S.barrier()
        A.release(m_base)

        CT = C // 128
        Wx = Ring(A, "Wx", 8, [KC, 512], BF16)
        xtm_ring = Ring(A, "xtm", 2, [CT, D], BF16)
        xsT_ring = Ring(A, "xsT", 2, [KC, C], BF16)
        hid_ring = Ring(A, "hid", 2, [8, C], BF16)
        sg_ring = Ring(A, "sg", 2, [C], F32)
        y_ring = Ring(A, "ysb", 2, [D], BF16)
        for e_ in range(NE):
            Wg2, Wu2, Wd2 = [], [], []
            for hf in range(2):
                wg_, kg_ = Wx.next()
                wu_, ku_ = Wx.next()
                if e_ < KPRE:
                    DMA("sp", wg_, wg_b[e_].rearrange("p (k n) -> p k n", n=1024)[:, :, hf * 512:(hf + 1) * 512],
                        R=[("pre_g", e_)], W=[kg_])
                    DMA("sp", wu_, wu_b[e_].rearrange("p (k n) -> p k n", n=1024)[:, :, hf * 512:(hf + 1) * 512],
                        R=[("pre_u", e_)], W=[ku_])
                else:
                    DMA("pool", wg_, w_gate[e_][:, hf * 512:(hf + 1) * 512].rearrange("(k p) n -> p k n", p=128), W=[kg_])
                    DMA("pool", wu_, w_up[e_][:, hf * 512:(hf + 1) * 512].rearrange("(k p) n -> p k n", p=128), W=[ku_])
                Wg2.append((wg_, kg_))
                Wu2.append((wu_, ku_))
            for hf in range(2):
                wd_, kd_ = Wx.next()
                wdv = wd_.rearrange("p k n -> p (k n)").rearrange("p (k n) -> p k n", n=D)
                if e_ < KPRE:
                    DMA("sp", wdv, wd_b[e_].rearrange("p (k n) -> p k n", n=D)[:, hf * 4:(hf + 1) * 4, :],
                        R=[("pre_d", e_)], W=[kd_])
                else:
                    DMA("pool", wdv, w_down[e_][hf * 512:(hf + 1) * 512, :].rearrange("(k p) n -> p k n", p=128), W=[kd_])
                Wd2.append((wdv, kd_))
            if e_ == 0:
                xtm_n = xtm_ring.next()
                DMA("act", xtm_n[0], Xs_d[0:C, :].rearrange("(a p) d -> p a d", p=128), W=[xtm_n[1]])
            xtm, kxtm = xtm_n
            if e_ + 1 < NE:
                xtm_n = xtm_ring.next()
                DMA("act", xtm_n[0], Xs_d[(e_ + 1) * C:(e_ + 2) * C, :].rearrange("(a p) d -> p a d", p=128),
                    W=[xtm_n[1]])
            xsT, kxsT = xsT_ring.next()
            for r in range(CT):
                for b in range(2):
                    pt, kp = PS()
                    pb = pt[:, :].bitcast(BF16)
                    for j in range(8):
                        kc = b * 8 + j
                        TR(pb[:, j * 128:(j + 1) * 128], xtm[:, r, kc * 128:(kc + 1) * 128], [kxtm, "ident"], [kp])
                    CP(ev_eng(), xsT[:, b * 8:(b + 1) * 8, r * 128:(r + 1) * 128],
                       pb.rearrange("p (a b) -> p a b", b=128), [kp], [(kxsT, r, b)])
            kxs_all = [(kxsT, r, b) for r in range(CT) for b in range(2)]
            hid, khid = hid_ring.next()
            for j in range(8):
                pg, kpg = PS()
                Wg, kWg = Wg2[j // 4]
                Wu, kWu = Wu2[j // 4]
                jl = j % 4
                for kc in range(KC):
                    MM(pg[:, 0:C], Wg[:, kc, jl * 128:(jl + 1) * 128], xsT[:, kc, :], kc == 0, kc == KC - 1,
                       [kWg] + kxs_all, [kpg])
                pu, kpu = PS()
                for kc in range(KC):
                    MM(pu[:, 0:C], Wu[:, kc, jl * 128:(jl + 1) * 128], xsT[:, kc, :], kc == 0, kc == KC - 1,
                       [kWu] + kxs_all, [kpu])
                sg, ksg = sg_ring.next()
                ACT(sg, pg[:, 0:C], AF.Silu, [kpg], [ksg])
                TT("dve", hid[:, j, :], pu[:, 0:C], sg, ALU.mult, [kpu, ksg], [(khid, j)])
            khid_all = [(khid, j) for j in range(8)]
            for r in range(CT):
                ysb, ky = y_ring.next()
                for blk in range(4):
                    p, kp = PS()
                    for j in range(8):
                        MM(p[:, :], hid[:, j, r * 128:(r + 1) * 128], Wd2[j // 4][0][:, j % 4, blk * 512:(blk + 1) * 512],
                           j == 0, j == 7, khid_all + [Wd2[j // 4][1]], [kp])
                    CP(ev_eng(), ysb[:, blk * 512:(blk + 1) * 512], p[:, :], [kp], [], Wm=[ky])
                DMA("act", Y_d[e_ * C + r * 128:e_ * C + (r + 1) * 128, :], ysb, R=[ky], W=[])
        S.barrier()
        A.release(m_base)

        load_gain(g_final)
        xt_ring = Ring(A, "xt", 2, [D], F32)
        y1_ring = Ring(A, "y1", 3, [D], BF16)
        y2_ring = Ring(A, "y2", 3, [D], BF16)
        o_ring = Ring(A, "o", 2, [D], F32)
        for i in range(NT):
            tsl = slice(i * 128, (i + 1) * 128)
            xt, kx = xt_ring.next()
            DMA("sp", xt, h_d[tsl, :], W=[kx])
            ys = []
            for k, ring in enumerate((y1_ring, y2_ring)):
                yb, kyb = ring.next()
                S.op("pool", (lambda yb_, ixap: (lambda e: e.indirect_dma_start(
                    out=yb_, out_offset=None, in_=Y_d, in_offset=bass.IndirectOffsetOnAxis(ap=ixap, axis=0),
                    bounds_check=BC(e), oob_is_err=False)))(yb, idx_all[:, 2 * i + k:2 * i + k + 1]),
                    R=[], W=[kyb], dma=True)
                ys.append((yb, kyb))
            STT(xt, ys[0][0], w_all[:, 2 * i:2 * i + 1], xt, ALU.mult, ALU.add, [ys[0][1], kx], [kx])
            STT(xt, ys[1][0], w_all[:, 2 * i + 1:2 * i + 2], xt, ALU.mult, ALU.add, [ys[1][1], kx], [kx])
            ss, kss = small.next()
            rs, krs = small.next()
            rstd, krstd = small.next()
            ACT(junk, xt, AF.Square, [kx], [kss], accum=ss)
            ACT(rs, ss, AF.Sqrt, [kss], [krs], bias=EPS, scale=1.0 / D)
            RECIP(rstd, rs, [krs], [krstd])
            ot, ko = o_ring.next()
            STT(ot, xt, rstd, gainb, ALU.mult, ALU.mult, [kx, krstd, "gainb"], [ko])
            DMA("act", out[tsl, :], ot, R=[ko], W=[("out", i)])
        S.barrier(final=True)
        S.emit()
        nc._marks = S.marks
    return nc


def _consts(cfg):
    s = np.arange(128)[:, None]
    t = np.arange(128)[None, :]
    tri = np.stack([(s <= t), (s >= t), (s > t), (s < t)], axis=1).astype(np.float32)
    slopes = 2.0 ** (-8.0 * (np.arange(8, dtype=np.float64) + 1.0) / 8.0)
    k = np.arange(128, dtype=np.float64)[:, None]
    q = np.arange(128, dtype=np.float64)[None, :]
    am = np.zeros((128, 3, 8, 128), np.float64)
    for pos in range(3):
        if pos == 0:
            dist = q + 128 - k
        elif pos == 1:
            dist = np.abs(q - k)
        else:
            dist = k + 128 - q
        valid = (dist <= 128)
        for h in range(8):
            am[:, pos, h, :] = np.where(valid, -slopes[h] * dist, -30000.0)
    ec = (np.arange(cfg.NE, dtype=np.float32) * cfg.C)[None, :]
    return dict(c_ident=np.eye(128, dtype=np.float32), c_tri=np.ascontiguousarray(tri),
                c_amask=np.ascontiguousarray(am.reshape(128, -1).astype(np.float32)), c_ec=ec)


def make_in_maps(inputs, cfg, n_cores):
    f32 = lambda a: np.ascontiguousarray(np.asarray(a, dtype=np.float32))
    T = cfg.T
    x = f32(inputs["x"])
    mem = f32(inputs["mem"])
    w_in = f32(inputs["w_in"][0])
    w_in_sw = np.concatenate([w_in[:, :4608], w_in[:, 4624:4640], w_in[:, 4608:4624]], axis=1)
    w_in_sw = np.ascontiguousarray(w_in_sw)
    wgf = f32(np.concatenate([inputs["w_gla_gf"][0], inputs["b_gla_gf"][0][None, :]], axis=0))
    wgb = f32(np.concatenate([inputs["w_gla_gb"][0], inputs["b_gla_gb"][0][None, :]], axis=0))
    shared = dict(
        g_mix=f32(inputs["norm_mix"][0][None, :]), g_cross=f32(inputs["norm_cross"][0][None, :]),
        g_mem=f32(inputs["norm_mem"][0][None, :]), g_ffn=f32(inputs["norm_ffn"][0][None, :]),
        g_final=f32(inputs["norm_final"][None, :]),
        ga_col=f32(np.asarray(inputs["attn_out_norm"][0]).reshape(8, 128).T),
        sink=f32(inputs["sink_logit"][0][None, :]),
        gg_col=f32(np.asarray(inputs["gla_out_norm"][0]).reshape(2, 128).T),
        w_out=f32(inputs["w_out"][0]), w_cq=f32(inputs["w_cq"][0]), w_ck=f32(inputs["w_ck"][0]),
        w_cv=f32(inputs["w_cv"][0]), w_co=f32(inputs["w_co"][0]),
        w_r=f32(np.concatenate([inputs["w_router_group"][0], inputs["w_router_expert"][0]], axis=1)),
        b_r=f32(np.concatenate([inputs["b_router_group"][0], inputs["b_router_expert"][0]])[None, :]),
        w_gate=f32(inputs["w_gate"][0]), w_up=f32(inputs["w_up"][0]), w_down=f32(inputs["w_down"][0]),
    )
    shared.update(_consts(cfg))
    maps = []
    for c in range(n_cores):
        b, half = c // 2, c % 2
        m = dict(shared)
        if half == 1:
            m["x_own"] = np.ascontiguousarray(x[b, T:2 * T])
            m["x_oth"] = np.ascontiguousarray(x[b, 0:T])
            m["w_in"] = w_in
            m["wgP"], m["wgM"] = wgf, wgb
        else:
            m["x_own"] = np.ascontiguousarray(x[b, 0:T][::-1])
            m["x_oth"] = np.ascontiguousarray(x[b, T:2 * T][::-1])
            m["w_in"] = w_in_sw
            m["wgP"], m["wgM"] = wgb, wgf
        m["mem"] = mem[b]
        maps.append(m)
    return maps


def assemble(results, cfg, n_cores, key="out"):
    T = cfg.T
    B = n_cores // 2
    outp = np.empty((B, 2 * T, D), np.float32)
    for c in range(n_cores):
        b, half = c // 2, c % 2
        o = np.asarray(results[c][key])
        if half == 1:
            outp[b, T:2 * T] = o
        else:
            outp[b, 0:T] = o[::-1]
    return outp


_NC_CACHE = {}


def kernel(**inputs):
    cfg = Cfg()
    if "nc" not in _NC_CACHE:
        _NC_CACHE["nc"] = build(cfg)
    nc = _NC_CACHE["nc"]
    in_maps = make_in_maps(inputs, cfg, 8)
    res = run_bass_kernel_spmd(nc, in_maps, core_ids=list(range(8)))
    return assemble(res.results, cfg, 8)
```

```python
import math
from contextlib import ExitStack

import numpy as np
import concourse.bass as bass
import concourse.mybir as mybir
from concourse.bass_utils import run_bass_kernel_spmd

F32 = mybir.dt.float32
BF16 = mybir.dt.bfloat16
I32 = mybir.dt.int32
AF = mybir.ActivationFunctionType
ALU = mybir.AluOpType
AX = mybir.AxisListType

D = 2048
KC = 16
MEM = 256
IN_COLS = 4640
EPS = 1e-6
NDSEM = 8


class Cfg:
    def __init__(self, T=2048, NE=32, C=256, KPRE=12, K1=12):
        self.KPRE = min(KPRE, NE)
        self.K1 = min(K1, self.KPRE)
        self.T = T
        self.NE = NE
        self.C = C
        self.NG = NE // 8
        self.NT = T // 128
        self.GS = min(512, T)
        self.NTG = T // self.GS


class Sched:
    ENG = ("pe", "act", "dve", "pool", "sp")
    DQ = ("sp", "act", "pool")

    def __init__(self, nc, es):
        self.nc = nc
        self.csem = {e: es.enter_context(nc.semaphore("c_" + e)) for e in self.ENG}
        self.ccnt = {e: 0 for e in self.ENG}
        self.nds = {"sp": 24, "act": 12, "pool": 8}
        self.dsem = {q: [es.enter_context(nc.semaphore("d_%s_%d" % (q, i))) for i in range(self.nds[q])]
                     for q in self.DQ}
        self.dcnt = {q: 0 for q in self.DQ}
        self.bgsem = [es.enter_context(nc.semaphore("bg_%d" % i)) for i in range(NDSEM)]
        self.bgcnt = 0
        self.bgw = {}
        self.prog = {e: [] for e in self.ENG}
        self.seen = {e: {} for e in self.ENG}
        self.lastw = {}
        self.readers = {}
        self.multi = {}
        self.genw = {}

    def _need(self, eng, toks, is_pe):
        waits = {}
        for t in toks:
            if t is None:
                continue
            sem, val, kind, src = t
            if is_pe and kind == "c" and src == "pe":
                continue
            k = id(sem)
            if self.seen[eng].get(k, 0) >= val:
                continue
            if k not in waits or waits[k][1] < val:
                waits[k] = (sem, val)
        for k, (sem, val) in waits.items():
            self.seen[eng][k] = val
        return list(waits.values())

    def op(self, eng, fn, R=(), W=(), dma=False, bg=False, Wm=()):
        deps = []
        for r in R:
            deps.append(self.lastw.get(r))
            deps.append(self.bgw.get(r))
            deps.extend(self.multi.get(r, ()))
        for w in W:
            deps.append(self.lastw.get(w))
            deps.extend(self.multi.get(w, ()))
            deps.extend(self.readers.get(w, ()))
        for w in Wm:
            rd = self.readers.get(w, ())
            if rd or self.lastw.get(w) is not None:
                self.genw[w] = list(rd) + [self.lastw.get(w)] + list(self.multi.get(w, ()))
                self.multi[w] = []
                self.lastw[w] = None
                self.readers[w] = []
            deps.extend(self.genw.get(w, ()))
        if bg:
            n = self.bgcnt
            self.bgcnt += 1
            slot = n % NDSEM
            sem = self.bgsem[slot]
            if n >= NDSEM:
                deps.append((sem, 16 * (n // NDSEM), "b", eng))
            tok = (sem, 16 * (n // NDSEM + 1), "b", eng)
            inc = (sem, 16)
            waits = self._need(eng, deps, False)
            self.prog[eng].append((waits, fn, inc))
            for w in W:
                self.bgw[w] = tok
            return tok
        if dma:
            n = self.dcnt[eng]
            self.dcnt[eng] += 1
            nd = self.nds[eng]
            slot = n % nd
            sem = self.dsem[eng][slot]
            if n >= nd:
                deps.append((sem, 16 * (n // nd), "d", eng))
            tok = (sem, 16 * (n // nd + 1), "d", eng)
            inc = (sem, 16)
        else:
            self.ccnt[eng] += 1
            tok = (self.csem[eng], self.ccnt[eng], "c", eng)
            inc = (self.csem[eng], 1)
        waits = self._need(eng, deps, eng == "pe" and not dma)
        self.prog[eng].append((waits, fn, inc))
        for r in R:
            self.readers.setdefault(r, []).append(tok)
        for w in W:
            self.lastw[w] = tok
            self.readers[w] = []
            self.multi[w] = []
            self.genw[w] = []
        for w in Wm:
            self.multi.setdefault(w, []).append(tok)
        return tok

    def barrier(self, final=False):
        self.marks = getattr(self, "marks", [])
        self.marks.append(dict(self.ccnt))
        toks = []
        if final:
            n = self.bgcnt
            for slot in range(NDSEM):
                k = (n - slot + NDSEM - 1) // NDSEM if n > slot else 0
                if k > 0:
                    toks.append((self.bgsem[slot], 16 * k, "b", "pool"))
        for e in self.ENG:
            if self.ccnt[e] > 0:
                toks.append((self.csem[e], self.ccnt[e], "c", e))
        for q in self.DQ:
            n = self.dcnt[q]
            nd = self.nds[q]
            for slot in range(nd):
                k = (n - slot + nd - 1) // nd if n > slot else 0
                if k > 0:
                    toks.append((self.dsem[q][slot], 16 * k, "d", q))
        for e in self.ENG:
            waits = self._need(e, toks, False)
            if waits:
                self.prog[e].append((waits, None, None))
        self.lastw.clear()
        self.readers.clear()
        self.multi.clear()
        self.genw.clear()

    def emit(self):
        prog = self.prog

        def run(name, eng):
            for waits, fn, inc in prog[name]:
                for sem, val in waits:
                    eng.wait_ge(sem, val)
                if fn is None:
                    continue
                fn(eng).then_inc(inc[0], inc[1])

        with self.nc.Block() as block:
            @block.tensor
            def _(e):
                run("pe", e)

            @block.scalar
            def _(e):
                run("act", e)

            @block.vector
            def _(e):
                run("dve", e)

            @block.gpsimd
            def _(e):
                run("pool", e)

            @block.sync
            def _(e):
                run("sp", e)


class Arena:
    def __init__(self, nc, es, nbytes):
        self.n = nbytes // 2
        self.t = es.enter_context(nc.sbuf_tensor("arena", [128, self.n], BF16))
        self.off = 0
        self.peak = 0

    def mark(self):
        return self.off

    def release(self, m):
        self.off = m

    def alloc(self, shape, dtype, parts=128):
        esz = 2 if dtype == BF16 else 4
        n = 1
        for s in shape:
            n *= s
        nb = (n * esz + 63) // 64 * 64
        a = self.off
        self.off += nb // 2
        assert self.off <= self.n, "SBUF arena overflow: %d > %d" % (self.off * 2, self.n * 2)
        self.peak = max(self.peak, self.off)
        v = self.t[0:parts, a:a + n * esz // 2]
        if dtype != BF16:
            v = v.bitcast(dtype)
        if len(shape) == 2:
            v = v.rearrange("p (a b) -> p a b", b=shape[1])
        elif len(shape) == 3:
            v = v.rearrange("p (a b c) -> p a b c", b=shape[1], c=shape[2])
        return v


class Ring:
    def __init__(self, A, name, n, shape, dtype, parts=128):
        self.bufs = [A.alloc(shape, dtype, parts) for _ in range(n)]
        self.name = name
        self.i = 0

    def next(self):
        k = self.i % len(self.bufs)
        self.i += 1
        return self.bufs[k], (self.name, k)


def build(cfg, debug=False):
    T, NE, C, NG, NT, GS, NTG = cfg.T, cfg.NE, cfg.C, cfg.NG, cfg.NT, cfg.GS, cfg.NTG
    NR = NG + NE
    NSLOT = NE * C
    nc = bass.Bass("TRN2", target_bir_lowering=False)

    def din(name, shape, dt=F32):
        return nc.dram_tensor(name, list(shape), dt, kind="ExternalInput").ap()

    skind = "ExternalOutput" if debug else "Internal"

    def dscr(name, shape, dt):
        return nc.dram_tensor(name, list(shape), dt, kind=skind).ap()

    x_own = din("x_own", [T, D])
    x_oth = din("x_oth", [T, D])
    mem = din("mem", [MEM, D])
    g_mix = din("g_mix", [1, D])
    g_cross = din("g_cross", [1, D])
    g_mem = din("g_mem", [1, D])
    g_ffn = din("g_ffn", [1, D])
    g_final = din("g_final", [1, D])
    w_in = din("w_in", [D, IN_COLS])
    ga_col = din("ga_col", [128, 8])
    sink = din("sink", [1, 8])
    wgP = din("wgP", [17, 512])
    wgM = din("wgM", [17, 512])
    gg_col = din("gg_col", [128, 2])
    w_out = din("w_out", [D, D])
    w_cq = din("w_cq", [D, D])
    w_ck = din("w_ck", [D, D])
    w_cv = din("w_cv", [D, D])
    w_co = din("w_co", [D, D])
    w_r = din("w_r", [D, NR])
    b_r = din("b_r", [1, NR])
    w_gate = din("w_gate", [NE, D, 1024])
    w_up = din("w_up", [NE, D, 1024])
    w_down = din("w_down", [NE, 1024, D])
    c_ident = din("c_ident", [128, 128])
    c_tri = din("c_tri", [128, 4, 128])
    c_amask = din("c_amask", [128, 3 * 8 * 128])
    c_ec = din("c_ec", [1, NE])

    out = nc.dram_tensor("out", [T, D], F32, kind="ExternalOutput").ap()

    kaT_d = dscr("kaT_d", [2, 128, T + 256], BF16)
    va_d = dscr("va_d", [T + 256, 256], BF16)
    qaT_d = dscr("qaT_d", [8, 128, T], BF16)
    qgT_d = dscr("qgT_d", [4, 128, T], BF16)
    kgT_d = dscr("kgT_d", [4, 128, T], BF16)
    kg_d = dscr("kg_d", [T, 512], BF16)
    vg_d = dscr("vg_d", [T, 1024], BF16)
    rgT_d = dscr("rgT_d", [8, 128, T], BF16)
    lP_d = dscr("lP_d", [T, 512], F32)
    lM_d = dscr("lM_d", [T, 512], F32)
    A_d = dscr("A_d", [NT, 128, 16, 128], BF16)
    h_d = dscr("h_d", [T, D], F32)
    Xs_d = dscr("Xs_d", [NSLOT, D], BF16)
    Hn_d = nc.dram_tensor("Hn_d", [T, D], BF16, kind="Internal").ap()
    Y_d = dscr("Y_d", [NSLOT, D], BF16)
    KPRE = cfg.KPRE
    w_in_b = nc.dram_tensor("w_in_b", [128, KC * IN_COLS], BF16, kind="Internal").ap()
    wbig_b = {}
    for nm in ("w_out", "w_cq", "w_ck", "w_cv", "w_co"):
        wbig_b[nm] = nc.dram_tensor(nm + "_b", [128, KC * D], BF16, kind="Internal").ap()
    wg_b = nc.dram_tensor("wg_b", [max(KPRE, 1), 128, KC * 1024], BF16, kind="Internal").ap()
    wu_b = nc.dram_tensor("wu_b", [max(KPRE, 1), 128, KC * 1024], BF16, kind="Internal").ap()
    wd_b = nc.dram_tensor("wd_b", [max(KPRE, 1), 128, 8 * D], BF16, kind="Internal").ap()
    if debug:
        dbg_S = dscr("dbg_S", [128, 1024], F32)
        dbg_idx = dscr("dbg_idx", [128, NT * 2], F32)
        dbg_w = dscr("dbg_w", [128, NT * 2], F32)

    with ExitStack() as es:
        S = Sched(nc, es)
        A = Arena(nc, es, 206 * 1024)
        psb = [es.enter_context(nc.psum_tensor("ps%d" % i, [128, 512], F32)) for i in range(8)]
        psi = [0]

        def PS():
            k = psi[0] % 8
            psi[0] += 1
            return psb[k], ("ps", k)

        def DMA(q, out_, in_, R=(), W=()):
            return S.op(q, lambda e: e.dma_start(out=out_, in_=in_), R, W, dma=True)

        def MM(out_, lhsT, rhs, start, stop, R, W):
            return S.op("pe", lambda e: e.matmul(out_, lhsT=lhsT, rhs=rhs, start=start, stop=stop), R, W)

        def TR(out_, in_, R, W):
            return S.op("pe", lambda e: e.transpose(out=out_, in_=in_, identity=ident), R, W)

        def ACT(out_, in_, func, R, W, bias=None, scale=None, accum=None, Wm=()):
            kw = {}
            if bias is not None:
                kw["bias"] = bias
            if scale is not None:
                kw["scale"] = scale
            if accum is not None:
                kw["accum_out"] = accum
            return S.op("act", lambda e: e.activation(out=out_, in_=in_, func=func, **kw), R, W, Wm=Wm)

        def TT(eng, out_, in0, in1, op, R, W):
            return S.op(eng, lambda e: e.tensor_tensor(out=out_, in0=in0, in1=in1, op=op), R, W)

        def TS(eng, out_, in0, s1, op0, R, W, s2=None, op1=None, Wm=()):
            if op1 is None:
                return S.op(eng, lambda e: e.tensor_scalar(out=out_, in0=in0, scalar1=s1, scalar2=None, op0=op0), R, W, Wm=Wm)
            return S.op(eng, lambda e: e.tensor_scalar(out=out_, in0=in0, scalar1=s1, scalar2=s2, op0=op0, op1=op1), R, W, Wm=Wm)

        def STT(out_, in0, scalar, in1, op0, op1, R, W):
            return S.op("dve", lambda e: e.scalar_tensor_tensor(out=out_, in0=in0, scalar=scalar, in1=in1, op0=op0, op1=op1), R, W)

        def CP(eng, out_, in_, R, W, Wm=()):
            if eng == "act":
                return ACT(out_, in_, AF.Copy, R, W, Wm=Wm)
            return S.op(eng, lambda e: e.tensor_copy(out=out_, in_=in_), R, W, Wm=Wm)

        def RECIP(out_, in_, R, W):
            return S.op("dve", lambda e: e.reciprocal(out=out_, in_=in_), R, W)

        def RMAX(out_, in_, axis, R, W):
            return S.op("dve", lambda e: e.tensor_reduce(out=out_, in_=in_, axis=axis, op=ALU.max), R, W)

        def RSUM(out_, in_, axis, R, W):
            return S.op("dve", lambda e: e.tensor_reduce(out=out_, in_=in_, axis=axis, op=ALU.add), R, W)

        bcreg = []

        def BC(e):
            if not bcreg:
                bcreg.append(e.to_reg(NSLOT - 1))
            return bcreg[0]

        def MEMSET(eng, ap, val, W):
            return S.op(eng, lambda e: e.memset(ap, val), (), W)

        identf = A.alloc([128], F32)
        ident = A.alloc([128], BF16)
        tri = A.alloc([4, 128], F32)
        ones_bf = A.alloc([128], BF16)
        ones_f = A.alloc([1], F32)
        onesF = A.alloc([128], F32)
        Sst = A.alloc([4, 256], F32)
        Sbf = A.alloc([4, 256], BF16)
        gainb = A.alloc([D], F32)
        idx_all = A.alloc([NT * 2], I32)
        w_all = A.alloc([NT * 2], F32)
        LE, GE, GT, LT = tri[:, 0, :], tri[:, 1, :], tri[:, 2, :], tri[:, 3, :]

        DMA("sp", identf, c_ident, W=["identf"])
        DMA("sp", tri, c_tri, W=["tri"])
        CP("dve", ident, identf, ["identf"], ["ident"])
        MEMSET("dve", ones_bf, 1.0, ["ones_bf"])
        MEMSET("dve", ones_f, 1.0, ["ones_f"])
        MEMSET("dve", onesF, 1.0, ["onesF"])
        MEMSET("dve", Sst, 0.0, ["Sst"])
        MEMSET("dve", Sbf, 0.0, ["Sbf"])

        small = Ring(A, "small", 6, [1], F32)
        junk = A.alloc([D], BF16)

        def load_gain(g_ap):
            DMA("sp", gainb, g_ap.partition_broadcast(128), W=["gainb"])

        def norm_pre(xt, kx, xs_ring):
            ss, kss = small.next()
            rs, krs = small.next()
            rstd, krstd = small.next()
            ACT(junk, xt, AF.Square, [kx], [kss], accum=ss)
            ACT(rs, ss, AF.Sqrt, [kss], [krs], bias=EPS, scale=1.0 / D)
            RECIP(rstd, rs, [krs], [krstd])
            xs, kxs = xs_ring.next()
            STT(xs, xt, rstd, gainb, ALU.mult, ALU.mult, [kx, krstd, "gainb"], [kxs])
            return xs, kxs

        def norm_post(xs, kxs, dstT, col0, kdst):
            for b in range(2):
                pt, kp = PS()
                pb = pt[:, :].bitcast(BF16)
                for j in range(8):
                    kc = b * 8 + j
                    TR(pb[:, j * 128:(j + 1) * 128], xs[:, kc * 128:(kc + 1) * 128], [kxs, "ident"], [kp])
                CP("act" if b == 0 else "dve", dstT[:, b * 8:(b + 1) * 8, col0:col0 + 128],
                   pb.rearrange("p (a b) -> p a b", b=128), [kp], [], Wm=[kdst])

        def norm_T(xt, kx, dstT, col0, kdst, xs_ring, hn_out=None):
            xs, kxs = norm_pre(xt, kx, xs_ring)
            norm_post(xs, kxs, dstT, col0, kdst)
            return xs, kxs

        def load_w(q, dst, src2d, c0, ncol, kdst):
            DMA(q, dst, src2d[:, c0:c0 + ncol].rearrange("(k p) n -> p k n", p=128), W=[kdst])

        def BGDMA(out_, in_, W):
            return S.op("pool", lambda e: e.dma_start(out=out_, in_=in_), (), W, bg=True)

        wq = [0]

        def load_wb(dst, wb, ncols_total, c0, ncol, kdst, kpre):
            q = "sp"
            DMA(q, dst, wb.rearrange("p (k n) -> p k n", n=ncols_total)[:, :, c0:c0 + ncol], R=[kpre], W=[kdst])

        def emit_precast_experts(e0, e1):
            for e_ in range(e0, min(e1, KPRE)):
                BGDMA(wg_b[e_].rearrange("p (k n) -> p k n", n=1024), w_gate[e_].rearrange("(k p) n -> p k n", p=128),
                      [("pre_g", e_)])
                BGDMA(wu_b[e_].rearrange("p (k n) -> p k n", n=1024), w_up[e_].rearrange("(k p) n -> p k n", p=128),
                      [("pre_u", e_)])
                BGDMA(wd_b[e_].rearrange("p (k n) -> p k n", n=D), w_down[e_].rearrange("(k p) n -> p k n", p=128),
                      [("pre_d", e_)])

        def emit_precast():
            for blk in range(0, IN_COLS, 1024):
                n = min(1024, IN_COLS - blk)
                BGDMA(w_in_b.rearrange("p (k n) -> p k n", n=IN_COLS)[:, :, blk:blk + n],
                      w_in[:, blk:blk + n].rearrange("(k p) n -> p k n", p=128), [("pre_w_in", blk // 1024)])
            emit_precast_experts(0, cfg.K1)

        m_base = A.mark()

        zt = A.alloc([D], BF16)
        MEMSET("pool", zt, 0.0, ["zt"])
        for r0 in range(0, NSLOT, 128):
            DMA("act", Xs_d[r0:r0 + 128, :], zt, R=["zt"], W=[])

        Wkg = A.alloc([KC, 512], BF16)
        Wvg = A.alloc([KC, 1024], BF16)
        Wlr = A.alloc([KC, 32], BF16)
        Wkva = A.alloc([KC, 512], BF16)
        wgP_sb = A.alloc([512], F32, parts=17)
        wgM_sb = A.alloc([512], F32, parts=17)
        load_w("pool", Wkg, w_in, 2048, 512, "Wkg")
        load_w("pool", Wvg[:, :, 0:512], w_in, 2560, 512, "Wvg0")
        load_w("pool", Wvg[:, :, 512:1024], w_in, 3072, 512, "Wvg1")
        load_w("pool", Wlr, w_in, 4608, 32, "Wlr")
        load_w("pool", Wkva, w_in, 1024, 512, "Wkva")
        DMA("sp", wgP_sb, wgP, W=["wgP"])
        DMA("sp", wgM_sb, wgM, W=["wgM"])
        load_gain(g_mix)
        emit_precast()

        xt_ring = Ring(A, "xt", 3, [D], F32)
        xs_ring = Ring(A, "xs", 2, [D], BF16)
        uTt_ring = Ring(A, "uTt", 3, [KC, 128], BF16)
        lrT_ring = Ring(A, "lrT", 3, [128], F32, parts=17)
        for b_, k_ in zip(lrT_ring.bufs, range(3)):
            MEMSET("dve", b_, 1.0, [("lrT", k_)])
        v_ring = Ring(A, "vsb", 3, [1024], BF16)
        ks_ring = Ring(A, "ksb", 3, [512], BF16)
        e1_ring = Ring(A, "e1", 2, [512], F32)
        l_ring = Ring(A, "l", 2, [512], F32)
        eK_ring = Ring(A, "eK", 2, [512], F32)
        kd_ring = Ring(A, "kd", 2, [512], BF16)
        dec_ring = Ring(A, "dec", 2, [4], F32)
        st_ring = Ring(A, "stg", 2, [512], BF16)

        def gate_l(lrT, klrT, wg_sb, kwg, l, kl):
            pz, kpz = PS()
            MM(pz[:, :], lrT, wg_sb, True, True, [klrT, kwg], [kpz])
            e1, ke1 = e1_ring.next()
            ACT(e1, pz[:, :], AF.Exp, [kpz], [ke1], scale=-1.0)
            ACT(l, e1, AF.Ln, [ke1], [kl], bias=1.0)

        p1 = {}

        def p1_N(i):
            xt, kx = xt_ring.next()
            DMA("sp", xt, x_oth[i * 128:(i + 1) * 128, :], W=[kx])
            uTt, ku = uTt_ring.next()
            norm_T(xt, kx, uTt, 0, ku, xs_ring)
            p1[i] = dict(uTt=uTt, ku=ku)

        def p1_Ak(i):
            c = p1[i]
            uTt, ku = c["uTt"], c["ku"]
            pk, kpk = PS()
            for kc in range(KC):
                MM(pk[:, :], uTt[:, kc, :], Wkg[:, kc, :], kc == 0, kc == KC - 1, [ku, "Wkg"], [kpk])
            ksb, kks = ks_ring.next()
            CP("dve", ksb, pk[:, :], [kpk], [kks])
            c.update(ksb=ksb, kks=kks)
            c["vsb"], c["kv"] = v_ring.next()

        def p1_Av(i, hf):
            c = p1[i]
            uTt, ku, vsb, kv = c["uTt"], c["ku"], c["vsb"], c["kv"]
            pv, kpv = PS()
            for kc in range(KC):
                MM(pv[:, :], uTt[:, kc, :], Wvg[:, kc, hf * 512:(hf + 1) * 512], kc == 0, kc == KC - 1,
                   [ku, "Wvg%d" % hf], [kpv])
            CP("act", vsb[:, hf * 512:(hf + 1) * 512], pv[:, :], [kpv], [(kv, hf)])

        def p1_Alr(i):
            c = p1[i]
            uTt, ku = c["uTt"], c["ku"]
            pl, kpl = PS()
            for kc in range(KC):
                MM(pl[0:16, 0:128], Wlr[:, kc, 0:16], uTt[:, kc, :], kc == 0, kc == KC - 1, [ku, "Wlr"], [kpl])
            lrT, klrT = lrT_ring.next()
            CP("dve", lrT[0:16, :], pl[0:16, 0:128], [kpl], [klrT])
            c.update(lrT=lrT, klrT=klrT)
            if i == NT - 1:
                for g in range(2):
                    pa, kpa = PS()
                    for kc in range(KC):
                        MM(pa[:, 0:128], Wkva[:, kc, g * 128:(g + 1) * 128], uTt[:, kc, :], kc == 0, kc == KC - 1,
                           [ku, "Wkva"], [kpa])
                    st, kst = st_ring.next()
                    CP("act", st[:, 0:128], pa[:, 0:128], [kpa], [kst])
                    DMA("act", kaT_d[g, :, 0:128], st[:, 0:128], R=[kst], W=[])
                pa, kpa = PS()
                for kc in range(KC):
                    MM(pa[:, 0:256], uTt[:, kc, :], Wkva[:, kc, 256:512], kc == 0, kc == KC - 1, [ku, "Wkva"], [kpa])
                st, kst = st_ring.next()
                CP("act", st[:, 0:256], pa[:, 0:256], [kpa], [kst])
                DMA("act", va_d[0:128, :], st[:, 0:256], R=[kst], W=[])

        def p1_A(i):
            p1_Ak(i)
            p1_Av(i, 0)
            p1_Av(i, 1)
            p1_Alr(i)

        def p1_B1(i):
            c = p1[i]
            c["l"], c["kl"] = l_ring.next()
            gate_l(c["lrT"], c["klrT"], wgP_sb, "wgP", c["l"], c["kl"])

        def p1_B2(i):
            c = p1[i]
            l, kl = c["l"], c["kl"]
            psu, kpsu = PS()
            MM(psu[:, :], GT, l, True, True, ["tri", kl], [kpsu])
            eK, keK = eK_ring.next()
            ACT(eK, psu[:, :], AF.Exp, [kpsu], [keK], scale=-1.0 / 16)
            kd, kkd = kd_ring.next()
            TT("dve", kd, c["ksb"], eK, ALU.mult, [c["kks"], keK], [kkd])
            ptot, kpt = PS()
            for h in range(4):
                MM(ptot[:, h:h + 1], l[:, h * 128:(h + 1) * 128], ones_f, True, True, [kl, "ones_f"], [kpt])
            dec, kdec = dec_ring.next()
            ACT(dec, ptot[:, 0:4], AF.Exp, [kpt], [kdec], scale=-1.0 / 16)
            c.update(kd=kd, kkd=kkd, dec=dec, kdec=kdec)

        def p1_B3(i, hs):
            c = p1[i]
            kd, kkd, dec, kdec, vsb, kv = (c[k] for k in ("kd", "kkd", "dec", "kdec", "vsb", "kv"))
            for h in hs:
                pkv, kpkv = PS()
                MM(pkv[:, 0:256], kd[:, h * 128:(h + 1) * 128], vsb[:, h * 256:(h + 1) * 256], True, True,
                   [kkd, (kv, 0), (kv, 1)], [kpkv])
                STT(Sst[:, h, :], Sst[:, h, :], dec[:, h:h + 1], pkv[:, 0:256], ALU.mult, ALU.add,
                    [("Sst", h), kdec, kpkv], [("Sst", h)])

        p1_N(0)
        if NT > 1:
            p1_N(1)
        p1_A(0)
        for i in range(NT):
            nx = i + 1 < NT
            p1_B1(i)
            if nx:
                p1_Ak(i + 1)
            p1_B2(i)
            if nx:
                p1_Av(i + 1, 0)
            p1_B3(i, (0, 1))
            if nx:
                p1_Av(i + 1, 1)
            p1_B3(i, (2, 3))
            if nx:
                p1_Alr(i + 1)
            if i + 2 < NT:
                p1_N(i + 2)
            p1.pop(i)
        CP("act", Sbf.rearrange("p a b -> p (a b)"), Sst.rearrange("p a b -> p (a b)"),
           [("Sst", h) for h in range(4)], [("Sbf", 0)])
        if debug:
            DMA("sp", dbg_S, Sst.rearrange("p a b -> p (a b)"), R=[("Sst", h) for h in range(4)])
        S.barrier()
        A.release(m_base)

        uT = A.alloc([KC, T], BF16)
        wgP_sb = A.alloc([512], F32, parts=17)
        wgM_sb = A.alloc([512], F32, parts=17)
        DMA("sp", wgP_sb, wgP, W=["wgP"])
        DMA("sp", wgM_sb, wgM, W=["wgM"])
        W_ring = Ring(A, "W", 3, [KC, 512], BF16)
        m2 = A.mark()
        xt_ring = Ring(A, "xt", 2, [D], F32)
        xs_ring = Ring(A, "xs", 2, [D], BF16)
        for i in range(NT):
            xt, kx = xt_ring.next()
            DMA("sp", xt, x_own[i * 128:(i + 1) * 128, :], W=[kx])
            norm_T(xt, kx, uT, i * 128, ("uT", i), xs_ring)
        S.barrier()
        A.release(m2)
        fst_ring = Ring(A, "fst", 3, [T], BF16)
        tst_ring = Ring(A, "tst", 2, [4, 512], BF16)
        lb_ring = Ring(A, "lb", 2, [4, 512], F32)
        e1b_ring = Ring(A, "e1b", 1, [4, 512], F32)
        lrTP = A.alloc([T], F32, parts=17)
        lrTM = A.alloc([T], F32, parts=17)
        MEMSET("dve", lrTP, 1.0, ["lrTP"])
        MEMSET("dve", lrTM, 1.0, ["lrTM"])
        uT_all = [("uT", i) for i in range(NT)]
        evq = [0]

        def ev_eng():
            evq[0] += 1
            return "act" if evq[0] % 2 else "dve"

        def proj_fm(Wt, kW, jl, dst_row, func=None, scale=None, c0=0):
            st, kst = fst_ring.next()
            for tg in range(NTG):
                p, kp = PS()
                for kc in range(KC):
                    MM(p[:, 0:GS], Wt[:, kc, jl * 128:(jl + 1) * 128], uT[:, kc, tg * GS:(tg + 1) * GS],
                       kc == 0, kc == KC - 1, [kW] + uT_all[tg * GS // 128:(tg + 1) * GS // 128], [kp])
                if func is not None:
                    ACT(st[:, tg * GS:(tg + 1) * GS], p[:, 0:GS], func, [kp], [], Wm=[kst])
                elif scale is not None:
                    e = ev_eng()
                    if e == "act":
                        ACT(st[:, tg * GS:(tg + 1) * GS], p[:, 0:GS], AF.Copy, [kp], [], scale=scale, Wm=[kst])
                    else:
                        TS("dve", st[:, tg * GS:(tg + 1) * GS], p[:, 0:GS], scale, ALU.mult, [kp], [], Wm=[kst])
                else:
                    CP(ev_eng(), st[:, tg * GS:(tg + 1) * GS], p[:, 0:GS], [kp], [], Wm=[kst])
            DMA("act", dst_row[:, c0:c0 + T], st, R=[kst], W=[])

        def proj_tm(Wt, kW, cl0, ncol, dst, dc0, r0=0):
            for i4 in range(0, NT, 4):
                n4 = min(4, NT - i4)
                st, kst = tst_ring.next()
                for ii in range(n4):
                    i = i4 + ii
                    p, kp = PS()
                    for kc in range(KC):
                        MM(p[:, 0:ncol], uT[:, kc, i * 128:(i + 1) * 128], Wt[:, kc, cl0:cl0 + ncol],
                           kc == 0, kc == KC - 1, [kW, ("uT", i)], [kp])
                    CP(ev_eng(), st[:, ii, 0:ncol], p[:, 0:ncol], [kp], [], Wm=[kst])
                DMA("act", dst[r0 + i4 * 128:r0 + (i4 + n4) * 128, dc0:dc0 + ncol].rearrange("(a p) c -> p a c", p=128),
                    st[:, 0:n4, 0:ncol], R=[kst], W=[])

        sc_a = 1.0 / math.sqrt(128.0)
        for blk in range(2):
            Wt, kW = W_ring.next()
            load_wb(Wt, w_in_b, IN_COLS, blk * 512, 512, kW, ("pre_w_in", (blk * 512) // 1024))
            for jl in range(4):
                proj_fm(Wt, kW, jl, qaT_d[blk * 4 + jl], scale=sc_a)
        Wt, kW = W_ring.next()
        load_wb(Wt, w_in_b, IN_COLS, 1024, 512, kW, ("pre_w_in", (1024) // 1024))
        for g in range(2):
            proj_fm(Wt, kW, g, kaT_d[g], c0=128)
        proj_tm(Wt, kW, 256, 256, va_d, 0, r0=128)
        Wt, kW = W_ring.next()
        load_wb(Wt, w_in_b, IN_COLS, 1536, 512, kW, ("pre_w_in", (1536) // 1024))
        for jl in range(4):
            proj_fm(Wt, kW, jl, qgT_d[jl], scale=sc_a)
        Wt, kW = W_ring.next()
        load_wb(Wt, w_in_b, IN_COLS, 2048, 512, kW, ("pre_w_in", (2048) // 1024))
        for jl in range(4):
            proj_fm(Wt, kW, jl, kgT_d[jl])
        proj_tm(Wt, kW, 0, 512, kg_d, 0)
        for blk in range(2):
            Wt, kW = W_ring.next()
            load_wb(Wt, w_in_b, IN_COLS, 2560 + blk * 512, 512, kW, ("pre_w_in", (2560 + blk * 512) // 1024))
            proj_tm(Wt, kW, 0, 512, vg_d, blk * 512)
        for blk in range(2):
            Wt, kW = W_ring.next()
            load_wb(Wt, w_in_b, IN_COLS, 3584 + blk * 512, 512, kW, ("pre_w_in", (3584 + blk * 512) // 1024))
            for jl in range(4):
                proj_fm(Wt, kW, jl, rgT_d[blk * 4 + jl], func=AF.Silu)
        Wt, kW = W_ring.next()
        load_wb(Wt[:, :, 0:32], w_in_b, IN_COLS, 4608, 32, kW, ("pre_w_in", (4608) // 1024))
        for dr, lrT in ((0, lrTP), (1, lrTM)):
            for tg in range(NTG):
                p, kp = PS()
                for kc in range(KC):
                    MM(p[0:16, 0:GS], Wt[:, kc, dr * 16:(dr + 1) * 16], uT[:, kc, tg * GS:(tg + 1) * GS],
                       kc == 0, kc == KC - 1, [kW] + uT_all[tg * GS // 128:(tg + 1) * GS // 128], [kp])
                CP("dve", lrT[0:16, tg * GS:(tg + 1) * GS], p[0:16, 0:GS], [kp], ["lrTP" if dr == 0 else "lrTM"])
        for dr, lrT, wg_sb, kwg, l_d in ((0, lrTP, wgP_sb, "wgP", lP_d), (1, lrTM, wgM_sb, "wgM", lM_d)):
            klr = "lrTP" if dr == 0 else "lrTM"
            for i4 in range(0, NT, 4):
                n4 = min(4, NT - i4)
                e1b, ke1b = e1b_ring.next()
                lb, klb = lb_ring.next()
                for ii in range(n4):
                    i = i4 + ii
                    pz, kpz = PS()
                    MM(pz[:, :], lrT[:, i * 128:(i + 1) * 128], wg_sb, True, True, [klr, kwg], [kpz])
                    ACT(e1b[:, ii, :], pz[:, :], AF.Exp, [kpz], [(ke1b, ii)], scale=-1.0)
                ACT(lb[:, 0:n4, :], e1b[:, 0:n4, :], AF.Ln, [(ke1b, ii) for ii in range(n4)], [klb], bias=1.0)
                DMA("sp", l_d[i4 * 128:(i4 + n4) * 128, :].rearrange("(a p) c -> p a c", p=128), lb[:, 0:n4, :],
                    R=[klb], W=[])
        S.barrier()
        A.release(m_base)

        amaskf = A.alloc([3, 8, 128], F32)
        amask = A.alloc([3, 8, 128], BF16)
        esink = A.alloc([8], F32)
        gacol = A.alloc([8], F32)
        DMA("sp", amaskf.rearrange("p a b c -> p (a b c)"), c_amask, W=["amaskf"])
        CP("dve", amask, amaskf, ["amaskf"], ["amask"])
        DMA("sp", esink, sink.partition_broadcast(128), W=["esink0"])
        DMA("sp", gacol, ga_col, W=["gacol"])
        ACT(esink, esink, AF.Exp, ["esink0"], ["esink"])
        NQG = GS // 128
        qa_ring = Ring(A, "qa", 2, [8, GS], BF16)
        ka_ring = Ring(A, "ka", 4, [2, 384], BF16)
        va_ring = Ring(A, "va", 5, [3, 256], BF16)
        E_ring = Ring(A, "E", 4, [512], F32)
        PT_ring = Ring(A, "PT", 20, [512], BF16)
        den_ring = Ring(A, "den", 2, [512], F32)
        rden_ring = Ring(A, "rden", 2, [512], F32)
        osb_ring = Ring(A, "osb", 3, [8, 128], F32)
        sq_ring = Ring(A, "sq", 2, [8, 128], BF16)
        rs_ring = Ring(A, "rs", 2, [128], F32)
        rstd_ring = Ring(A, "rstd", 2, [128], F32)
        oast_ring = Ring(A, "oast", 3, [8, 128], BF16)
        p3 = {}
        qa_cur = [None]

        def p3_S1(n):
            npos = 3 if n < NT - 1 else 2
            if n % NQG == 0:
                qa_cur[0] = qa_ring.next()
                t0 = n * 128
                DMA("sp", qa_cur[0][0], qaT_d[:, :, t0:t0 + GS].rearrange("h d t -> d h t"), R=[], W=[qa_cur[0][1]])
            qa, kqa = qa_cur[0]
            q0 = (n % NQG) * 128
            ka, kka = ka_ring.next()
            va, kva = va_ring.next()
            DMA("sp", ka[:, :, 0:npos * 128], kaT_d[:, :, n * 128:n * 128 + npos * 128].rearrange("g d t -> d g t"),
                R=[], W=[kka])
            DMA("sp", va[:, 0:npos, :], va_d[n * 128:n * 128 + npos * 128, :].rearrange("(b p) c -> p b c", p=128),
                R=[], W=[kva])
            PTs = {}
            for g in range(2):
                for pos in range(npos):
                    p, kp = PS()
                    MM(p[:, :].rearrange("p (h t) -> p h t", t=128), ka[:, g, pos * 128:(pos + 1) * 128],
                       qa[:, g * 4:(g + 1) * 4, q0:q0 + 128], True, False, [kka, kqa], [kp])
                    MM(p[:, :], ident, amask[:, pos, g * 4:(g + 1) * 4, :].rearrange("p h t -> p (h t)"), False, True,
                       ["ident", "amask"], [kp])
                    PT, kPT = PT_ring.next()
                    ACT(PT, p[:, :], AF.Exp, [kp], [kPT])
                    PTs[(g, pos)] = (PT, kPT)
            p3[n] = dict(npos=npos, va=va, kva=kva, PTs=PTs)

        def p3_S2(n):
            c = p3[n]
            npos, va, kva, PTs = c["npos"], c["va"], c["kva"], c["PTs"]
            osb, kosb = osb_ring.next()
            for g in range(2):
                po, kpo = PS()
                for pos in range(npos):
                    MM(po[:, :], va[:, pos, g * 128:(g + 1) * 128], PTs[(g, pos)][0], pos == 0, pos == npos - 1,
                       [kva, PTs[(g, pos)][1]], [kpo])
                pd, kpd = PS()
                for pos in range(npos):
                    MM(pd[:, :], ones_bf, PTs[(g, pos)][0], pos == 0, pos == npos - 1,
                       ["ones_bf", PTs[(g, pos)][1]], [kpd])
                den, kden = den_ring.next()
                TT("dve", den.rearrange("p (h t) -> p h t", t=128), pd[:, :].rearrange("p (h t) -> p h t", t=128),
                   esink[:, g * 4:(g + 1) * 4].unsqueeze(2).to_broadcast([128, 4, 128]), ALU.add,
                   [kpd, "esink"], [kden])
                rden, krden = rden_ring.next()
                RECIP(rden, den, [kden], [krden])
                TT("dve", osb[:, g * 4:(g + 1) * 4, :].rearrange("p h t -> p (h t)"), po[:, :], rden, ALU.mult,
                   [kpo, krden], [(kosb, g)])
            c.update(osb=osb, kosb=kosb)

        def p3_S3(n):
            c = p3.pop(n)
            osb, kosb = c["osb"], c["kosb"]
            sq, ksq = sq_ring.next()
            ACT(sq, osb, AF.Square, [(kosb, 0), (kosb, 1)], [ksq])
            pss, kpss = PS()
            for h in range(8):
                MM(pss[:, 0:128], ones_bf, sq[:, h, :], h == 0, h == 7, ["ones_bf", ksq], [kpss])
            rs, krs = rs_ring.next()
            ACT(rs, pss[:, 0:128], AF.Sqrt, [kpss], [krs], bias=EPS, scale=1.0 / 1024)
            rstd, krstd = rstd_ring.next()
            RECIP(rstd, rs, [krs], [krstd])
            oast, koast = oast_ring.next()
            for h in range(8):
                STT(oast[:, h, :], osb[:, h, :], gacol[:, h:h + 1], rstd, ALU.mult, ALU.mult,
                    [(kosb, 0), (kosb, 1), "gacol", krstd], [(koast, h)])
            DMA("act", A_d[n, :, 0:8, :], oast, R=[(koast, h) for h in range(8)], W=[])

        for step in range(NT + 2):
            if step < NT:
                p3_S1(step)
            if 0 <= step - 1 < NT:
                p3_S2(step - 1)
            if 0 <= step - 2 < NT:
                p3_S3(step - 2)
        S.barrier()
        A.release(m_base)

        oP = A.alloc([NT, 8, 128], F32)
        ggcol = A.alloc([2], F32)
        DMA("sp", ggcol, gg_col, W=["ggcol"])
        qg_ring = Ring(A, "qg", 3, [4, 128], BF16)
        kgT_ring = Ring(A, "kgT", 3, [4, 128], BF16)
        kg_ring = Ring(A, "kg", 3, [512], BF16)
        vg_ring = Ring(A, "vg", 3, [1024], BF16)
        lg_ring = Ring(A, "lg", 3, [512], F32)
        rg_ring = Ring(A, "rg", 2, [8, 128], BF16)
        eK_ring = Ring(A, "eK", 2, [512], F32)
        kd_ring = Ring(A, "kd", 3, [512], BF16)
        eP_ring = Ring(A, "eP", 3, [512], F32)
        eN_ring = Ring(A, "eN", 2, [512], F32)
        qd_ring = Ring(A, "qd", 3, [4, 128], BF16)
        ki_ring = Ring(A, "ki", 2, [4, 128], BF16)
        at_ring = Ring(A, "at", 3, [4, 128], BF16)
        og_ring = Ring(A, "og", 2, [8, 128], F32)
        sqg_ring = Ring(A, "sqg", 2, [8, 128], BF16)
        rsg_ring = Ring(A, "rsg", 2, [512], F32)
        rstdg_ring = Ring(A, "rstdg", 2, [512], F32)
        tmpg_ring = Ring(A, "tmpg", 2, [8, 128], F32)
        ogst_ring = Ring(A, "ogst", 3, [8, 128], BF16)
        Sk = [("Sst", h) for h in range(4)]
        Sbk = [("Sbf", h) for h in range(4)]
        Sbf2 = [Sbf, A.alloc([4, 256], BF16)]
        sb_par = [0]

        def gla_front(c, dr):
            l_d = lP_d if dr == 0 else lM_d
            TRI = LE if dr == 0 else GE
            STR = GT if dr == 0 else LT
            tsl = slice(c * 128, (c + 1) * 128)
            qg, kqg = qg_ring.next()
            kgT, kkgT = kgT_ring.next()
            kg, kkg = kg_ring.next()
            vg, kvg = vg_ring.next()
            lg, klg = lg_ring.next()
            DMA("sp", qg, qgT_d[:, :, tsl].rearrange("h d t -> d h t"), R=[], W=[kqg])
            DMA("sp", kgT, kgT_d[:, :, tsl].rearrange("h d t -> d h t"), R=[], W=[kkgT])
            DMA("sp", kg, kg_d[tsl, :], R=[], W=[kkg])
            DMA("sp", vg, vg_d[tsl, :], R=[], W=[kvg])
            DMA("sp", lg, l_d[tsl, :], R=[], W=[klg])
            psu, kpsu = PS()
            MM(psu[:, :], STR, lg, True, True, ["tri", klg], [kpsu])
            pb, kpb = PS()
            for h in range(4):
                MM(pb[:, h * 128:(h + 1) * 128], lg[:, h * 128:(h + 1) * 128], TRI, True, True, ["tri", klg], [kpb])
            eK, keK = eK_ring.next()
            ACT(eK, psu[:, :], AF.Exp, [kpsu], [keK], scale=-1.0 / 16)
            eP, keP = eP_ring.next()
            ACT(eP, pb[:, :], AF.Exp, [kpb], [keP], scale=-1.0 / 16)
            eN, keN = eN_ring.next()
            ACT(eN, pb[:, :], AF.Exp, [kpb], [keN], scale=1.0 / 16)
            kd, kkd = kd_ring.next()
            TT("dve", kd, kg, eK, ALU.mult, [kkg, keK], [kkd])
            qd, kqd = qd_ring.next()
            TT("dve", qd.rearrange("p h t -> p (h t)"), qg.rearrange("p h t -> p (h t)"), eP, ALU.mult,
               [kqg, keP], [kqd])
            ki, kki = ki_ring.next()
            TT("dve", ki.rearrange("p h t -> p (h t)"), kgT.rearrange("p h t -> p (h t)"), eN, ALU.mult,
               [kkgT, keN], [kki])
            pa, kpa = PS()
            for h in range(4):
                MM(pa[:, h * 128:(h + 1) * 128], ki[:, h, :], qd[:, h, :], True, True, [kki, kqd], [kpa])
            at, kat = at_ring.next()
            TT("dve", at, pa[:, :].rearrange("p (h t) -> p h t", t=128),
               TRI.unsqueeze(1).to_broadcast([128, 4, 128]), ALU.mult, [kpa, "tri"], [kat])
            return dict(c=c, dr=dr, vg=vg, kvg=kvg, kd=kd, kkd=kkd, eP=eP, keP=keP, qd=qd, kqd=kqd, at=at, kat=kat)

        def gla_back(f):
            c, dr = f["c"], f["dr"]
            vg, kvg, kd, kkd, eP, keP, qd, kqd, at, kat = (f[k] for k in
                                                         ("vg", "kvg", "kd", "kkd", "eP", "keP", "qd", "kqd", "at", "kat"))
            cur = sb_par[0]
            nxt = 1 - cur
            Scur, Snxt = Sbf2[cur], Sbf2[nxt]
            pkvs = []
            for hp in range(2):
                pkv, kpkv = PS()
                pkvs.append((pkv, kpkv))
                for hh in range(2):
                    h = 2 * hp + hh
                    MM(pkv[:, hh * 256:(hh + 1) * 256], kd[:, h * 128:(h + 1) * 128], vg[:, h * 256:(h + 1) * 256],
                       True, True, [kkd, kvg], [kpkv])
            pos_ = []
            for half in range(2):
                po, kpo = PS()
                pos_.append((po, kpo))
            for h in range(4):
                for dvc in range(2):
                    r = 2 * h + dvc
                    po, kpo = pos_[r // 4]
                    reg = po[:, (r % 4) * 128:(r % 4 + 1) * 128]
                    MM(reg, vg[:, h * 256 + dvc * 128:h * 256 + (dvc + 1) * 128], at[:, h, :], True, False,
                       [kvg, kat], [kpo])
                    MM(reg, Scur[:, h, dvc * 128:(dvc + 1) * 128], qd[:, h, :], False, True,
                       [("Sbf", cur), kqd], [kpo])
            dcol = 127 if dr == 0 else 0
            for h in range(4):
                pkv, kpkv = pkvs[h // 2]
                STT(Sst[:, h, :], Sst[:, h, :], eP[:, h * 128 + dcol:h * 128 + dcol + 1],
                    pkv[:, (h % 2) * 256:(h % 2 + 1) * 256], ALU.mult, ALU.add,
                    [("Sst", h), keP, kpkv], [("Sst", h)])
            CP("act", Snxt.rearrange("p a b -> p (a b)"), Sst.rearrange("p a b -> p (a b)"), Sk, [("Sbf", nxt)])
            sb_par[0] = nxt
            if dr == 0:
                for half in range(2):
                    po, kpo = pos_[half]
                    CP("act" if half == 0 else "dve", oP[:, c, half * 4:(half + 1) * 4, :].rearrange("p r t -> p (r t)"),
                       po[:, :], [kpo], [], Wm=[("oP", c)])
                return
            og, kog = og_ring.next()
            for half in range(2):
                po, kpo = pos_[half]
                TT("dve", og[:, half * 4:(half + 1) * 4, :].rearrange("p r t -> p (r t)"), po[:, :],
                   oP[:, c, half * 4:(half + 1) * 4, :].rearrange("p r t -> p (r t)"), ALU.add,
                   [kpo, ("oP", c)], [(kog, half)])
            sq, ksq = sqg_ring.next()
            ACT(sq, og, AF.Square, [(kog, 0), (kog, 1)], [ksq])
            pss, kpss = PS()
            for h in range(4):
                for dvc in range(2):
                    MM(pss[:, h * 128:(h + 1) * 128], ones_bf, sq[:, 2 * h + dvc, :], dvc == 0, dvc == 1,
                       ["ones_bf", ksq], [kpss])
            rs, krs = rsg_ring.next()
            ACT(rs, pss[:, :], AF.Sqrt, [kpss], [krs], bias=EPS, scale=1.0 / 256)
            rstd, krstd = rstdg_ring.next()
            RECIP(rstd, rs, [krs], [krstd])
            rg, krg = rg_ring.next()
            DMA("sp", rg, rgT_d[:, :, c * 128:(c + 1) * 128].rearrange("j d t -> d j t"), R=[], W=[krg])
            tmp, ktmp = tmpg_ring.next()
            for h in range(4):
                for dvc in range(2):
                    r = 2 * h + dvc
                    STT(tmp[:, r, :], og[:, r, :], ggcol[:, dvc:dvc + 1], rstd[:, h * 128:(h + 1) * 128],
                        ALU.mult, ALU.mult, [(kog, 0), (kog, 1), "ggcol", krstd], [(ktmp, r)])
            ogst, kogst = ogst_ring.next()
            TT("dve", ogst, tmp, rg, ALU.mult, [(ktmp, r) for r in range(8)] + [krg], [kogst])
            DMA("act", A_d[c, :, 8:16, :], ogst, R=[kogst], W=[])

        for dr in range(2):
            order = list(range(NT)) if dr == 0 else list(range(NT - 1, -1, -1))
            if dr == 1:
                MEMSET("dve", Sst, 0.0, Sk)
                MEMSET("dve", Sbf2[sb_par[0]], 0.0, [("Sbf", sb_par[0])])
            ogst_box = [None, None]
            fr = gla_front(order[0], dr)
            for ix, c in enumerate(order):
                nxt = gla_front(order[ix + 1], dr) if ix + 1 < NT else None
                fr["ogst"] = ogst_box
                fr["first_in_group"] = (ix % NQG == 0)
                fr["last_in_group"] = (ix % NQG == NQG - 1)
                gla_back(fr)
                fr = nxt
        S.barrier()
        A.release(m_base)

        emit_precast_experts(cfg.K1, KPRE)
        Wbig = A.alloc([KC, D], BF16)
        U = A.alloc([KC, T], BF16)
        m5 = A.mark()
        for blk in range(4):
            load_w("pool", Wbig[:, :, blk * 512:(blk + 1) * 512], w_out, blk * 512, 512, ("Wbig", blk))
        load_gain(g_cross)
        xt_ring = Ring(A, "xt", 3, [D], F32)
        xs_ring = Ring(A, "xs", 2, [D], BF16)
        aT_ring = Ring(A, "aT", 2, [KC, 128], BF16)
        p5 = {}

        def p5_A(i):
            tsl = slice(i * 128, (i + 1) * 128)
            aT, kaT = aT_ring.next()
            DMA("sp", aT, A_d[i], R=[], W=[(kaT, 0), (kaT, 1)])
            xt, kx = xt_ring.next()
            DMA("sp", xt, x_own[tsl, :], W=[kx])
            for blk in range(4):
                p, kp = PS()
                for fc in range(KC):
                    MM(p[:, :], aT[:, fc, :], Wbig[:, fc, blk * 512:(blk + 1) * 512], fc == 0, fc == KC - 1,
                       [(kaT, 0), (kaT, 1), ("Wbig", blk)], [kp])
                TT("dve", xt[:, blk * 512:(blk + 1) * 512], p[:, :], xt[:, blk * 512:(blk + 1) * 512], ALU.add,
                   [kp, kx], [kx])
            DMA("act", h_d[tsl, :], xt, R=[kx], W=[("h_d", i)])
            p5[i] = (xt, kx)

        p5_A(0)
        for i in range(NT):
            xt, kx = p5.pop(i)
            xs, kxs = norm_pre(xt, kx, xs_ring)
            if i + 1 < NT:
                p5_A(i + 1)
            norm_post(xs, kxs, U, i * 128, ("U", i))
        S.barrier()
        A.release(m5)

        Q = Wbig.rearrange("p k n -> p (k n)")[:, 0:KC * T].rearrange("p (k t) -> p k t", t=T)
        memT = A.alloc([KC, MEM], BF16)
        kcT = A.alloc([KC, MEM], BF16)
        vc = A.alloc([2, D], BF16)
        m6 = A.mark()
        load_gain(g_mem)
        xt_ring = Ring(A, "xt", 2, [D], F32)
        xs_ring = Ring(A, "xs", 2, [D], BF16)
        for mt in range(2):
            xt, kx = xt_ring.next()
            DMA("sp", xt, mem[mt * 128:(mt + 1) * 128, :], W=[kx])
            norm_T(xt, kx, memT, mt * 128, ("memT", mt), xs_ring)
        S.barrier()
        A.release(m6)
        W6 = Ring(A, "W6", 3, [KC, 256], BF16)
        mk = [("memT", 0), ("memT", 1)]
        for blk in range(8):
            Wt, kW = W6.next()
            load_w("pool", Wt, w_ck, blk * 256, 256, kW)
            for jl in range(2):
                p, kp = PS()
                for kc in range(KC):
                    MM(p[:, 0:MEM], Wt[:, kc, jl * 128:(jl + 1) * 128], memT[:, kc, :], kc == 0, kc == KC - 1,
                       [kW] + mk, [kp])
                CP(ev_eng(), kcT[:, blk * 2 + jl, :], p[:, 0:MEM], [kp], [("kcT", blk * 2 + jl)])
        for blk in range(8):
            Wt, kW = W6.next()
            load_w("pool", Wt, w_cv, blk * 256, 256, kW)
            for mt in range(2):
                p, kp = PS()
                for kc in range(KC):
                    MM(p[:, 0:256], memT[:, kc, mt * 128:(mt + 1) * 128], Wt[:, kc, :], kc == 0, kc == KC - 1,
                       [kW] + mk, [kp])
                CP(ev_eng(), vc[:, mt, blk * 256:(blk + 1) * 256], p[:, 0:256], [kp], [], Wm=[("vc", blk // 2)])
        sc_x = 1.0 / math.sqrt(512.0)
        U_all = [("U", i) for i in range(NT)]
        for blk in range(8):
            Wt, kW = W6.next()
            load_w("pool", Wt, w_cq, blk * 256, 256, kW)
            for jl in range(2):
                j = blk * 2 + jl
                for tg in range(NTG):
                    p, kp = PS()
                    for kc in range(KC):
                        MM(p[:, 0:GS], Wt[:, kc, jl * 128:(jl + 1) * 128], U[:, kc, tg * GS:(tg + 1) * GS],
                           kc == 0, kc == KC - 1, [kW] + U_all[tg * GS // 128:(tg + 1) * GS // 128], [kp])
                    if ev_eng() == "act":
                        ACT(Q[:, j, tg * GS:(tg + 1) * GS], p[:, 0:GS], AF.Copy, [kp], [("Q", j, tg)], scale=sc_x)
                    else:
                        TS("dve", Q[:, j, tg * GS:(tg + 1) * GS], p[:, 0:GS], sc_x, ALU.mult, [kp], [("Q", j, tg)])
        S.barrier()
        A.release(m6)
        PT_ring = Ring(A, "PTx", 4, [GS], BF16)
        rden_ring = Ring(A, "rdenx", 2, [GS], F32)
        for hd in range(4):
            for tg in range(NTG):
                PTs = []
                for mt in range(2):
                    p, kp = PS()
                    for cch in range(4):
                        j = hd * 4 + cch
                        MM(p[:, 0:GS], kcT[:, j, mt * 128:(mt + 1) * 128], Q[:, j, tg * GS:(tg + 1) * GS],
                           cch == 0, cch == 3, [("kcT", j), ("Q", j, tg)], [kp])
                    PT, kPT = PT_ring.next()
                    ACT(PT, p[:, 0:GS], AF.Exp, [kp], [kPT])
                    PTs.append((PT, kPT))
                pd, kpd = PS()
                for mt in range(2):
                    MM(pd[:, 0:GS], ones_bf, PTs[mt][0], mt == 0, mt == 1, ["ones_bf", PTs[mt][1]], [kpd])
                rden, krden = rden_ring.next()
                RECIP(rden, pd[:, 0:GS], [kpd], [krden])
                for cch in range(4):
                    j = hd * 4 + cch
                    po, kpo = PS()
                    for mt in range(2):
                        MM(po[:, 0:GS], vc[:, mt, j * 128:(j + 1) * 128], PTs[mt][0], mt == 0, mt == 1,
                           [("vc", j // 4), PTs[mt][1]], [kpo])
                    TT("dve", U[:, j, tg * GS:(tg + 1) * GS], po[:, 0:GS], rden, ALU.mult, [kpo, krden],
                       [("oc", j, tg)])
        S.barrier()
        A.release(m5)
        for blk in range(4):
            load_w("pool", Wbig[:, :, blk * 512:(blk + 1) * 512], w_co, blk * 512, 512, ("Wbig", blk))
        load_gain(g_ffn)
        Wr = A.alloc([KC, NR], BF16)
        Wrf = A.alloc([KC, NR], F32)
        DMA("sp", Wrf, w_r.rearrange("(k p) n -> p k n", p=128), W=["Wrf"])
        CP("dve", Wr, Wrf, ["Wrf"], ["Wr"])
        brb = A.alloc([NR], F32)
        DMA("sp", brb, b_r.partition_broadcast(128), W=["brb"])
        ecb = A.alloc([NE], F32)
        DMA("sp", ecb, c_ec.partition_broadcast(128), W=["ecb"])
        L_all = A.alloc([NT, NR], F32)
        idxf_all = A.alloc([NT * 2], F32)
        m6c = A.mark()
        xt_ring = Ring(A, "xt", 3, [D], F32)
        xs_ring = Ring(A, "xs", 2, [D], BF16)
        hT_ring = Ring(A, "hT", 2, [KC, 128], BF16)
        c6 = {}

        def c6_A(i):
            tsl = slice(i * 128, (i + 1) * 128)
            xt, kx = xt_ring.next()
            DMA("sp", xt, h_d[tsl, :], R=[("h_d", i)], W=[kx])
            for blk in range(4):
                p, kp = PS()
                for fc in range(KC):
                    MM(p[:, :], U[:, fc, tsl], Wbig[:, fc, blk * 512:(blk + 1) * 512], fc == 0, fc == KC - 1,
                       [("oc", fc, i * 128 // GS), ("Wbig", blk)], [kp])
                TT("dve", xt[:, blk * 512:(blk + 1) * 512], p[:, :], xt[:, blk * 512:(blk + 1) * 512], ALU.add,
                   [kp, kx], [kx])
            DMA("act", h_d[tsl, :], xt, R=[kx], W=[("h_d", i)])
            c6[i] = (xt, kx)

        def c6_Bpre(i):
            xt, kx = c6.pop(i)
            hn, khn = norm_pre(xt, kx, xs_ring)
            DMA("act", Hn_d[i * 128:(i + 1) * 128, :], hn, R=[khn], W=[])
            c6[("hn", i)] = (hn, khn)

        def c6_B(i):
            hn, khn = c6.pop(("hn", i))
            hT, khT = hT_ring.next()
            norm_post(hn, khn, hT, 0, khT)
            pr, kpr = PS()
            for kc in range(KC):
                MM(pr[:, 0:NR], hT[:, kc, :], Wr[:, kc, :], kc == 0, kc == KC - 1, [khT, "Wr"], [kpr])
            TT("dve", L_all[:, i, :], pr[:, 0:NR], brb, ALU.add, [kpr, "brb"], [("L_all", i)])

        c6_A(0)
        for i in range(NT):
            c6_Bpre(i)
            if i + 1 < NT:
                c6_A(i + 1)
            c6_B(i)
        S.barrier()
        A.release(m6c)

        NTE = NT * NE
        assert NTE <= 512
        tcnt = [0]

        def tmp(shape):
            tcnt[0] += 1
            return A.alloc(shape, F32), ("rt", tcnt[0])

        Lg = L_all[:, :, 0:NG]
        Le = L_all[:, :, NG:NR]
        gmax, kgmax = tmp([NT])
        RMAX(gmax, Lg, AX.X, [], [kgmax])
        gmb = gmax.unsqueeze(2).to_broadcast([128, NT, NG])
        eg, keg = tmp([NT, NG])
        TT("dve", eg, Lg, gmb, ALU.subtract, [kgmax], [keg])
        ACT(eg, eg, AF.Exp, [keg], [(keg, 1)])
        gsum, kgsum = tmp([NT])
        RSUM(gsum, eg, AX.X, [(keg, 1)], [kgsum])
        pgrp, kpgrp = tmp([NT])
        RECIP(pgrp, gsum, [kgsum], [kpgrp])
        pen, kpen = tmp([NT, NG])
        TT("dve", pen, Lg, gmb, ALU.is_equal, [kgmax], [kpen])
        TS("dve", pen, pen, 1.0, ALU.subtract, [kpen], [(kpen, 1)], s2=1e30, op1=ALU.mult)
        EL, kEL = tmp([NT, NE])
        TT("dve", EL.rearrange("p t (g e) -> p t g e", e=8), Le.rearrange("p t (g e) -> p t g e", e=8),
           pen.unsqueeze(3).to_broadcast([128, NT, NG, 8]), ALU.add, [(kpen, 1)], [kEL])
        m1, km1 = tmp([NT])
        RMAX(m1, EL, AX.X, [kEL], [km1])
        oh1, koh1 = tmp([NT, NE])
        TT("dve", oh1, EL, m1.unsqueeze(2).to_broadcast([128, NT, NE]), ALU.is_equal, [kEL, km1], [koh1])
        EL2, kEL2 = tmp([NT, NE])
        STT(EL2.rearrange("p t e -> p (t e)"), oh1.rearrange("p t e -> p (t e)"), -1e30,
            EL.rearrange("p t e -> p (t e)"), ALU.mult, ALU.add, [koh1, kEL], [kEL2])
        m2_, km2 = tmp([NT])
        RMAX(m2_, EL2, AX.X, [kEL2], [km2])
        oh2, koh2 = tmp([NT, NE])
        TT("dve", oh2, EL2, m2_.unsqueeze(2).to_broadcast([128, NT, NE]), ALU.is_equal, [kEL2, km2], [koh2])
        e2, ke2 = tmp([NT])
        TT("dve", e2, m2_, m1, ALU.subtract, [km1, km2], [ke2])
        ACT(e2, e2, AF.Exp, [ke2], [(ke2, 1)])
        rr, krr = tmp([NT])
        TS("dve", rr, e2, 1.0, ALU.add, [(ke2, 1)], [krr])
        RECIP(rr, rr, [krr], [(krr, 1)])
        w3 = w_all.rearrange("p (t k) -> p t k", k=2)
        TT("dve", w3[:, :, 0], pgrp, rr, ALU.mult, [kpgrp, (krr, 1)], [("w_all", 0)])
        TT("dve", w3[:, :, 1], pgrp, w3[:, :, 0], ALU.subtract, [kpgrp, ("w_all", 0)], [("w_all", 1)])
        Ab, kAb = tmp([NT, NE])
        TT("dve", Ab, oh1, oh2, ALU.add, [koh1, koh2], [kAb])
        Ab2 = Ab.rearrange("p t e -> p (t e)")
        pk_, kpk_ = PS()
        MM(pk_[:, 0:NTE], LT, Ab2, True, True, ["tri", kAb], [kpk_])
        pc_, kpc_ = PS()
        MM(pc_[:, 0:NTE], onesF, Ab2, True, True, ["onesF", kAb], [kpc_])
        cnt, kcnt = tmp([NT, NE])
        CP("act", cnt.rearrange("p t e -> p (t e)"), pc_[:, 0:NTE], [kpc_], [kcnt])
        base, kbase = tmp([NT, NE])
        MEMSET("dve", base[:, 0, :], 0.0, [(kbase, 0)])
        for t in range(1, NT):
            TT("dve", base[:, t, :], base[:, t - 1, :], cnt[:, t - 1, :], ALU.add, [(kbase, t - 1), kcnt], [(kbase, t)])
        kbase_all = [(kbase, t) for t in range(NT)]
        RE, kRE = tmp([NT, NE])
        TT("dve", RE.rearrange("p t e -> p (t e)"), pk_[:, 0:NTE], base.rearrange("p t e -> p (t e)"), ALU.add,
           [kpk_] + kbase_all, [kRE])
        REc, kREc = tmp([NT, NE])
        TT("dve", REc, RE, ecb.unsqueeze(1).to_broadcast([128, NT, NE]), ALU.add, [kRE, "ecb"], [kREc])
        ix3 = idxf_all.rearrange("p (t k) -> p t k", k=2)
        ii3 = idx_all.rearrange("p (t k) -> p t k", k=2)
        for k, (oh, koh) in enumerate(((oh1, koh1), (oh2, koh2))):
            t1, kt1 = tmp([NT, NE])
            TT("dve", t1, oh, REc, ALU.mult, [koh, kREc], [kt1])
            ix, kix = tmp([NT])
            RSUM(ix, t1, AX.X, [kt1], [kix])
            TT("dve", t1, oh, RE, ALU.mult, [koh, kRE, kix], [(kt1, 1)])
            rsel, krsel = tmp([NT])
            RSUM(rsel, t1, AX.X, [(kt1, 1)], [krsel])
            TS("dve", rsel, rsel, float(C) - 0.5, ALU.is_gt, [krsel], [(krsel, 1)], s2=1e7, op1=ALU.mult)
            TT("dve", ix3[:, :, k], ix, rsel, ALU.add, [kix, (krsel, 1)], [("idxf", k)])
            CP("dve", ii3[:, :, k], ix3[:, :, k], [("idxf", k)], [("idx_all", k)])
        hn_ring = Ring(A, "hnr", 3, [D], BF16)
        for i in range(NT):
            hn, khn = hn_ring.next()
            DMA("sp", hn, Hn_d[i * 128:(i + 1) * 128, :], W=[khn])
            for k in range(2):
                S.op("pool", (lambda hn_, ixap: (lambda e: e.indirect_dma_start(
                    out=Xs_d, out_offset=bass.IndirectOffsetOnAxis(ap=ixap, axis=0), in_=hn_, in_offset=None,
                    bounds_check=BC(e), oob_is_err=False)))(hn, idx_all[:, 2 * i + k:2 * i + k + 1]),
                    R=[khn, ("idx_all", k)], W=[], dma=True)
        if debug:
            DMA("sp", dbg_idx, idxf_all, R=[("idxf", k) for k in range(2)])
            DMA("sp", dbg_w, w_all, R=[("w_all", k) for k in range(2)])
        S.barrier()
        A.release(m_base)

        CT = C // 128
        Wx = Ring(A, "Wx", 8, [KC, 512], BF16)
        xtm_ring = Ring(A, "xtm", 2, [CT, D], BF16)
        xsT_ring = Ring(A, "xsT", 2, [KC, C], BF16)
        hid_ring = Ring(A, "hid", 2, [8, C], BF16)
        sg_ring = Ring(A, "sg", 4, [C], F32)
        y_ring = Ring(A, "ysb", 2, [D], BF16)
        for e_ in range(NE):
            Wg2, Wu2, Wd2 = [], [], []
            for hf in range(2):
                wg_, kg_ = Wx.next()
                wu_, ku_ = Wx.next()
                if e_ < KPRE:
                    DMA("sp", wg_, wg_b[e_].rearrange("p (k n) -> p k n", n=1024)[:, :, hf * 512:(hf + 1) * 512],
                        R=[("pre_g", e_)], W=[kg_])
                    DMA("sp", wu_, wu_b[e_].rearrange("p (k n) -> p k n", n=1024)[:, :, hf * 512:(hf + 1) * 512],
                        R=[("pre_u", e_)], W=[ku_])
                else:
                    DMA("pool", wg_, w_gate[e_][:, hf * 512:(hf + 1) * 512].rearrange("(k p) n -> p k n", p=128), W=[kg_])
                    DMA("pool", wu_, w_up[e_][:, hf * 512:(hf + 1) * 512].rearrange("(k p) n -> p k n", p=128), W=[ku_])
                Wg2.append((wg_, kg_))
                Wu2.append((wu_, ku_))
            for hf in range(2):
                wd_, kd_ = Wx.next()
                wdv = wd_.rearrange("p k n -> p (k n)").rearrange("p (k n) -> p k n", n=D)
                if e_ < KPRE:
                    DMA("sp", wdv, wd_b[e_].rearrange("p (k n) -> p k n", n=D)[:, hf * 4:(hf + 1) * 4, :],
                        R=[("pre_d", e_)], W=[kd_])
                else:
                    DMA("pool", wdv, w_down[e_][hf * 512:(hf + 1) * 512, :].rearrange("(k p) n -> p k n", p=128), W=[kd_])
                Wd2.append((wdv, kd_))
            if e_ == 0:
                xtm_n = xtm_ring.next()
                DMA("act", xtm_n[0], Xs_d[0:C, :].rearrange("(a p) d -> p a d", p=128), W=[xtm_n[1]])
            xtm, kxtm = xtm_n
            if e_ + 1 < NE:
                xtm_n = xtm_ring.next()
                DMA("act", xtm_n[0], Xs_d[(e_ + 1) * C:(e_ + 2) * C, :].rearrange("(a p) d -> p a d", p=128),
                    W=[xtm_n[1]])
            xsT, kxsT = xsT_ring.next()
            for r in range(CT):
                for b in range(2):
                    pt, kp = PS()
                    pb = pt[:, :].bitcast(BF16)
                    for j in range(8):
                        kc = b * 8 + j
                        TR(pb[:, j * 128:(j + 1) * 128], xtm[:, r, kc * 128:(kc + 1) * 128], [kxtm, "ident"], [kp])
                    CP(ev_eng(), xsT[:, b * 8:(b + 1) * 8, r * 128:(r + 1) * 128],
                       pb.rearrange("p (a b) -> p a b", b=128), [kp], [(kxsT, r, b)])
            kxs_all = [(kxsT, r, b) for r in range(CT) for b in range(2)]
            hid, khid = hid_ring.next()
            for j in range(8):
                pg, kpg = PS()
                Wg, kWg = Wg2[j // 4]
                Wu, kWu = Wu2[j // 4]
                jl = j % 4
                for kc in range(KC):
                    MM(pg[:, 0:C], Wg[:, kc, jl * 128:(jl + 1) * 128], xsT[:, kc, :], kc == 0, kc == KC - 1,
                       [kWg] + kxs_all, [kpg])
                pu, kpu = PS()
                for kc in range(KC):
                    MM(pu[:, 0:C], Wu[:, kc, jl * 128:(jl + 1) * 128], xsT[:, kc, :], kc == 0, kc == KC - 1,
                       [kWu] + kxs_all, [kpu])
                sg, ksg = sg_ring.next()
                ACT(sg, pg[:, 0:C], AF.Silu, [kpg], [ksg])
                TT("dve", hid[:, j, :], pu[:, 0:C], sg, ALU.mult, [kpu, ksg], [(khid, j)])
            khid_all = [(khid, j) for j in range(8)]
            for r in range(CT):
                ysb, ky = y_ring.next()
                for blk in range(4):
                    p, kp = PS()
                    for j in range(8):
                        MM(p[:, :], hid[:, j, r * 128:(r + 1) * 128], Wd2[j // 4][0][:, j % 4, blk * 512:(blk + 1) * 512],
                           j == 0, j == 7, khid_all + [Wd2[j // 4][1]], [kp])
                    CP(ev_eng(), ysb[:, blk * 512:(blk + 1) * 512], p[:, :], [kp], [], Wm=[ky])
                DMA("act", Y_d[e_ * C + r * 128:e_ * C + (r + 1) * 128, :], ysb, R=[ky], W=[])
        S.barrier()
        A.release(m_base)

        load_gain(g_final)
        xt_ring = Ring(A, "xt", 2, [D], F32)
        y1_ring = Ring(A, "y1", 3, [D], BF16)
        y2_ring = Ring(A, "y2", 3, [D], BF16)
        o_ring = Ring(A, "o", 2, [D], F32)
        for i in range(NT):
            tsl = slice(i * 128, (i + 1) * 128)
            xt, kx = xt_ring.next()
            DMA("sp", xt, h_d[tsl, :], W=[kx])
            ys = []
            for k, ring in enumerate((y1_ring, y2_ring)):
                yb, kyb = ring.next()
                S.op("pool", (lambda yb_, ixap: (lambda e: e.indirect_dma_start(
                    out=yb_, out_offset=None, in_=Y_d, in_offset=bass.IndirectOffsetOnAxis(ap=ixap, axis=0),
                    bounds_check=BC(e), oob_is_err=False)))(yb, idx_all[:, 2 * i + k:2 * i + k + 1]),
                    R=[], W=[kyb], dma=True)
                ys.append((yb, kyb))
            STT(xt, ys[0][0], w_all[:, 2 * i:2 * i + 1], xt, ALU.mult, ALU.add, [ys[0][1], kx], [kx])
            STT(xt, ys[1][0], w_all[:, 2 * i + 1:2 * i + 2], xt, ALU.mult, ALU.add, [ys[1][1], kx], [kx])
            ss, kss = small.next()
            rs, krs = small.next()
            rstd, krstd = small.next()
            ACT(junk, xt, AF.Square, [kx], [kss], accum=ss)
            ACT(rs, ss, AF.Sqrt, [kss], [krs], bias=EPS, scale=1.0 / D)
            RECIP(rstd, rs, [krs], [krstd])
            ot, ko = o_ring.next()
            STT(ot, xt, rstd, gainb, ALU.mult, ALU.mult, [kx, krstd, "gainb"], [ko])
            DMA("act", out[tsl, :], ot, R=[ko], W=[("out", i)])
        S.barrier(final=True)
        S.emit()
        nc._marks = S.marks
    return nc


def _consts(cfg):
    s = np.arange(128)[:, None]
    t = np.arange(128)[None, :]
    tri = np.stack([(s <= t), (s >= t), (s > t), (s < t)], axis=1).astype(np.float32)
    slopes = 2.0 ** (-8.0 * (np.arange(8, dtype=np.float64) + 1.0) / 8.0)
    k = np.arange(128, dtype=np.float64)[:, None]
    q = np.arange(128, dtype=np.float64)[None, :]
    am = np.zeros((128, 3, 8, 128), np.float64)
    for pos in range(3):
        if pos == 0:
            dist = q + 128 - k
        elif pos == 1:
            dist = np.abs(q - k)
        else:
            dist = k + 128 - q
        valid = (dist <= 128)
        for h in range(8):
            am[:, pos, h, :] = np.where(valid, -slopes[h] * dist, -30000.0)
    ec = (np.arange(cfg.NE, dtype=np.float32) * cfg.C)[None, :]
    return dict(c_ident=np.eye(128, dtype=np.float32), c_tri=np.ascontiguousarray(tri),
                c_amask=np.ascontiguousarray(am.reshape(128, -1).astype(np.float32)), c_ec=ec)


def make_in_maps(inputs, cfg, n_cores):
    f32 = lambda a: np.ascontiguousarray(np.asarray(a, dtype=np.float32))
    T = cfg.T
    x = f32(inputs["x"])
    mem = f32(inputs["mem"])
    w_in = f32(inputs["w_in"][0])
    w_in_sw = np.concatenate([w_in[:, :4608], w_in[:, 4624:4640], w_in[:, 4608:4624]], axis=1)
    w_in_sw = np.ascontiguousarray(w_in_sw)
    wgf = f32(np.concatenate([inputs["w_gla_gf"][0], inputs["b_gla_gf"][0][None, :]], axis=0))
    wgb = f32(np.concatenate([inputs["w_gla_gb"][0], inputs["b_gla_gb"][0][None, :]], axis=0))
    shared = dict(
        g_mix=f32(inputs["norm_mix"][0][None, :]), g_cross=f32(inputs["norm_cross"][0][None, :]),
        g_mem=f32(inputs["norm_mem"][0][None, :]), g_ffn=f32(inputs["norm_ffn"][0][None, :]),
        g_final=f32(inputs["norm_final"][None, :]),
        ga_col=f32(np.asarray(inputs["attn_out_norm"][0]).reshape(8, 128).T),
        sink=f32(inputs["sink_logit"][0][None, :]),
        gg_col=f32(np.asarray(inputs["gla_out_norm"][0]).reshape(2, 128).T),
        w_out=f32(inputs["w_out"][0]), w_cq=f32(inputs["w_cq"][0]), w_ck=f32(inputs["w_ck"][0]),
        w_cv=f32(inputs["w_cv"][0]), w_co=f32(inputs["w_co"][0]),
        w_r=f32(np.concatenate([inputs["w_router_group"][0], inputs["w_router_expert"][0]], axis=1)),
        b_r=f32(np.concatenate([inputs["b_router_group"][0], inputs["b_router_expert"][0]])[None, :]),
        w_gate=f32(inputs["w_gate"][0]), w_up=f32(inputs["w_up"][0]), w_down=f32(inputs["w_down"][0]),
    )
    shared.update(_consts(cfg))
    maps = []
    for c in range(n_cores):
        b, half = c // 2, c % 2
        m = dict(shared)
        if half == 1:
            m["x_own"] = np.ascontiguousarray(x[b, T:2 * T])
            m["x_oth"] = np.ascontiguousarray(x[b, 0:T])
            m["w_in"] = w_in
            m["wgP"], m["wgM"] = wgf, wgb
        else:
            m["x_own"] = np.ascontiguousarray(x[b, 0:T][::-1])
            m["x_oth"] = np.ascontiguousarray(x[b, T:2 * T][::-1])
            m["w_in"] = w_in_sw
            m["wgP"], m["wgM"] = wgb, wgf
        m["mem"] = mem[b]
        maps.append(m)
    return maps


def assemble(results, cfg, n_cores, key="out"):
    T = cfg.T
    B = n_cores // 2
    outp = np.empty((B, 2 * T, D), np.float32)
    for c in range(n_cores):
        b, half = c // 2, c % 2
        o = np.asarray(results[c][key])
        if half == 1:
            outp[b, T:2 * T] = o
        else:
            outp[b, 0:T] = o[::-1]
    return outp


_NC_CACHE = {}


def kernel(**inputs):
    cfg = Cfg()
    if "nc" not in _NC_CACHE:
        _NC_CACHE["nc"] = build(cfg)
    nc = _NC_CACHE["nc"]
    in_maps = make_in_maps(inputs, cfg, 8)
    res = run_bass_kernel_spmd(nc, in_maps, core_ids=list(range(8)))
    return assemble(res.results, cfg, 8)
```
